# Optimizing a Trainium2 kernel written in Bass

```python
import math
import jax, jax.numpy as jnp
from jax import lax
import numpy as np

D_MODEL = 1024
BATCH = 1
SEQ = 16384
DEPTH = 2

CTX_LEN = 256
GRID_W = 64
N_MIXERS = 2
N_HYENA_LAYERS = (DEPTH + 1) // 2
N_ATTN_LAYERS = DEPTH // 2
N_MOD = 6
NORM_EPS = 1e-6
HY_ORDER = 2
HY_SHORT = 3
HY_EMB_BANDS = 16
HY_EMB_DIM = 1 + 2 * HY_EMB_BANDS
HY_FILTER_HIDDEN = 64
HY_DECAY_FAST = 0.3
HY_DECAY_SLOW = 1.5
HY_DECAY_TARGET = 1e-2
HY_FILTER_EPS = 1e-6
DA_HEADS = 8
DA_HEAD_DIM = D_MODEL // DA_HEADS // 2
DA_V_DIM = 2 * DA_HEAD_DIM
ROPE_AXIS_DIM = DA_HEAD_DIM // 2
ROPE_THETA = 10000.0
Q_BLOCK = 128
SUBLN_EPS = 1e-5
N_EXPERTS = 16
EC_CAPACITY = 2
D_EXPERT = 1024

kernel_name = "hyena_diffattn_ec_moe_diffusion_trunk"


def rms_norm(x, g, eps=NORM_EPS):
    xf = x.astype(jnp.float32)
    y = xf * lax.rsqrt(jnp.mean(xf * xf, axis=-1, keepdims=True) + eps) * g.astype(jnp.float32)
    return y.astype(x.dtype)


def adaln(cond, w, b):
    return (jax.nn.silu(cond) @ w + b).reshape(cond.shape[0], N_MOD, 1, D_MODEL)


def modulate(h, shift, scale):
    return h * (1.0 + scale) + shift


def short_conv(u, w, b):
    L = u.shape[1]
    pad = HY_SHORT // 2
    up = jnp.pad(u, ((0, 0), (pad, pad), (0, 0)))
    out = b
    for k in range(HY_SHORT):
        out = out + up[:, k:k + L] * w[k]
    return out


def hyena_filter_spectrum(L, w1, b1, w2, b2, w3, b3, w4, freq):
    f32 = jnp.float32
    t = jnp.linspace(0.0, 1.0, L, dtype=f32)[:, None]
    w = 2.0 * math.pi * jnp.arange(L, dtype=f32)[:, None] / L
    bands = jnp.linspace(1e-4, HY_EMB_BANDS - 1, HY_EMB_BANDS, dtype=f32)[None]
    z = jnp.concatenate([t, jnp.cos(w * bands), -jnp.sin(w * bands)], axis=-1)
    fr = freq.astype(f32)
    hid = jnp.sin(fr * (z @ w1.astype(f32) + b1.astype(f32)))
    hid = jnp.sin(fr * (hid @ w2.astype(f32) + b2.astype(f32)))
    hid = jnp.sin(fr * (hid @ w3.astype(f32) + b3.astype(f32)))
    h = (hid @ w4.astype(f32)).reshape(L, HY_ORDER, 2, D_MODEL)
    max_decay = math.log(HY_DECAY_TARGET) / HY_DECAY_FAST
    min_decay = math.log(HY_DECAY_TARGET) / HY_DECAY_SLOW
    deltas = jnp.linspace(min_decay, max_decay, D_MODEL, dtype=f32)
    decay = jnp.exp(-t * jnp.abs(deltas))
    h = h * decay[:, None, None, :]
    k = jnp.concatenate([h[:, :, 0], jnp.zeros((1, HY_ORDER, D_MODEL), f32), h[:0:-1, :, 1]], axis=0)
    k = k / (jnp.sum(jnp.abs(k), axis=0, keepdims=True) + HY_FILTER_EPS)
    return jnp.fft.rfft(k, axis=0)


def long_conv(z, K):
    L = z.shape[1]
    Z = jnp.fft.rfft(z.astype(jnp.float32), n=2 * L, axis=1)
    y = jnp.fft.irfft(Z * K[None], n=2 * L, axis=1)[:, :L]
    return y.astype(z.dtype)


def hyena_mix(h, w_in, b_in, conv_w, conv_b, f_w1, f_b1, f_w2, f_b2, f_w3, f_b3, f_w4, f_freq,
              skip, w_out, b_out):
    L = h.shape[1]
    u = short_conv(h @ w_in + b_in, conv_w, conv_b)
    v, x1, x2 = jnp.split(u, 3, axis=-1)
    K = hyena_filter_spectrum(L, f_w1, f_b1, f_w2, f_b2, f_w3, f_b3, f_w4, f_freq)
    z = v
    for o, gate in enumerate((x1, x2)):
        z = gate * (long_conv(z, K[:, o]) + skip[o] * z)
    return z @ w_out + b_out


def axial_rope(x, row, col):
    f32 = jnp.float32
    inv = ROPE_THETA ** (-jnp.arange(0, ROPE_AXIS_DIM, 2, dtype=f32) / ROPE_AXIS_DIM)

    def rot(xa, pos):
        ang = pos.astype(f32)[:, None] * inv[None]
        cos = jnp.cos(ang)[None, :, None, :]
        sin = jnp.sin(ang)[None, :, None, :]
        a1, a2 = jnp.split(xa.astype(f32), 2, axis=-1)
        return jnp.concatenate([a1 * cos - a2 * sin, a2 * cos + a1 * sin], axis=-1)

    xr, xc = jnp.split(x, 2, axis=-1)
    return jnp.concatenate([rot(xr, row), rot(xc, col)], axis=-1).astype(x.dtype)


def diff_core(q, k, v, lam):
    B, Q = q.shape[0], q.shape[1]
    s = jnp.einsum("bqcd,bkcd->bcqk", q, k).astype(jnp.float32) * (DA_HEAD_DIM ** -0.5)
    p = jax.nn.softmax(s, axis=-1).reshape(B, DA_HEADS, 2, Q, k.shape[1])
    a = p[:, :, 0] - lam * p[:, :, 1]
    return jnp.einsum("bhqk,bkhe->bqhe", a.astype(v.dtype), v)


def diff_attention(hx, hc, row, col, lam_init, need_ctx_out, w_qkv, q_norm, k_norm,
                   lam_q1, lam_k1, lam_q2, lam_k2, subln, w_out):
    B, N, _ = hx.shape
    f32 = jnp.float32
    lam = (jnp.exp(jnp.sum(lam_q1.astype(f32) * lam_k1.astype(f32)))
           - jnp.exp(jnp.sum(lam_q2.astype(f32) * lam_k2.astype(f32))) + lam_init)

    def heads_q(q):
        return rms_norm(q.reshape(q.shape[0], q.shape[1], 2 * DA_HEADS, DA_HEAD_DIM), q_norm)

    def heads_k(k):
        return rms_norm(k.reshape(k.shape[0], k.shape[1], 2 * DA_HEADS, DA_HEAD_DIM), k_norm)

    def heads_v(v):
        return v.reshape(v.shape[0], v.shape[1], DA_HEADS, DA_V_DIM)

    def finish(o):
        o = rms_norm(o, subln, SUBLN_EPS) * (1.0 - lam_init)
        return o.reshape(o.shape[0], o.shape[1], D_MODEL) @ w_out

    qx, kx, vx = jnp.split(hx @ w_qkv, 3, axis=-1)
    qx = axial_rope(heads_q(qx), row, col)
    kx = axial_rope(heads_k(kx), row, col)
    vx = heads_v(vx)
    kc, vc = jnp.split(hc @ w_qkv[:, D_MODEL:], 2, axis=-1)
    kc = heads_k(kc)
    vc = heads_v(vc)
    k_all = jnp.concatenate([kc, kx], axis=1)
    v_all = jnp.concatenate([vc, vx], axis=1)
    n_blk = N // Q_BLOCK
    qb = qx.reshape(B, n_blk, Q_BLOCK, 2 * DA_HEADS, DA_HEAD_DIM).swapaxes(0, 1)
    ob = lax.map(lambda qq: diff_core(qq, k_all, v_all, lam), qb)
    yx = finish(ob.swapaxes(0, 1).reshape(B, N, DA_HEADS, DA_V_DIM))
    yc = None
    if need_ctx_out:
        qc = heads_q(hc @ w_qkv[:, :D_MODEL])
        yc = finish(diff_core(qc, kc, vc, lam))
    return yx, yc


def ec_moe(h, w_router, w_gate, w_up, w_down):
    B, N, _ = h.shape
    cap = EC_CAPACITY * N // N_EXPERTS
    aff = jax.nn.softmax((h @ w_router).astype(jnp.float32), axis=-1)
    g, idx = lax.top_k(aff.swapaxes(1, 2), cap)
    bidx = jnp.arange(B)[:, None]
    xe = h[bidx, idx.reshape(B, -1)].reshape(B, N_EXPERTS, cap, D_MODEL)
    a = jax.nn.silu(jnp.einsum("becd,edf->becf", xe, w_gate)) * jnp.einsum("becd,edf->becf", xe, w_up)
    y = jnp.einsum("becf,efd->becd", a, w_down) * g[..., None].astype(h.dtype)
    return jnp.zeros_like(h).at[jnp.arange(B)[:, None, None], idx].add(y)


def setup_inputs(seed: int = 0) -> dict:
    key = jax.random.key(seed)
    ks = jax.random.split(key, 40)
    D, F, E = D_MODEL, D_EXPERT, N_EXPERTS
    NHy, NA = N_HYENA_LAYERS, N_ATTN_LAYERS

    def nrm(i, shape, scale):
        return jax.random.normal(ks[i], shape, jnp.float32) * scale

    return {
        "x": nrm(0, (BATCH, SEQ, D), 1.0),
        "c": nrm(1, (BATCH, D), 1.0),
        "ctx": nrm(2, (BATCH, CTX_LEN, D), 1.0),
        "c_ctx": nrm(3, (D,), 1.0),
        "ada_w": nrm(4, (DEPTH, D, N_MOD * D), 0.5 * D ** -0.5),
        "ada_b": nrm(5, (DEPTH, N_MOD * D), 0.02),
        "norm_mix": 1.0 + nrm(6, (DEPTH, D), 0.02),
        "norm_ffn": 1.0 + nrm(7, (DEPTH, D), 0.02),
        "hy_w_in": nrm(8, (NHy, D, 3 * D), D ** -0.5),
        "hy_b_in": nrm(9, (NHy, 3 * D), 0.02),
        "hy_conv_w": nrm(10, (NHy, HY_SHORT, 3 * D), HY_SHORT ** -0.5),
        "hy_conv_b": nrm(11, (NHy, 3 * D), 0.02),
        "hy_f_w1": nrm(12, (NHy, HY_EMB_DIM, HY_FILTER_HIDDEN), HY_EMB_DIM ** -0.5),
        "hy_f_b1": nrm(13, (NHy, HY_FILTER_HIDDEN), 0.1),
        "hy_f_w2": nrm(14, (NHy, HY_FILTER_HIDDEN, HY_FILTER_HIDDEN), HY_FILTER_HIDDEN ** -0.5),
        "hy_f_b2": nrm(15, (NHy, HY_FILTER_HIDDEN), 0.1),
        "hy_f_w3": nrm(16, (NHy, HY_FILTER_HIDDEN, HY_FILTER_HIDDEN), HY_FILTER_HIDDEN ** -0.5),
        "hy_f_b3": nrm(17, (NHy, HY_FILTER_HIDDEN), 0.1),
        "hy_f_w4": nrm(18, (NHy, HY_FILTER_HIDDEN, HY_ORDER * 2 * D), HY_FILTER_HIDDEN ** -0.5),
        "hy_f_freq": 1.0 + nrm(19, (NHy, HY_FILTER_HIDDEN), 0.02),
        "hy_skip": nrm(20, (NHy, HY_ORDER, D), 0.5),
        "hy_w_out": nrm(21, (NHy, D, D), D ** -0.5),
        "hy_b_out": nrm(22, (NHy, D), 0.02),
        "da_w_qkv": nrm(23, (NA, D, 3 * D), D ** -0.5),
        "da_q_norm": 1.0 + nrm(24, (NA, DA_HEAD_DIM), 0.02),
        "da_k_norm": 1.0 + nrm(25, (NA, DA_HEAD_DIM), 0.02),
        "da_lam_q1": nrm(26, (NA, DA_HEAD_DIM), 0.1),
        "da_lam_k1": nrm(27, (NA, DA_HEAD_DIM), 0.1),
        "da_lam_q2": nrm(28, (NA, DA_HEAD_DIM), 0.1),
        "da_lam_k2": nrm(29, (NA, DA_HEAD_DIM), 0.1),
        "da_subln": 1.0 + nrm(30, (NA, DA_V_DIM), 0.02),
        "da_w_out": nrm(31, (NA, D, D), D ** -0.5),
        "moe_router": nrm(32, (DEPTH, D, E), D ** -0.5),
        "moe_w_gate": nrm(33, (DEPTH, E, D, F), D ** -0.5),
        "moe_w_up": nrm(34, (DEPTH, E, D, F), D ** -0.5),
        "moe_w_down": nrm(35, (DEPTH, E, F, D), F ** -0.5),
    }


def reference(x, c, ctx, c_ctx, ada_w, ada_b, norm_mix, norm_ffn,
              hy_w_in, hy_b_in, hy_conv_w, hy_conv_b, hy_f_w1, hy_f_b1, hy_f_w2, hy_f_b2,
              hy_f_w3, hy_f_b3, hy_f_w4, hy_f_freq, hy_skip, hy_w_out, hy_b_out,
              da_w_qkv, da_q_norm, da_k_norm, da_lam_q1, da_lam_k1, da_lam_q2, da_lam_k2,
              da_subln, da_w_out, moe_router, moe_w_gate, moe_w_up, moe_w_down):
    n_lat = x.shape[1]
    rows = n_lat // GRID_W
    row = jnp.repeat(jnp.arange(rows, dtype=jnp.int32), GRID_W)
    col = jnp.tile(jnp.arange(GRID_W, dtype=jnp.int32), rows)
    for i in range(DEPTH):
        last = i == DEPTH - 1
        j = i // N_MIXERS
        mx = adaln(c, ada_w[i], ada_b[i])
        mc = adaln(c_ctx[None], ada_w[i], ada_b[i])
        hx = modulate(rms_norm(x, norm_mix[i]), mx[:, 0], mx[:, 1])
        hc = modulate(rms_norm(ctx, norm_mix[i]), mc[:, 0], mc[:, 1])
        if i % N_MIXERS == 0:
            hp = (hy_w_in[j], hy_b_in[j], hy_conv_w[j], hy_conv_b[j], hy_f_w1[j], hy_f_b1[j],
                  hy_f_w2[j], hy_f_b2[j], hy_f_w3[j], hy_f_b3[j], hy_f_w4[j], hy_f_freq[j],
                  hy_skip[j], hy_w_out[j], hy_b_out[j])
            yx = hyena_mix(hx, *hp)
            yc = None if last else hyena_mix(hc, *hp)
        else:
            lam_init = 0.8 - 0.6 * math.exp(-0.3 * i)
            yx, yc = diff_attention(hx, hc, row, col, lam_init, not last, da_w_qkv[j],
                                    da_q_norm[j], da_k_norm[j], da_lam_q1[j], da_lam_k1[j],
                                    da_lam_q2[j], da_lam_k2[j], da_subln[j], da_w_out[j])
        x = x + mx[:, 2] * yx
        mp = (moe_router[i], moe_w_gate[i], moe_w_up[i], moe_w_down[i])
        if not last:
            ctx = ctx + mc[:, 2] * yc
            ctx = ctx + mc[:, 5] * ec_moe(modulate(rms_norm(ctx, norm_ffn[i]), mc[:, 3], mc[:, 4]), *mp)
        x = x + mx[:, 5] * ec_moe(modulate(rms_norm(x, norm_ffn[i]), mx[:, 3], mx[:, 4]), *mp)
    return x
```

```python
import numpy as np
from contextlib import ExitStack
import concourse.bass as bass
import concourse.mybir as mybir
from concourse.bass_utils import run_bass_kernel_spmd

F32 = mybir.dt.float32
BF16 = mybir.dt.bfloat16
I32 = mybir.dt.int32
ALU = mybir.AluOpType
AF = mybir.ActivationFunctionType
AX = mybir.AxisListType

EPOCH = 30000
N_DMA_SEMS = 24


class Buf:
    __slots__ = ("name", "w", "r")

    def __init__(self, name=""):
        self.name = name
        self.w = None
        self.r = {}


class Prog:
    ENGS = ("pe", "act", "dve", "pool", "sp")

    def __init__(self, nc, es):
        self.nc = nc
        self.es = es
        self.items = {e: [] for e in self.ENGS}
        self.cnt = {e: 0 for e in self.ENGS}
        self.esems = {e: [es.enter_context(nc.semaphore(f"s_{e}_0"))] for e in self.ENGS}
        self.seen = {e: {} for e in self.ENGS}
        self.dsems = {}
        self.dcount = {}
        self.drr = {}
        for q in ("sp", "pool", "act"):
            self.dsems[q] = [es.enter_context(nc.semaphore(f"d_{q}_{i}")) for i in range(N_DMA_SEMS)]
            self.dcount[q] = [0] * N_DMA_SEMS
            self.drr[q] = 0
        self.all_dma_tokens = []
        self.nbuf = 0

    def sb(self, name, shape, dt):
        t = self.es.enter_context(self.nc.sbuf_tensor("sb_" + name, list(shape), dt))
        return t

    def ps(self, name, shape, dt=F32):
        t = self.es.enter_context(self.nc.psum_tensor("ps_" + name, list(shape), dt))
        return t

    def buf(self, name=""):
        self.nbuf += 1
        return Buf(name or f"b{self.nbuf}")

    def _wait(self, eng, tok):
        if tok is None:
            return
        key, sem, val = tok
        if self.seen[eng].get(key, 0) >= val:
            return
        self.seen[eng][key] = val
        self.items[eng].append(("wait", sem, val))

    def _deps(self, eng, reads, writes):
        for b in reads:
            if b.w is not None:
                self._dep1(eng, b.w)
        for b in writes:
            if b.w is not None and b.w[0][0] != eng:
                self._dep1(eng, b.w)
            for t in b.r.values():
                if t[0][0] != eng:
                    self._dep1(eng, t)

    def _dep1(self, eng, tok):
        if eng == "pe" and tok[0][0] == "pe":
            return
        self._wait(eng, tok)

    def _mark(self, eng, tok, reads, writes):
        for b in reads:
            b.r[tok[0]] = tok
        for b in writes:
            b.w = tok
            b.r = {}

    def op(self, eng, fn, reads=(), writes=()):
        self._deps(eng, reads, writes)
        ep = self.cnt[eng] // EPOCH
        if ep >= len(self.esems[eng]):
            self.esems[eng].append(self.es.enter_context(self.nc.semaphore(f"s_{eng}_{ep}")))
        self.cnt[eng] += 1
        sem = self.esems[eng][ep]
        val = self.cnt[eng] - ep * EPOCH
        self.items[eng].append(("ins", fn, sem, 1))
        tok = ((eng, ep), sem, val)
        self._mark(eng, tok, reads, writes)
        return tok

    def group(self, eng, fns, reads=(), writes=()):
        self._deps(eng, reads, writes)
        tok = None
        for fn in fns:
            ep = self.cnt[eng] // EPOCH
            if ep >= len(self.esems[eng]):
                self.esems[eng].append(self.es.enter_context(self.nc.semaphore(f"s_{eng}_{ep}")))
            self.cnt[eng] += 1
            sem = self.esems[eng][ep]
            val = self.cnt[eng] - ep * EPOCH
            self.items[eng].append(("ins", fn, sem, 1))
            tok = ((eng, ep), sem, val)
        self._mark(eng, tok, reads, writes)
        return tok

    def dma(self, q, out, in_, reads=(), writes=(), **kw):
        self._deps(q, reads, writes)
        k = self.drr[q]
        self.drr[q] = (k + 1) % N_DMA_SEMS
        n = self.dcount[q][k]
        sem = self.dsems[q][k]
        if n > 0:
            self._wait(q, (("d", q, k), sem, 16 * n))
        self.dcount[q][k] = n + 1
        tok = (("d", q, k), sem, 16 * (n + 1))

        def fn(e, out=out, in_=in_, kw=kw):
            return e.dma_start(out=out, in_=in_, **kw)

        self.items[q].append(("ins", fn, sem, 16))
        self._mark(q, tok, reads, writes)
        self.all_dma_tokens.append(tok)
        return tok

    def finish(self):
        last = {}
        for tok in self.all_dma_tokens:
            last[tok[0]] = tok
        for tok in last.values():
            self._wait("sp", tok)

    def emit(self):
        nc = self.nc
        self.finish()
        items = self.items
        with nc.Block() as block:
            def run(e, lst):
                for it in lst:
                    if it[0] == "wait":
                        e.wait_ge(it[1], it[2])
                    else:
                        it[1](e).then_inc(it[2], it[3])

            @block.tensor
            def _(e):
                run(e, items["pe"])

            @block.scalar
            def _(e):
                run(e, items["act"])

            @block.vector
            def _(e):
                run(e, items["dve"])

            @block.gpsimd
            def _(e):
                run(e, items["pool"])

            @block.sync
            def _(e):
                run(e, items["sp"])


class TT:
    def __init__(self, p, name, shape, dt, ntiles):
        self.t = p.sb(name, shape, dt)
        self.b = [p.buf(f"{name}_{i}") for i in range(ntiles)]


class Pool_:
    def __init__(self, p, name, shape, dt, n, psum=False):
        mk = p.ps if psum else p.sb
        self.ts = [mk(f"{name}{i}", shape, dt) for i in range(n)]
        self.bs = [p.buf(f"{name}{i}") for i in range(n)]
        self.i = 0

    def get(self):
        k = self.i
        self.i = (k + 1) % len(self.ts)
        return self.ts[k], self.bs[k]


def col_tiles(T, w=512):
    out = []
    c = 0
    while c < T:
        out.append((c, min(w, T - c)))
        c += w
    return out


def ACT(p, out, in_, func, reads, writes, **kw):
    return p.op("act", lambda e: e.activation(out=out, in_=in_, func=func, **kw), reads, writes)


def TT_(p, eng, out, in0, in1, op, reads, writes):
    return p.op(eng, lambda e: e.tensor_tensor(out=out, in0=in0, in1=in1, op=op), reads, writes)


def TS(p, eng, out, in0, s1, s2, op0, op1, reads, writes, **kw):
    if op1 is None:
        return p.op(eng, lambda e: e.tensor_scalar(out=out, in0=in0, scalar1=s1, scalar2=None, op0=op0, **kw), reads, writes)
    return p.op(eng, lambda e: e.tensor_scalar(out=out, in0=in0, scalar1=s1, scalar2=s2, op0=op0, op1=op1, **kw), reads, writes)


def STT(p, out, in0, scalar, in1, op0, op1, reads, writes):
    return p.op("dve", lambda e: e.scalar_tensor_tensor(out=out, in0=in0, scalar=scalar, in1=in1, op0=op0, op1=op1), reads, writes)


def CP(p, eng, out, in_, reads, writes):
    if eng == "act":
        return p.op("act", lambda e: e.activation(out=out, in_=in_, func=AF.Copy), reads, writes)
    return p.op(eng, lambda e: e.tensor_copy(out=out, in_=in_), reads, writes)


def MM(p, out, pairs, reads, writes):
    n = len(pairs)
    fns = []
    for i, (l, r) in enumerate(pairs):
        fns.append(lambda e, l=l, r=r, i=i: e.matmul(out, l, r, start=(i == 0), stop=(i == n - 1)))
    return p.group("pe", fns, reads, writes)


def LOAD(p, q, shape, dt, name, src, es=None):
    t = p.sb(name, shape, dt)
    b = p.buf(name)
    p.dma(q, t[:], src, writes=[b])
    return t, b


class Ctx:
    pass


def make_common(p, nc, ones_src):
    C = Ctx()
    C.ones, C.bones = LOAD(p, "pool", [128, 128], BF16, "ones", ones_src)
    C.sq = Pool_(p, "sq", [128, 8, 512], BF16, 2)
    C.ps = Pool_(p, "psA", [128, 512], F32, 4, psum=True)
    C.rs = Pool_(p, "rs", [128, 512], F32, 2)
    C.tmp = Pool_(p, "tmp", [128, 512], F32, 3)
    return C


def emit_gmod(p, name, g, bg, scale, bscale, ncol=8):
    gm = p.sb(name, [128, ncol], F32)
    bgm = p.buf(name)
    STT(p, gm[:], scale, 1.0, g, ALU.add, ALU.mult, [bg, bscale], [bgm])
    return gm, bgm


def emit_rms_mod(p, C, X, tiles, mods_of_tile, OUT, OUTF=None, eps=1e-6, D=1024):
    for ti, (c0, w) in enumerate(tiles):
        gm, bgm, sh, bsh = mods_of_tile(ti)
        sq, bsq = C.sq.get()
        ACT(p, sq[:, :, 0:w], X.t[:, :, c0:c0 + w], AF.Square, [X.b[ti]], [bsq])
        ps, bps = C.ps.get()
        MM(p, ps[:, 0:w], [(C.ones[:], sq[:, kc, 0:w]) for kc in range(8)], [bsq, C.bones], [bps])
        rs, brs = C.rs.get()
        ACT(p, rs[:, 0:w], ps[:, 0:w], AF.Sqrt, [bps], [brs], scale=1.0 / D, bias=eps)
        p.op("dve", lambda e, rs=rs, w=w: e.reciprocal(out=rs[:, 0:w], in_=rs[:, 0:w]), [brs], [brs])
        for kc in range(8):
            tmp, btmp = C.tmp.get()
            STT(p, tmp[:, 0:w], X.t[:, kc, c0:c0 + w], gm[:, kc:kc + 1], rs[:, 0:w], ALU.mult, ALU.mult,
                [X.b[ti], brs, bgm], [btmp])
            ACT(p, OUT.t[:, kc, c0:c0 + w], tmp[:, 0:w], AF.Identity, [btmp, bsh], [OUT.b[ti]],
                bias=sh[:, kc:kc + 1], scale=1.0)
            if OUTF is not None:
                ACT(p, OUTF.t[:, kc, c0:c0 + w], tmp[:, 0:w], AF.Identity, [btmp, bsh], [OUTF.b[ti]],
                    bias=sh[:, kc:kc + 1], scale=1.0)


def fm(a):
    T = a.shape[0]
    return np.ascontiguousarray(a.T.reshape(8, 128, T).transpose(1, 0, 2))


def unfm(a):
    T = a.shape[2]
    return np.ascontiguousarray(a.transpose(1, 0, 2).reshape(1024, T).T)


def vec_fm(v, n=8):
    return np.ascontiguousarray(v.reshape(n, 128).T)


NCORE = 8
TX = 2048
TC = 32
ONES_F32 = np.ones((128, 128), np.float32)


def run(nc, in_maps):
    res = run_bass_kernel_spmd(nc, in_maps, core_ids=list(range(NCORE)))
    return res.results


def build_L0():
    nc = bass.Bass("TRN2", target_bir_lowering=False)
    condT = nc.dram_tensor("condT", [128, 8, 2], F32, kind="ExternalInput").ap()
    w = nc.dram_tensor("w", [2, 128, 8, 768], F32, kind="ExternalInput").ap()
    b = nc.dram_tensor("b", [2, 2, 768], F32, kind="ExternalInput").ap()
    out = nc.dram_tensor("out", [2, 2, 768], F32, kind="ExternalOutput").ap()
    with ExitStack() as es:
        p = Prog(nc, es)
        ct, bct = LOAD(p, "sp", [128, 8, 2], F32, "ct", condT)
        sc = p.sb("sc", [128, 8, 2], F32); bsc = p.buf()
        ACT(p, sc[:], ct[:], AF.Silu, [bct], [bsc])
        psp = Pool_(p, "ps", [128, 512], F32, 2, psum=True)
        for l in range(2):
            wt, bwt = LOAD(p, "sp", [128, 8, 768], F32, f"w{l}", w[l])
            bt, bbt = LOAD(p, "sp", [2, 768], F32, f"b{l}", b[l])
            ot = p.sb(f"o{l}", [2, 768], F32); bot = p.buf()
            for h in range(2):
                ps, bps = psp.get()
                MM(p, ps[0:2, 0:384], [(sc[:, kc, :], wt[:, kc, h * 384:(h + 1) * 384]) for kc in range(8)],
                   [bsc, bwt], [bps])
                TT_(p, "dve", ot[:, h * 384:(h + 1) * 384], ps[0:2, 0:384], bt[:, h * 384:(h + 1) * 384], ALU.add,
                    [bps, bbt], [bot])
            p.dma("sp", out[l], ot[:], reads=[bot])
        p.emit()
    return nc


def run_L0(inp):
    nc = build_L0()
    cond = np.stack([inp["c"][0], inp["c_ctx"]], axis=1)
    condT = np.ascontiguousarray(cond.reshape(8, 128, 2).transpose(1, 0, 2))
    maps = []
    for c in range(NCORE):
        sl = slice(c * 768, (c + 1) * 768)
        w = np.ascontiguousarray(inp["ada_w"][:, :, sl].reshape(2, 8, 128, 768).transpose(0, 2, 1, 3))
        b = np.ascontiguousarray(np.broadcast_to(inp["ada_b"][:, None, sl], (2, 2, 768)))
        maps.append({"condT": condT, "w": w, "b": b})
    res = run(nc, maps)
    mods = np.concatenate([r["out"] for r in res], axis=2)
    return mods


def mods_fm(mods, l):
    m = mods[l].reshape(2, 6, 8, 128)
    return np.ascontiguousarray(m.transpose(3, 0, 1, 2))


def tiles_of(T):
    return col_tiles(T, 512)


def build_L1(T):
    nc = bass.Bass("TRN2", target_bir_lowering=False)
    xT = nc.dram_tensor("xT", [128, 8, T], F32, kind="ExternalInput").ap()
    md = nc.dram_tensor("md", [128, 2, 6, 8], F32, kind="ExternalInput").ap()
    g = nc.dram_tensor("g", [128, 8], F32, kind="ExternalInput").ap()
    ones = nc.dram_tensor("ones", [128, 128], F32, kind="ExternalInput").ap()
    hT = nc.dram_tensor("hT", [128, 8, T], BF16, kind="ExternalOutput").ap()
    tiles = tiles_of(T)
    with ExitStack() as es:
        p = Prog(nc, es)
        C = make_common(p, nc, ones)
        mdt, bmd = LOAD(p, "sp", [128, 2, 6, 8], F32, "md", md)
        gt, bg = LOAD(p, "sp", [128, 8], F32, "g", g)
        X = TT(p, "X", [128, 8, T], F32, len(tiles))
        H = TT(p, "H", [128, 8, T], BF16, len(tiles))
        for ti, (c0, w) in enumerate(tiles):
            p.dma("sp", X.t[:, :, c0:c0 + w], xT[:, :, c0:c0 + w], writes=[X.b[ti]])
        gmx, bgmx = emit_gmod(p, "gmx", gt[:], bg, mdt[:, 0, 1, :], bmd)
        gmc, bgmc = emit_gmod(p, "gmc", gt[:], bg, mdt[:, 1, 1, :], bmd)

        def mods_of_tile(ti):
            c0, w = tiles[ti]
            if c0 >= TX:
                return gmc, bgmc, mdt[:, 1, 0, :], bmd
            return gmx, bgmx, mdt[:, 0, 0, :], bmd

        emit_rms_mod(p, C, X, tiles, mods_of_tile, H)
        for ti, (c0, w) in enumerate(tiles):
            p.dma("sp", hT[:, :, c0:c0 + w], H.t[:, :, c0:c0 + w], reads=[H.b[ti]])
        p.emit()
    return nc


def shard_tokens(x, ctx):
    out = []
    for c in range(NCORE):
        parts = [x[c * TX:(c + 1) * TX]]
        if ctx is not None:
            parts.append(ctx[c * TC:(c + 1) * TC])
        out.append(fm(np.concatenate(parts, axis=0)))
    return out


def unshard_tokens(slabs, has_ctx):
    xs, cs = [], []
    for s in slabs:
        a = unfm(s)
        xs.append(a[:TX])
        if has_ctx:
            cs.append(a[TX:])
    return np.concatenate(xs, 0), (np.concatenate(cs, 0) if has_ctx else None)


def run_L1(xsl, mods, l, g, T):
    nc = build_L1(T)
    md = mods_fm(mods, l)
    maps = [{"xT": xsl[c], "md": md, "g": vec_fm(g), "ones": ONES_F32} for c in range(NCORE)]
    res = run(nc, maps)
    return [r["hT"] for r in res]


PI = float(np.pi)


def build_LF(P):
    nc = bass.Bass("TRN2", target_bir_lowering=False)
    zT = nc.dram_tensor("zT", [33, P], F32, kind="ExternalInput").ap()
    w1 = nc.dram_tensor("w1", [33, 64], F32, kind="ExternalInput").ap()
    w23 = nc.dram_tensor("w23", [64, 2, 64], F32, kind="ExternalInput").ap()
    bf = nc.dram_tensor("bf", [64, 4], F32, kind="ExternalInput").ap()
    hid = nc.dram_tensor("hid", [64, P], BF16, kind="ExternalOutput").ap()
    tiles = col_tiles(P)
    with ExitStack() as es:
        p = Prog(nc, es)
        w1t, bw1 = LOAD(p, "sp", [33, 64], F32, "w1", w1)
        w23t, bw23 = LOAD(p, "sp", [64, 2, 64], F32, "w23", w23)
        bft, bbf = LOAD(p, "sp", [64, 4], F32, "bf", bf)
        bfr = p.sb("bfr", [64, 3], F32); bbfr = p.buf()
        TS(p, "dve", bfr[:], bft[:, 0:3], bft[:, 3:4], None, ALU.mult, None, [bbf], [bbfr])
        zp = Pool_(p, "z", [33, 512], F32, 2)
        psp = Pool_(p, "ps", [64, 512], F32, 3, psum=True)
        ap_ = Pool_(p, "arg", [64, 512], F32, 3)
        hp = Pool_(p, "h", [64, 512], F32, 3)
        op_ = Pool_(p, "o", [64, 512], BF16, 2)
        wp = Pool_(p, "wr", [64, 512], F32, 2)
        for (c0, w) in tiles:
            zt, bz = zp.get()
            p.dma("sp", zt[:, 0:w], zT[:, c0:c0 + w], writes=[bz])
            cur, bcur = zt, bz
            for l in range(3):
                ps, bps = psp.get()
                lhsT = w1t[:] if l == 0 else w23t[:, l - 1, :]
                bl = bw1 if l == 0 else bw23
                MM(p, ps[:, 0:w], [(lhsT, cur[:, 0:w])], [bl, bcur], [bps])
                a, ba = ap_.get()
                ACT(p, a[:, 0:w], ps[:, 0:w], AF.Identity, [bps, bbf, bbfr], [ba], scale=bft[:, 3:4], bias=bfr[:, l:l + 1])
                wt_, bwt_ = wp.get()
                TS(p, "dve", wt_[:, 0:w], a[:, 0:w], -PI, 2 * PI, ALU.is_lt, ALU.mult, [ba], [bwt_])
                TT_(p, "dve", a[:, 0:w], a[:, 0:w], wt_[:, 0:w], ALU.add, [ba, bwt_], [ba])
                wt_, bwt_ = wp.get()
                TS(p, "dve", wt_[:, 0:w], a[:, 0:w], PI, -2 * PI, ALU.is_gt, ALU.mult, [ba], [bwt_])
                TT_(p, "dve", a[:, 0:w], a[:, 0:w], wt_[:, 0:w], ALU.add, [ba, bwt_], [ba])
                if l < 2:
                    h, bh = hp.get()
                else:
                    h, bh = op_.get()
                ACT(p, h[:, 0:w], a[:, 0:w], AF.Sin, [ba], [bh])
                cur, bcur = h, bh
            p.dma("sp", hid[:, c0:c0 + w], cur[:, 0:w], reads=[bcur])
        p.emit()
    return nc


def hyena_pos_tables(L):
    N = 32768
    t = np.linspace(0.0, 1.0, L, dtype=np.float32)[:, None]
    w = (2.0 * np.float32(np.pi) * np.arange(L, dtype=np.float32)[:, None] / np.float32(L)).astype(np.float32)
    bands = np.linspace(1e-4, 15, 16, dtype=np.float32)[None]
    z = np.concatenate([t, np.cos(w * bands), -np.sin(w * bands)], axis=-1).astype(np.float32)
    maxd = np.log(1e-2) / 0.3
    mind = np.log(1e-2) / 1.5
    deltas = np.linspace(mind, maxd, 1024, dtype=np.float32)
    decay = np.exp(-t * np.abs(deltas)).astype(np.float32)
    zext = np.zeros((N, 33), np.float32)
    dext = np.zeros((N, 1024), np.float32)
    zext[:L] = z
    dext[:L] = decay
    j = np.arange(1, L)
    zext[N - j] = z[j]
    dext[N - j] = decay[j]
    return zext, dext


def perm_pos(a):
    F_ = a.shape[1]
    return np.ascontiguousarray(a.reshape(256, 128, F_).transpose(2, 1, 0))


def run_LF(inp, zext):
    P = 32768 // NCORE
    nc = build_LF(P)
    zp = perm_pos(zext).reshape(33, 32768)
    w23 = np.ascontiguousarray(np.stack([inp["hy_f_w2"][0], inp["hy_f_w3"][0]], axis=1))
    bf = np.ascontiguousarray(np.stack([inp["hy_f_b1"][0], inp["hy_f_b2"][0], inp["hy_f_b3"][0], inp["hy_f_freq"][0]], axis=1))
    maps = [{"zT": np.ascontiguousarray(zp[:, c * P:(c + 1) * P]), "w1": inp["hy_f_w1"][0], "w23": w23, "bf": bf}
            for c in range(NCORE)]
    res = run(nc, maps)
    hid = np.concatenate([np.asarray(r["hid"]) for r in res], axis=1)
    return hid.reshape(64, 128, 256)


def build_HA(segs):
    Th = sum(n + 2 for _, n in segs)
    T = sum(n for _, n in segs)
    nc = bass.Bass("TRN2", target_bir_lowering=False)
    hT = nc.dram_tensor("hT", [128, 8, Th], BF16, kind="ExternalInput").ap()
    valid = nc.dram_tensor("valid", [1, Th], BF16, kind="ExternalInput").ap()
    win = nc.dram_tensor("win", [24, 128, 8, 128], F32, kind="ExternalInput").ap()
    cw = nc.dram_tensor("cw", [24, 128, 3, 128], F32, kind="ExternalInput").ap()
    brow = nc.dram_tensor("brow", [1, 4, 3072], F32, kind="ExternalInput").ap()
    cb = nc.dram_tensor("cb", [128, 24], F32, kind="ExternalInput").ap()
    uT = nc.dram_tensor("uT", [128, 24, T], F32, kind="ExternalOutput").ap()
    tiles = []
    o0 = 0
    for hb, n in segs:
        for (c0, w) in col_tiles(n):
            tiles.append((hb + c0, o0 + c0, w))
        o0 += n
    with ExitStack() as es:
        p = Prog(nc, es)
        H, bH = LOAD(p, "sp", [128, 8, Th], BF16, "H", hT)
        V, bV = LOAD(p, "sp", [1, Th], BF16, "V", valid)
        BR, bBR = LOAD(p, "sp", [1, 4, 3072], F32, "BR", brow)
        CB, bCB = LOAD(p, "sp", [128, 24], F32, "CB", cb)
        bk = p.sb("bk", [1, 3, 3072], BF16); bbk = p.buf()
        for k in range(3):
            TT_(p, "dve", bk[:, k, :], BR[:, 0, :], BR[:, 1 + k, :], ALU.mult, [bBR], [bbk])
        wp = Pool_(p, "w", [128, 8, 128], F32, 2)
        cp = Pool_(p, "c", [128, 3, 128], F32, 2)
        wkp = Pool_(p, "wk", [128, 3, 8, 128], BF16, 2)
        psp = Pool_(p, "ps", [128, 512], F32, 4, psum=True)
        op_ = Pool_(p, "o", [128, T], F32, 3)
        for m in range(24):
            wt, bw = wp.get()
            p.dma("sp", wt[:], win[m], writes=[bw])
            ct, bc = cp.get()
            p.dma("sp", ct[:], cw[m], writes=[bc])
            wk, bwk = wkp.get()
            for k in range(3):
                TT_(p, "dve" if k < 2 else "pool", wk[:, k, :, :], wt[:],
                    ct[:, k, :].unsqueeze(1).to_broadcast([128, 8, 128]), ALU.mult, [bw, bc], [bwk])
            ot, bo = op_.get()
            for (hb, ob, w) in tiles:
                ps, bps = psp.get()
                pairs = []
                for k in range(3):
                    for kc in range(8):
                        pairs.append((wk[:, k, kc, :], H[:, kc, hb + k:hb + k + w]))
                    pairs.append((bk[0:1, k, m * 128:(m + 1) * 128], V[0:1, hb + k:hb + k + w]))
                MM(p, ps[:, 0:w], pairs, [bwk, bH, bV, bbk], [bps])
                ACT(p, ot[:, ob:ob + w], ps[:, 0:w], AF.Identity, [bps, bCB], [bo], bias=CB[:, m:m + 1], scale=1.0)
            p.dma("sp", uT[:, m, :], ot[:], reads=[bo])
        p.emit()
    return nc


def halo_slabs(hx, hc):
    import ml_dtypes
    out = []
    for c in range(NCORE):
        parts, val = [], []
        for a, n in ((hx, TX), (hc, TC)):
            if a is None:
                continue
            L = a.shape[0]
            seg = np.zeros((n + 2, 1024), a.dtype)
            v = np.zeros((n + 2,), np.float32)
            lo, hi = c * n - 1, (c + 1) * n + 1
            slo, shi = max(lo, 0), min(hi, L)
            seg[slo - lo:shi - lo] = a[slo:shi]
            v[slo - lo:shi - lo] = 1.0
            parts.append(seg)
            val.append(v)
        out.append((fm(np.concatenate(parts, 0)), np.concatenate(val)[None].astype(ml_dtypes.bfloat16)))
    return out


def run_HA(inp, hx, hc):
    segs = [(0, TX)] + ([(TX + 2, TC)] if hc is not None else [])
    nc = build_HA(segs)
    W = inp["hy_w_in"][0]
    win = np.ascontiguousarray(W.reshape(8, 128, 24, 128).transpose(2, 1, 0, 3))
    cwv = inp["hy_conv_w"][0]
    cw = np.ascontiguousarray(np.broadcast_to(cwv.reshape(3, 24, 128).transpose(1, 0, 2)[:, None], (24, 128, 3, 128)))
    brow = np.ascontiguousarray(np.concatenate([inp["hy_b_in"][0][None], cwv], 0)[None])
    cb = vec_fm(inp["hy_conv_b"][0], 24)
    sl = halo_slabs(hx, hc)
    maps = [{"hT": sl[c][0], "valid": sl[c][1], "win": win, "cw": cw, "brow": brow, "cb": cb} for c in range(NCORE)]
    res = run(nc, maps)
    us, ucs = [], []
    for r in res:
        a = np.asarray(r["uT"]).transpose(1, 0, 2).reshape(3072, -1).T
        us.append(a[:TX])
        ucs.append(a[TX:])
    return np.concatenate(us, 0), (np.concatenate(ucs, 0) if hc is not None else None)


NG, GC = 8, 16


def hc_tables():
    import ml_dtypes
    bf = ml_dtypes.bfloat16
    N = 32768
    n1 = np.arange(128)[:, None].astype(np.float64)
    k1 = np.arange(256)[None].astype(np.float64)
    F1 = np.zeros((128, 2, 512), np.float64)
    for h in range(2):
        a = 2 * np.pi * (n1 + 128 * h) * k1 / 256
        F1[:, h, :256] = np.cos(a)
        F1[:, h, 256:] = -np.sin(a)
    phi = 2 * np.pi * np.arange(128)[:, None] * np.arange(256)[None] / N
    TW = np.stack([np.cos(phi), np.sin(phi)], 1)
    th = 2 * np.pi * np.arange(128)[:, None] * np.arange(128)[None] / 128
    F2 = np.stack([np.cos(th), np.sin(th), -np.sin(th)], 1)
    M3 = np.zeros((128, 2, 256), np.float64)
    M3[:, 0, :128] = np.cos(th); M3[:, 0, 128:] = np.sin(th)
    M3[:, 1, :128] = -np.sin(th); M3[:, 1, 128:] = np.cos(th)
    TWI = np.zeros((128, 2, 2, 128), np.float64)
    S4 = np.zeros((128, 2, 2, 128), np.float64)
    kp = np.arange(128)[:, None]
    for half in range(2):
        ph = 2 * np.pi * np.arange(128)[None] * (half * 128 + kp) / N
        TWI[:, half, 0] = np.cos(ph); TWI[:, half, 1] = np.sin(ph)
        ps_ = 2 * np.pi * np.arange(128)[None] * (half * 128 + kp) / 256
        S4[:, half, 0] = np.cos(ps_) / N; S4[:, half, 1] = -np.sin(ps_) / N
    return {"F1": F1.astype(bf), "TW": TW.astype(np.float32), "F2": F2.astype(bf), "M3": M3.astype(bf),
            "TWI": TWI.astype(np.float32), "S4": S4.astype(bf), "ones32": np.ones((128, 128), np.float32)}


HC_STOP = [0]


class _Stop(Exception):
    pass


def _chk(n):
    if HC_STOP[0] == n:
        raise _Stop()


def build_HC():
    nc = bass.Bass("TRN2", target_bir_lowering=False)
    dU = nc.dram_tensor("U", [NG, 128, 3, GC, 128], F32, kind="ExternalInput").ap()
    dhid = nc.dram_tensor("hid", [64, 128, 256], BF16, kind="ExternalInput").ap()
    dw4 = nc.dram_tensor("w4", [NG, 64, 2, 2 * GC], F32, kind="ExternalInput").ap()
    ddec = nc.dram_tensor("dec", [NG, 128, 2, 128, GC], F32, kind="ExternalInput").ap()
    dskip = nc.dram_tensor("skip", [128, NG, 2, GC], F32, kind="ExternalInput").ap()
    dF1 = nc.dram_tensor("F1", [128, 2, 512], BF16, kind="ExternalInput").ap()
    dTW = nc.dram_tensor("TW", [128, 2, 256], F32, kind="ExternalInput").ap()
    dF2 = nc.dram_tensor("F2", [128, 3, 128], BF16, kind="ExternalInput").ap()
    dM3 = nc.dram_tensor("M3", [128, 2, 256], BF16, kind="ExternalInput").ap()
    dTWI = nc.dram_tensor("TWI", [128, 2, 2, 128], F32, kind="ExternalInput").ap()
    dS4 = nc.dram_tensor("S4", [128, 2, 2, 128], BF16, kind="ExternalInput").ap()
    dones = nc.dram_tensor("ones32", [128, 128], F32, kind="ExternalInput").ap()
    dout = nc.dram_tensor("z2", [NG, 128, GC, 128], BF16, kind="ExternalOutput").ap()
    with ExitStack() as es:
        p = Prog(nc, es)
        F1, bF1 = LOAD(p, "sp", [128, 2, 512], BF16, "F1", dF1)
        TW, bTW = LOAD(p, "sp", [128, 2, 256], F32, "TW", dTW)
        F2, bF2 = LOAD(p, "sp", [128, 3, 128], BF16, "F2", dF2)
        M3, bM3 = LOAD(p, "sp", [128, 2, 256], BF16, "M3", dM3)
        TWI, bTWI = LOAD(p, "sp", [128, 2, 2, 128], F32, "TWI", dTWI)
        S4, bS4 = LOAD(p, "sp", [128, 2, 2, 128], BF16, "S4", dS4)
        ON, bON = LOAD(p, "sp", [128, 128], F32, "ON", dones)
        SK, bSK = LOAD(p, "sp", [128, NG, 2, GC], F32, "SK", dskip)
        cst = [bF1, bTW, bF2, bM3, bTWI, bS4]
        Up = Pool_(p, "U", [128, 3, GC, 128], F32, 1)
        Dp = Pool_(p, "D", [128, 2, 128, GC], F32, 1)
        W4p = Pool_(p, "W4", [64, 2, 2 * GC], BF16, 2)
        Hp = Pool_(p, "Hd", [64, 16, 256], BF16, 2)
        Kt = p.sb("Kt", [128, 2, GC, 2, 128], BF16); bKt = p.buf()
        Kr = p.sb("Kr", [128, 2, 128, 2, GC], BF16); bKr = p.buf()
        A = p.sb("A", [128, GC, 2, 256], BF16); bA = [p.buf() for _ in range(GC // 2)]
        Kf = p.sb("Kf", [128, GC, 2, 256], BF16); bKf = [p.buf() for _ in range(GC // 2)]
        G = p.sb("G", [128, GC, 2, 256], BF16); bG = [p.buf() for _ in range(GC // 2)]
        Bp = p.sb("Bp", [128, 2, 2, GC, 128], BF16); bBp = [p.buf() for _ in range(GC // 4)]
        Zb = p.sb("Zb", [128, GC, 128], BF16); bZb = [p.buf() for _ in range(GC // 4)]
        Z1 = p.sb("Z1", [128, GC, 128], F32); bZ1 = [p.buf() for _ in range(GC // 4)]
        Op = Pool_(p, "O", [128, GC, 128], BF16, 2)
        red = p.sb("red", [128, 2 * GC], F32); bred = p.buf()
        sN = p.sb("sN", [128, 2 * GC], F32); bsN = p.buf()
        psSp = Pool_(p, "psS", [128, 512], F32, 2, psum=True)
        ps1 = Pool_(p, "ps1", [128, 512], F32, 2, psum=True)
        psY = Pool_(p, "psY", [128, 512], F32, 3, psum=True)
        ps4 = Pool_(p, "ps4", [128, 512], F32, 1, psum=True)
        T1p = Pool_(p, "T1", [128, 2, 256], F32, 2)
        T2p = Pool_(p, "T2", [128, 2, 256], F32, 2)
        E1p = Pool_(p, "E1", [128, 4, 128], F32, 2)
        E2p = Pool_(p, "E2", [128, 4, 128], F32, 2)

        def fwd_twiddle(ps, bps, c, bdst):
            t1, b1 = T1p.get()
            t2, b2 = T2p.get()
            pv = ps[:, :].rearrange("p (r k) -> p r k", r=2)
            TT_(p, "dve", t1[:], pv, TW[:, 0, :].unsqueeze(1).to_broadcast([128, 2, 256]), ALU.mult, [bps, bTW], [b1])
            TT_(p, "dve", t2[:], pv, TW[:, 1, :].unsqueeze(1).to_broadcast([128, 2, 256]), ALU.mult, [bps, bTW], [b2])
            TT_(p, "pool", A[:, c, 0, :], t1[:, 0, :], t2[:, 1, :], ALU.add, [b1, b2], [bdst])
            TT_(p, "pool", A[:, c, 1, :], t1[:, 1, :], t2[:, 0, :], ALU.subtract, [b1, b2], [bdst])

        def stage2(pr):
            c0 = 2 * pr
            yr, byr = psY.get()
            yi, byi = psY.get()
            ar = A[:, c0:c0 + 2, 0, :]
            ai = A[:, c0:c0 + 2, 1, :]
            MM(p, yr[:, :], [(F2[:, 0, :], ar), (F2[:, 1, :], ai)], [bF2, bA[pr]], [byr])
            MM(p, yi[:, :], [(F2[:, 0, :], ai), (F2[:, 2, :], ar)], [bF2, bA[pr]], [byi])
            return yr, byr, yi, byi

        for g in range(NG):
          try:
            Ug, bU = Up.get()
            p.dma("sp", Ug[:], dU[g], writes=[bU])
            Dg, bD = Dp.get()
            p.dma("sp", Dg[:], ddec[g], writes=[bD])
            W4, bW4 = W4p.get()
            p.dma("pool", W4[:], dw4[g], writes=[bW4])
            _chk(10)
            for nb in range(8):
                Hd, bHd = Hp.get()
                p.dma("sp", Hd[:], dhid[:, nb * 16:(nb + 1) * 16, :], writes=[bHd])
                for jb in range(2):
                    bank, bbank = psSp.get()
                    fns = []
                    for jj in range(8):
                        j = jb * 8 + jj
                        for h in range(2):
                            k = jj * 2 + h
                            fns.append(lambda e, bank=bank, k=k, j=j, h=h, Hd=Hd, W4=W4: e.matmul(
                                bank[:, k * 32:(k + 1) * 32], Hd[:, j, h * 128:(h + 1) * 128], W4[:, h, :], start=True, stop=True))
                    p.group("pe", fns, [bHd, bW4], [bbank])
                    for jj in range(8):
                        j = jb * 8 + jj
                        n2 = nb * 16 + j
                        for h in range(2):
                            k = jj * 2 + h
                            TT_(p, "dve", Kr[:, h, n2, :, :], bank[:, k * 32:(k + 1) * 32].rearrange("p (o c) -> p o c", o=2),
                                Dg[:, h, n2, :].unsqueeze(1).to_broadcast([128, 2, GC]), ALU.mult, [bbank, bD], [bKr])
            for o_ in range(2):
                for c_ in range(GC):
                    CP(p, "act", Kt[:, o_, c_, :, :], Kr[:, :, :, o_, c_], [bKr], [bKt])
            _chk(1)
            p.op("dve", lambda e: e.tensor_reduce(out=red[:], in_=Kt[:].rearrange("p o c h n -> p (o c) (h n)"),
                                                   axis=AX.X, op=ALU.add, apply_absolute_value=True), [bKt], [bred])
            bank, bbank = psSp.get()
            sl = bank[:, 0:32]
            MM(p, sl, [(ON[:], red[:])], [bON, bred], [bbank])
            TS(p, "dve", sN[:], sl, 1e-6, None, ALU.add, None, [bbank], [bsN])
            p.op("dve", lambda e: e.reciprocal(out=sN[:], in_=sN[:]), [bsN], [bsN])
            _chk(2)
            Og, bO = Op.get()
            for o in range(2):
                for c in range(GC):
                    ps, bps = ps1.get()
                    MM(p, ps[:, :], [(Kt[:, o, c, 0, :], F1[:, 0, :]), (Kt[:, o, c, 1, :], F1[:, 1, :])], [bKt, bF1], [bps])
                    fwd_twiddle(ps, bps, c, bA[c // 2])
                for pr in range(GC // 2):
                    yr, byr, yi, byi = stage2(pr)
                    CP(p, "act", Kf[:, 2 * pr:2 * pr + 2, 0, :], yr[:, :].rearrange("p (c k) -> p c k", c=2), [byr], [bKf[pr]])
                    CP(p, "act", Kf[:, 2 * pr:2 * pr + 2, 1, :], yi[:, :].rearrange("p (c k) -> p c k", c=2), [byi], [bKf[pr]])
                _chk(3)
                if o == 0:
                    for q in range(GC // 4):
                        CP(p, "act", Zb[:, 4 * q:4 * q + 4, :], Ug[:, 0, 4 * q:4 * q + 4, :], [bU], [bZb[q]])
                for c in range(GC):
                    ps, bps = ps1.get()
                    MM(p, ps[:, :], [(Zb[:, c, :], F1[:, 0, :])], [bZb[c // 4], bF1], [bps])
                    fwd_twiddle(ps, bps, c, bA[c // 2])
                for pr in range(GC // 2):
                    c0 = 2 * pr
                    yr, byr, yi, byi = stage2(pr)
                    yrv = yr[:, :].rearrange("p (c k) -> p c k", c=2)
                    yiv = yi[:, :].rearrange("p (c k) -> p c k", c=2)
                    kr = Kf[:, c0:c0 + 2, 0, :]
                    ki = Kf[:, c0:c0 + 2, 1, :]
                    t1, b1 = T1p.get()
                    t2, b2 = T2p.get()
                    TT_(p, "dve", t1[:], yrv, kr, ALU.mult, [byr, bKf[pr]], [b1])
                    TT_(p, "dve", t2[:], yiv, ki, ALU.mult, [byi, bKf[pr]], [b2])
                    TT_(p, "pool", G[:, c0:c0 + 2, 0, :], t1[:], t2[:], ALU.subtract, [b1, b2], [bG[pr]])
                    t1, b1 = T1p.get()
                    t2, b2 = T2p.get()
                    TT_(p, "dve", t1[:], yrv, ki, ALU.mult, [byr, bKf[pr]], [b1])
                    TT_(p, "dve", t2[:], yiv, kr, ALU.mult, [byi, bKf[pr]], [b2])
                    TT_(p, "pool", G[:, c0:c0 + 2, 1, :], t1[:], t2[:], ALU.add, [b1, b2], [bG[pr]])
                _chk(4)
                for c in range(GC):
                    for half in range(2):
                        ps, bps = ps1.get()
                        MM(p, ps[:, 0:256], [(G[:, c, 0, half * 128:(half + 1) * 128], M3[:, 0, :]),
                                             (G[:, c, 1, half * 128:(half + 1) * 128], M3[:, 1, :])], [bG[c // 2], bM3], [bps])
                        t1, b1 = T1p.get()
                        t2, b2 = T2p.get()
                        pv = ps[:, 0:256].rearrange("p (r k) -> p r k", r=2)
                        TT_(p, "dve", t1[:, :, 0:128], pv, TWI[:, half, 0, :].unsqueeze(1).to_broadcast([128, 2, 128]), ALU.mult,
                            [bps, bTWI], [b1])
                        TT_(p, "dve", t2[:, :, 0:128], pv, TWI[:, half, 1, :].unsqueeze(1).to_broadcast([128, 2, 128]), ALU.mult,
                            [bps, bTWI], [b2])
                        TT_(p, "pool", Bp[:, half, 0, c, :], t1[:, 0, 0:128], t2[:, 1, 0:128], ALU.subtract, [b1, b2], [bBp[c // 4]])
                        TT_(p, "pool", Bp[:, half, 1, c, :], t2[:, 0, 0:128], t1[:, 1, 0:128], ALU.add, [b1, b2], [bBp[c // 4]])
                _chk(5)
                for q in range(GC // 4):
                    c0 = 4 * q
                    ps, bps = ps4.get()
                    MM(p, ps[:, :], [(S4[:, half, ri, :], Bp[:, half, ri, c0:c0 + 4, :]) for half in range(2) for ri in range(2)],
                       [bS4, bBp[q]], [bps])
                    e1, be1 = E1p.get()
                    e2, be2 = E2p.get()
                    pv = ps[:, :].rearrange("p (c n) -> p c n", c=4)
                    TT_(p, "dve", e1[:], pv, sN[:, o * GC + c0:o * GC + c0 + 4].unsqueeze(2).to_broadcast([128, 4, 128]), ALU.mult,
                        [bps, bsN], [be1])
                    if o == 0:
                        zc, bzc = Ug[:, 0, c0:c0 + 4, :], bU
                    else:
                        zc, bzc = Z1[:, c0:c0 + 4, :], bZ1[q]
                    TT_(p, "pool", e2[:], zc, SK[:, g, o, c0:c0 + 4].unsqueeze(2).to_broadcast([128, 4, 128]), ALU.mult,
                        [bzc, bSK], [be2])
                    TT_(p, "pool", e2[:], e2[:], e1[:], ALU.add, [be1, be2], [be2])
                    if o == 0:
                        TT_(p, "dve", Z1[:, c0:c0 + 4, :], e2[:], Ug[:, 1, c0:c0 + 4, :], ALU.mult, [be2, bU], [bZ1[q]])
                        CP(p, "act", Zb[:, c0:c0 + 4, :], Z1[:, c0:c0 + 4, :], [bZ1[q]], [bZb[q]])
                    else:
                        TT_(p, "dve", Og[:, c0:c0 + 4, :], e2[:], Ug[:, 2, c0:c0 + 4, :], ALU.mult, [be2, bU], [bO])
            p.dma("sp", dout[g], Og[:], reads=[bO])
          except _Stop:
            break
        p.emit()
    return nc


def run_HC(inp, u, hid, dext, tabs, nc=None):
    if nc is None:
        nc = build_HC()
    L = u.shape[0]
    up = np.zeros((16384, 3072), np.float32)
    up[:L] = u
    u4 = up.reshape(128, 128, 3, 1024)
    d4 = dext.reshape(2, 128, 128, 1024).transpose(1, 0, 2, 3)
    w4 = inp["hy_f_w4"][0].reshape(64, 2, 2, 1024)
    sk = inp["hy_skip"][0]
    maps = []
    for c in range(NCORE):
        ch = slice(c * 128, (c + 1) * 128)
        U = np.ascontiguousarray(u4[:, :, :, ch].reshape(128, 128, 3, NG, GC).transpose(3, 0, 2, 4, 1))
        D = np.ascontiguousarray(d4[:, :, :, ch].reshape(128, 2, 128, NG, GC).transpose(3, 0, 1, 2, 4))
        W = np.ascontiguousarray(w4[:, :, :, ch].reshape(64, 2, 2, NG, GC).transpose(3, 0, 2, 1, 4).reshape(NG, 64, 2, 2 * GC))
        S = np.ascontiguousarray(np.broadcast_to(sk[:, ch].reshape(2, NG, GC).transpose(1, 0, 2)[None], (128, NG, 2, GC)))
        m = {"U": U, "hid": hid, "w4": W, "dec": D, "skip": S}
        m.update(tabs)
        maps.append(m)
    res = run(nc, maps)
    zs = []
    for r in res:
        a = np.asarray(r["z2"])
        zs.append(a.transpose(1, 3, 0, 2).reshape(16384, 128))
    return np.concatenate(zs, axis=1)[:L]


def MMx(p, out, lhsT, rhs, start, stop, reads, writes):
    return p.group("pe", [lambda e: e.matmul(out, lhsT, rhs, start=start, stop=stop)], reads, writes)


def rms_mod_tile(p, C, xs, bx, w, gm, bgm, sh, bsh, obf, bobf, of=None, bof=None, eps=1e-6, D=1024):
    sq, bsq = C.sq.get()
    for kc in range(8):
        ACT(p, sq[:, kc, 0:w], xs(kc), AF.Square, [bx], [bsq])
    ps, bps = C.ps.get()
    MM(p, ps[:, 0:w], [(C.ones[:], sq[:, kc, 0:w]) for kc in range(8)], [bsq, C.bones], [bps])
    rs, brs = C.rs.get()
    ACT(p, rs[:, 0:w], ps[:, 0:w], AF.Sqrt, [bps], [brs], scale=1.0 / D, bias=eps)
    p.op("dve", lambda e: e.reciprocal(out=rs[:, 0:w], in_=rs[:, 0:w]), [brs], [brs])
    for kc in range(8):
        tmp, btmp = C.tmp.get()
        STT(p, tmp[:, 0:w], xs(kc), gm[:, kc:kc + 1], rs[:, 0:w], ALU.mult, ALU.mult, [bx, brs, bgm], [btmp])
        ACT(p, obf(kc), tmp[:, 0:w], AF.Identity, [btmp, bsh], [bobf], bias=sh[:, kc:kc + 1], scale=1.0)
        if of is not None:
            ACT(p, of(kc), tmp[:, 0:w], AF.Identity, [btmp, bsh], [bof], bias=sh[:, kc:kc + 1], scale=1.0)


def build_L3(T, has_bias):
    nc = bass.Bass("TRN2", target_bir_lowering=False)
    dz = nc.dram_tensor("zT", [128, 8, T], BF16, kind="ExternalInput").ap()
    dx = nc.dram_tensor("xT", [128, 8, T], F32, kind="ExternalInput").ap()
    dw = nc.dram_tensor("wout", [1024, 1024], F32, kind="ExternalInput").ap()
    db = nc.dram_tensor("bout", [128, 8], F32, kind="ExternalInput").ap()
    dmd = nc.dram_tensor("md", [128, 2, 6, 8], F32, kind="ExternalInput").ap()
    dg = nc.dram_tensor("g", [128, 8], F32, kind="ExternalInput").ap()
    dwr = nc.dram_tensor("wr", [128, 8, 16], F32, kind="ExternalInput").ap()
    dones = nc.dram_tensor("ones", [128, 128], F32, kind="ExternalInput").ap()
    ox = nc.dram_tensor("x1T", [128, 8, T], F32, kind="ExternalOutput").ap()
    oh = nc.dram_tensor("h2T", [128, 8, T], BF16, kind="ExternalOutput").ap()
    oa = nc.dram_tensor("aff", [T, 16], F32, kind="ExternalOutput").ap()
    tiles = tiles_of(T)
    with ExitStack() as es:
        p = Prog(nc, es)
        C = make_common(p, nc, dones)
        mdt, bmd = LOAD(p, "sp", [128, 2, 6, 8], F32, "md", dmd)
        gt, bg = LOAD(p, "sp", [128, 8], F32, "g", dg)
        bt, bb = LOAD(p, "sp", [128, 8], F32, "bo", db)
        wr, bwr = LOAD(p, "sp", [128, 8, 16], F32, "wr", dwr)
        W = p.sb("W", [128, 8, 1024], BF16); bW = p.buf()
        for kc in range(8):
            p.dma("pool", W[:, kc, :], dw[kc * 128:(kc + 1) * 128, :], writes=[bW])
        X = TT(p, "X", [128, 8, T], F32, len(tiles))
        Z = TT(p, "Z", [128, 8, T], BF16, len(tiles))
        for ti, (c0, w) in enumerate(tiles):
            p.dma("sp", Z.t[:, :, c0:c0 + w], dz[:, :, c0:c0 + w], writes=[Z.b[ti]])
            p.dma("sp", X.t[:, :, c0:c0 + w], dx[:, :, c0:c0 + w], writes=[X.b[ti]])
        gmx, bgmx = emit_gmod(p, "gmx", gt[:], bg, mdt[:, 0, 4, :], bmd)
        gmc, bgmc = emit_gmod(p, "gmc", gt[:], bg, mdt[:, 1, 4, :], bmd)
        Hb = Pool_(p, "Hb", [128, 8, 512], BF16, 2)
        Hf = Pool_(p, "Hf", [128, 8, 512], F32, 2)
        psL = Pool_(p, "psL", [128, 512], F32, 2, psum=True)
        sm = Pool_(p, "sm", [128, 40], F32, 3)
        for ti, (c0, w) in enumerate(tiles):
            cond = 1 if c0 >= TX else 0
            for m in range(8):
                ps, bps = C.ps.get()
                MM(p, ps[:, 0:w], [(W[:, kc, m * 128:(m + 1) * 128], Z.t[:, kc, c0:c0 + w]) for kc in range(8)], [bW, Z.b[ti]], [bps])
                if has_bias:
                    tmp, btmp = C.tmp.get()
                    ACT(p, tmp[:, 0:w], ps[:, 0:w], AF.Identity, [bps, bb], [btmp], bias=bt[:, m:m + 1], scale=1.0)
                    src, bsrc = tmp, btmp
                else:
                    src, bsrc = ps, bps
                STT(p, X.t[:, m, c0:c0 + w], src[:, 0:w], mdt[:, cond, 2, m:m + 1], X.t[:, m, c0:c0 + w], ALU.mult, ALU.add,
                    [bsrc, bmd, X.b[ti]], [X.b[ti]])
            p.dma("sp", ox[:, :, c0:c0 + w], X.t[:, :, c0:c0 + w], reads=[X.b[ti]])
            hb, bhb = Hb.get()
            hf, bhf = Hf.get()
            gm, bgm = (gmc, bgmc) if cond else (gmx, bgmx)
            rms_mod_tile(p, C, lambda kc: X.t[:, kc, c0:c0 + w], X.b[ti], w, gm, bgm, mdt[:, cond, 3, :], bmd,
                         lambda kc: hb[:, kc, 0:w], bhb, lambda kc: hf[:, kc, 0:w], bhf)
            p.dma("sp", oh[:, :, c0:c0 + w], hb[:, :, 0:w], reads=[bhb])
            for j0 in range(0, w, 128):
                tw = min(128, w - j0)
                pl, bpl = psL.get()
                MM(p, pl[0:tw, 0:16], [(hf[:, kc, j0:j0 + tw], wr[:, kc, :]) for kc in range(8)], [bhf, bwr], [bpl])
                s_, bs_ = sm.get()
                p.op("dve", lambda e, s_=s_, pl=pl, tw=tw: e.tensor_reduce(out=s_[0:tw, 32:33], in_=pl[0:tw, 0:16], axis=AX.X, op=ALU.max),
                     [bpl], [bs_])
                TS(p, "dve", s_[0:tw, 33:34], s_[0:tw, 32:33], -1.0, None, ALU.mult, None, [bs_], [bs_])
                ACT(p, s_[0:tw, 0:16], pl[0:tw, 0:16], AF.Exp, [bpl, bs_], [bs_], bias=s_[0:tw, 33:34], scale=1.0,
                    accum_out=s_[0:tw, 34:35])
                p.op("dve", lambda e, s_=s_, tw=tw: e.reciprocal(out=s_[0:tw, 35:36], in_=s_[0:tw, 34:35]), [bs_], [bs_])
                TS(p, "dve", s_[0:tw, 16:32], s_[0:tw, 0:16], s_[0:tw, 35:36], None, ALU.mult, None, [bs_], [bs_])
                p.dma("sp", oa[c0 + j0:c0 + j0 + tw, :], s_[0:tw, 16:32], reads=[bs_])
        p.emit()
    return nc


def run_L3(zsl, xsl, wout, bout, mods, l, g, wr, T, has_bias):
    nc = build_L3(T, has_bias)
    md = mods_fm(mods, l)
    wrl = np.ascontiguousarray(wr.reshape(8, 128, 16).transpose(1, 0, 2))
    bo = vec_fm(bout) if bout is not None else np.zeros((128, 8), np.float32)
    maps = [{"zT": zsl[c], "xT": xsl[c], "wout": wout, "bout": bo, "md": md, "g": vec_fm(g), "wr": wrl, "ones": ONES_F32}
            for c in range(NCORE)]
    res = run(nc, maps)
    return [r["x1T"] for r in res], [r["h2T"] for r in res], [np.asarray(r["aff"]) for r in res]


def emit_bisect(p, name, a, ba, n, cap, Gm, bGm, psb, iters=30):
    st = p.sb(name + "_st", [128, 8], F32); bst = p.buf()
    cmp_ = p.sb(name + "_cmp", [128, n], BF16); bcmp = p.buf()
    p.op("dve", lambda e: e.memset(st[:, 0:1], 0.0), [], [bst])
    p.op("dve", lambda e: e.memset(st[:, 1:2], 1.0), [bst], [bst])
    p.op("dve", lambda e: e.memset(st[:, 2:3], 0.5), [bst], [bst])
    p.op("dve", lambda e: e.memset(st[:, 3:5], 0.0), [bst], [bst])
    for it in range(iters):
        p.op("dve", lambda e: e.tensor_scalar(out=cmp_[:], in0=a, scalar1=st[:, 2:3], scalar2=0.0, op0=ALU.is_ge, op1=ALU.add,
                                               accum_out=st[:, 3:4]), [ba, bst], [bcmp, bst])
        ps, bps = psb
        MM(p, ps[:, 0:2], [(Gm, st[:, 3:5])], [bGm, bst], [bps])
        TS(p, "dve", st[:, 5:6], ps[:, 0:1], float(cap) - 0.5, None, ALU.is_ge, None, [bps, bst], [bst])
        TT_(p, "dve", st[:, 6:7], st[:, 2:3], st[:, 0:1], ALU.subtract, [bst], [bst])
        STT(p, st[:, 0:1], st[:, 6:7], st[:, 5:6], st[:, 0:1], ALU.mult, ALU.add, [bst], [bst])
        TT_(p, "dve", st[:, 6:7], st[:, 1:2], st[:, 2:3], ALU.subtract, [bst], [bst])
        STT(p, st[:, 1:2], st[:, 6:7], st[:, 5:6], st[:, 2:3], ALU.mult, ALU.add, [bst], [bst])
        TT_(p, "dve", st[:, 6:7], st[:, 0:1], st[:, 1:2], ALU.add, [bst], [bst])
        TS(p, "dve", st[:, 2:3], st[:, 6:7], 0.5, None, ALU.mult, None, [bst], [bst])
    return st[:, 0:1], bst


def moe_consts():
    Gm = np.zeros((128, 128), np.float32)
    for k in range(128):
        Gm[k, (k // 8) * 8:(k // 8) * 8 + 8] = 1.0
    Sel = np.zeros((128, 16, 128), np.float32)
    for e in range(16):
        Sel[e * 8, e, :] = 1.0
    return Gm, Sel


def build_L4(T, has_ctx):
    nc = bass.Bass("TRN2", target_bir_lowering=False)
    dx = nc.dram_tensor("xT", [128, 8, T], F32, kind="ExternalInput").ap()
    dh = nc.dram_tensor("hT", [128, 8, T], BF16, kind="ExternalInput").ap()
    daf = nc.dram_tensor("afull", [128, 2048], F32, kind="ExternalInput").ap()
    dac = nc.dram_tensor("acfull", [128, 32], F32, kind="ExternalInput").ap()
    dao = nc.dram_tensor("aown", [128, T], F32, kind="ExternalInput").ap()
    dmd = nc.dram_tensor("md", [128, 2, 6, 8], F32, kind="ExternalInput").ap()
    dGm = nc.dram_tensor("Gm", [128, 128], F32, kind="ExternalInput").ap()
    dSel = nc.dram_tensor("Sel", [128, 16, 128], F32, kind="ExternalInput").ap()
    dwg = nc.dram_tensor("wg", [16, 1024, 1024], F32, kind="ExternalInput").ap()
    dwu = nc.dram_tensor("wu", [16, 1024, 1024], F32, kind="ExternalInput").ap()
    dwd = nc.dram_tensor("wd", [16, 1024, 1024], F32, kind="ExternalInput").ap()
    ox = nc.dram_tensor("x2T", [128, 8, T], F32, kind="ExternalOutput").ap()
    tiles = tiles_of(T)
    with ExitStack() as es:
        p = Prog(nc, es)
        mdt, bmd = LOAD(p, "sp", [128, 2, 6, 8], F32, "md", dmd)
        Gm, bGm = LOAD(p, "sp", [128, 128], F32, "Gm", dGm)
        Sel, bSel = LOAD(p, "sp", [128, 16, 128], F32, "Sel", dSel)
        af, baf = LOAD(p, "sp", [128, 2048], F32, "af", daf)
        ao, bao = LOAD(p, "sp", [128, T], F32, "ao", dao)
        X = TT(p, "X", [128, 8, T], F32, len(tiles))
        H = TT(p, "H", [128, 8, T], BF16, len(tiles))
        for ti, (c0, w) in enumerate(tiles):
            p.dma("sp", H.t[:, :, c0:c0 + w], dh[:, :, c0:c0 + w], writes=[H.b[ti]])
            p.dma("sp", X.t[:, :, c0:c0 + w], dx[:, :, c0:c0 + w], writes=[X.b[ti]])
        psb = Pool_(p, "psb", [128, 512], F32, 1, psum=True)
        tau, btau = emit_bisect(p, "bx", af[:], baf, 2048, 2048, Gm[:], bGm, (psb.ts[0], psb.bs[0]))
        gw, bgw = ao, bao
        STT(p, gw[:, 0:TX], ao[:, 0:TX], tau, ao[:, 0:TX], ALU.is_ge, ALU.mult, [bao, btau], [bgw])
        if has_ctx:
            ac, bac = LOAD(p, "sp", [128, 32], F32, "ac", dac)
            tauc, btauc = emit_bisect(p, "bc", ac[:], bac, 32, 32, Gm[:], bGm, (psb.ts[0], psb.bs[0]))
            STT(p, gw[:, TX:T], ao[:, TX:T], tauc, ao[:, TX:T], ALU.is_ge, ALU.mult, [bao, btauc, bgw], [bgw])
        Wp = Pool_(p, "W", [128, 8, 1024], BF16, 4)
        Ap = Pool_(p, "A", [128, 8, 512], BF16, 1)
        sgp = Pool_(p, "sg", [128, 512], BF16, 2)
        atp = Pool_(p, "at", [128, 512], BF16, 2)
        gbp = Pool_(p, "gb", [128, 512], BF16, 2)
        psG = Pool_(p, "psG", [128, 512], F32, 4, psum=True)
        psD = Pool_(p, "psD", [128, 512], F32, 2, psum=True)
        psB = Pool_(p, "psB", [128, 512], F32, 1, psum=True)

        def loadw(src):
            wt, bw = Wp.get()
            for kc in range(8):
                p.dma("pool", wt[:, kc, :], src[kc * 128:(kc + 1) * 128, :], writes=[bw])
            return wt, bw

        for e in range(16):
            Wg, bWg = loadw(dwg[e])
            Wu, bWu = loadw(dwu[e])
            Wd, bWd = loadw(dwd[e])
            for ti, (c0, w) in enumerate(tiles):
                cond = 1 if c0 >= TX else 0
                pb, bpb = psB.get()
                MM(p, pb[:, 0:w], [(Sel[:, e, :], gw[:, c0:c0 + w])], [bSel, bgw], [bpb])
                gb, bgb = gbp.get()
                CP(p, "act", gb[:, 0:w], pb[:, 0:w], [bpb], [bgb])
                A_, bA = Ap.get()
                for fc in range(8):
                    pg, bpg = psG.get()
                    MM(p, pg[:, 0:w], [(Wg[:, kc, fc * 128:(fc + 1) * 128], H.t[:, kc, c0:c0 + w]) for kc in range(8)],
                       [bWg, H.b[ti]], [bpg])
                    sg, bsg = sgp.get()
                    ACT(p, sg[:, 0:w], pg[:, 0:w], AF.Silu, [bpg], [bsg])
                    pu, bpu = psG.get()
                    MM(p, pu[:, 0:w], [(Wu[:, kc, fc * 128:(fc + 1) * 128], H.t[:, kc, c0:c0 + w]) for kc in range(8)],
                       [bWu, H.b[ti]], [bpu])
                    at, bat = atp.get()
                    TT_(p, "dve", at[:, 0:w], pu[:, 0:w], sg[:, 0:w], ALU.mult, [bpu, bsg], [bat])
                    TT_(p, "dve", A_[:, fc, 0:w], at[:, 0:w], gb[:, 0:w], ALU.mult, [bat, bgb], [bA])
                for dc in range(8):
                    pd, bpd = psD.get()
                    MM(p, pd[:, 0:w], [(Wd[:, fc, dc * 128:(dc + 1) * 128], A_[:, fc, 0:w]) for fc in range(8)], [bWd, bA], [bpd])
                    STT(p, X.t[:, dc, c0:c0 + w], pd[:, 0:w], mdt[:, cond, 5, dc:dc + 1], X.t[:, dc, c0:c0 + w], ALU.mult, ALU.add,
                        [bpd, bmd, X.b[ti]], [X.b[ti]])
        for ti, (c0, w) in enumerate(tiles):
            p.dma("sp", ox[:, :, c0:c0 + w], X.t[:, :, c0:c0 + w], reads=[X.b[ti]])
        p.emit()
    return nc


def grp_layout(aT):
    n = aT.shape[1]
    return np.ascontiguousarray(aT.reshape(16, 8, n // 8).reshape(128, n // 8))


def run_L4(xsl, hsl, affs, mods, l, wg, wu, wd, T, has_ctx, nc=None):
    if nc is None:
        nc = build_L4(T, has_ctx)
    md = mods_fm(mods, l)
    Gm, Sel = moe_consts()
    ax = np.concatenate([a[:TX] for a in affs], 0)
    afull = grp_layout(np.ascontiguousarray(ax.T))
    if has_ctx:
        ac = np.concatenate([a[TX:] for a in affs], 0)
        acfull = grp_layout(np.ascontiguousarray(ac.T))
    else:
        acfull = np.zeros((128, 32), np.float32)
    maps = []
    for c in range(NCORE):
        aown = np.ascontiguousarray(np.repeat(affs[c].T, 8, axis=0))
        maps.append({"xT": xsl[c], "hT": hsl[c], "afull": afull, "acfull": acfull, "aown": aown, "md": md, "Gm": Gm, "Sel": Sel,
                     "wg": wg, "wu": wu, "wd": wd})
    res = run(nc, maps)
    return [r["x2T"] for r in res]


def rope_consts(core):
    t = core * TX + np.arange(TX)
    row = (t // 64).astype(np.float32)
    col = (t % 64).astype(np.float32)
    inv = (10000.0 ** (-np.arange(0, 32, 2, dtype=np.float32) / 32)).astype(np.float32)
    C = np.zeros((128, TX), np.float32)
    S = np.zeros((128, TX), np.float32)
    for p_ in range(128):
        d = p_ % 64
        pos = row if d < 32 else col
        i = (d % 32) % 16
        ang = pos * inv[i]
        C[p_] = np.cos(ang)
        S[p_] = np.sin(ang)
    PT = np.zeros((128, 128), np.float32)
    for m in range(128):
        if (m % 32) < 16:
            PT[m + 16, m] = -1.0
        else:
            PT[m - 16, m] = 1.0
    BD = np.zeros((128, 128), np.float32)
    BD[:64, :64] = 1.0
    BD[64:, 64:] = 1.0
    return C, S, PT, BD


def build_L5(T):
    nc = bass.Bass("TRN2", target_bir_lowering=False)
    dx = nc.dram_tensor("xT", [128, 8, T], F32, kind="ExternalInput").ap()
    dmd = nc.dram_tensor("md", [128, 2, 6, 8], F32, kind="ExternalInput").ap()
    dg = nc.dram_tensor("g", [128, 8], F32, kind="ExternalInput").ap()
    dw = nc.dram_tensor("wqkv", [1024, 3072], F32, kind="ExternalInput").ap()
    dqk = nc.dram_tensor("qkg", [128, 2], F32, kind="ExternalInput").ap()
    dC = nc.dram_tensor("ropeC", [128, TX], F32, kind="ExternalInput").ap()
    dS = nc.dram_tensor("ropeS", [128, TX], F32, kind="ExternalInput").ap()
    dPT = nc.dram_tensor("PT", [128, 128], F32, kind="ExternalInput").ap()
    dBD = nc.dram_tensor("BD", [128, 128], F32, kind="ExternalInput").ap()
    dones = nc.dram_tensor("ones", [128, 128], F32, kind="ExternalInput").ap()
    oq = nc.dram_tensor("qT", [128, 8, TX], BF16, kind="ExternalOutput").ap()
    ok = nc.dram_tensor("kT", [128, 8, T], BF16, kind="ExternalOutput").ap()
    ov = nc.dram_tensor("v", [T, 1024], BF16, kind="ExternalOutput").ap()
    tiles = tiles_of(T)
    with ExitStack() as es:
        p = Prog(nc, es)
        C = make_common(p, nc, dones)
        mdt, bmd = LOAD(p, "sp", [128, 2, 6, 8], F32, "md", dmd)
        gt, bg = LOAD(p, "sp", [128, 8], F32, "g", dg)
        qkg, bqkg = LOAD(p, "sp", [128, 2], F32, "qkg", dqk)
        RC, bRC = LOAD(p, "sp", [128, TX], F32, "RC", dC)
        RS, bRS = LOAD(p, "sp", [128, TX], F32, "RS", dS)
        PT, bPT = LOAD(p, "sp", [128, 128], F32, "PT", dPT)
        BD, bBD = LOAD(p, "pool", [128, 128], BF16, "BD", dBD)
        X = TT(p, "X", [128, 8, T], F32, len(tiles))
        H = TT(p, "H", [128, 8, T], BF16, len(tiles))
        for ti, (c0, w) in enumerate(tiles):
            p.dma("sp", X.t[:, :, c0:c0 + w], dx[:, :, c0:c0 + w], writes=[X.b[ti]])
        gmx, bgmx = emit_gmod(p, "gmx", gt[:], bg, mdt[:, 0, 1, :], bmd)
        gmc, bgmc = emit_gmod(p, "gmc", gt[:], bg, mdt[:, 1, 1, :], bmd)
        for ti, (c0, w) in enumerate(tiles):
            cond = 1 if c0 >= TX else 0
            gm, bgm = (gmc, bgmc) if cond else (gmx, bgmx)
            rms_mod_tile(p, C, lambda kc: X.t[:, kc, c0:c0 + w], X.b[ti], w, gm, bgm, mdt[:, cond, 0, :], bmd,
                         lambda kc: H.t[:, kc, c0:c0 + w], H.b[ti])
        Wm = Pool_(p, "Wm", [128, 8, 128], BF16, 3)
        qr = Pool_(p, "qr", [128, 512], F32, 2)
        sqp = Pool_(p, "sq1", [128, 512], BF16, 2)
        qn = Pool_(p, "qn", [128, 512], F32, 2)
        t1p = Pool_(p, "t1", [128, 512], F32, 2)
        t2p = Pool_(p, "t2", [128, 512], F32, 2)
        ob = Pool_(p, "ob", [128, 512], BF16, 3)
        ps2 = Pool_(p, "ps2", [128, 512], F32, 4, psum=True)
        for m in range(16):
            isq = m < 8
            wt, bw = Wm.get()
            p.dma("pool", wt[:], dw[:, m * 128:(m + 1) * 128].rearrange("(kc p) n -> p kc n", p=128), writes=[bw])
            gcol = qkg[:, 0:1] if isq else qkg[:, 1:2]
            for ti, (c0, w) in enumerate(tiles):
                isctx = c0 >= TX
                if isq and isctx:
                    continue
                ps, bps = ps2.get()
                MM(p, ps[:, 0:w], [(wt[:, kc, :], H.t[:, kc, c0:c0 + w]) for kc in range(8)], [bw, H.b[ti]], [bps])
                q_, bq_ = qr.get()
                CP(p, "act", q_[:, 0:w], ps[:, 0:w], [bps], [bq_])
                sq, bsq = sqp.get()
                ACT(p, sq[:, 0:w], ps[:, 0:w], AF.Square, [bps], [bsq])
                pss, bpss = ps2.get()
                MM(p, pss[:, 0:w], [(BD[:], sq[:, 0:w])], [bBD, bsq], [bpss])
                rs, brs = C.rs.get()
                ACT(p, rs[:, 0:w], pss[:, 0:w], AF.Sqrt, [bpss], [brs], scale=1.0 / 64, bias=1e-6)
                p.op("dve", lambda e, rs=rs, w=w: e.reciprocal(out=rs[:, 0:w], in_=rs[:, 0:w]), [brs], [brs])
                n_, bn_ = qn.get()
                STT(p, n_[:, 0:w], q_[:, 0:w], gcol, rs[:, 0:w], ALU.mult, ALU.mult, [bq_, bqkg, brs], [bn_])
                o_, bo_ = ob.get()
                if not isctx:
                    pr, bpr = ps2.get()
                    MM(p, pr[:, 0:w], [(PT[:], n_[:, 0:w])], [bPT, bn_], [bpr])
                    t1, b1 = t1p.get()
                    t2, b2 = t2p.get()
                    TT_(p, "pool", t1[:, 0:w], n_[:, 0:w], RC[:, c0:c0 + w], ALU.mult, [bn_, bRC], [b1])
                    TT_(p, "dve", t2[:, 0:w], pr[:, 0:w], RS[:, c0:c0 + w], ALU.mult, [bpr, bRS], [b2])
                    TT_(p, "pool", o_[:, 0:w], t1[:, 0:w], t2[:, 0:w], ALU.add, [b1, b2], [bo_])
                else:
                    CP(p, "act", o_[:, 0:w], n_[:, 0:w], [bn_], [bo_])
                if isq:
                    p.dma("sp", oq[:, m, c0:c0 + w], o_[:, 0:w], reads=[bo_])
                else:
                    p.dma("sp", ok[:, m - 8, c0:c0 + w], o_[:, 0:w], reads=[bo_])
        Wv = p.sb("Wv", [128, 8, 1024], BF16); bWv = p.buf()
        for kc in range(8):
            p.dma("pool", Wv[:, kc, :], dw[kc * 128:(kc + 1) * 128, 2048:3072], writes=[bWv])
        vb = Pool_(p, "vb", [128, 512], BF16, 3)
        for ti, (c0, w) in enumerate(tiles):
            for j0 in range(0, w, 128):
                tw = min(128, w - j0)
                for hh in range(2):
                    ps, bps = ps2.get()
                    MM(p, ps[0:tw, :], [(H.t[:, kc, c0 + j0:c0 + j0 + tw], Wv[:, kc, hh * 512:(hh + 1) * 512]) for kc in range(8)],
                       [H.b[ti], bWv], [bps])
                    v_, bv_ = vb.get()
                    CP(p, "act", v_[0:tw, :], ps[0:tw, :], [bps], [bv_])
                    p.dma("sp", ov[c0 + j0:c0 + j0 + tw, hh * 512:(hh + 1) * 512], v_[0:tw, :], reads=[bv_])
        p.emit()
    return nc


def run_L5(inp, xsl, mods, T):
    nc = build_L5(T)
    md = mods_fm(mods, 1)
    qkg = np.ascontiguousarray(np.stack([np.tile(inp["da_q_norm"][0], 2), np.tile(inp["da_k_norm"][0], 2)], axis=1))
    maps = []
    for c in range(NCORE):
        C_, S_, PT, BD = rope_consts(c)
        maps.append({"xT": xsl[c], "md": md, "g": vec_fm(inp["norm_mix"][1]), "wqkv": inp["da_w_qkv"][0], "qkg": qkg,
                     "ropeC": C_, "ropeS": S_, "PT": PT, "BD": BD, "ones": ONES_F32})
    res = run(nc, maps)
    return [np.asarray(r["qT"]) for r in res], [np.asarray(r["kT"]) for r in res], [np.asarray(r["v"]) for r in res]


LAM_INIT = 0.8 - 0.6 * float(np.exp(-0.3 * 1))
NKEY = 16384 + 256
NKC = NKEY // 128


def build_L6():
    nc = bass.Bass("TRN2", target_bir_lowering=False)
    dq = nc.dram_tensor("qT", [128, 8, TX], BF16, kind="ExternalInput").ap()
    dk = nc.dram_tensor("kT", [8, 128, NKEY], BF16, kind="ExternalInput").ap()
    dv = nc.dram_tensor("v", [8, 128, NKC, 128], BF16, kind="ExternalInput").ap()
    dlam = nc.dram_tensor("lamv", [128, 4, 64], F32, kind="ExternalInput").ap()
    dgn = nc.dram_tensor("gains", [128, 2, 64], F32, kind="ExternalInput").ap()
    dsub = nc.dram_tensor("subln", [128, 1], F32, kind="ExternalInput").ap()
    dones = nc.dram_tensor("ones", [128, 128], F32, kind="ExternalInput").ap()
    oo_ = nc.dram_tensor("oT", [128, 8, TX], BF16, kind="ExternalOutput").ap()
    with ExitStack() as es:
        p = Prog(nc, es)
        ones, bones = LOAD(p, "pool", [128, 128], BF16, "ones", dones)
        lamv, blamv = LOAD(p, "sp", [128, 4, 64], F32, "lamv", dlam)
        gn, bgn = LOAD(p, "sp", [128, 2, 64], F32, "gn", dgn)
        st = p.sb("st", [128, 16], F32); bst = p.buf()
        p.dma("sp", st[:, 0:1], dsub, writes=[bst])
        sc = p.sb("scr", [128, 64], F32); bsc = p.buf()
        for i in range(2):
            TT_(p, "dve", sc[:], lamv[:, 2 * i, :], lamv[:, 2 * i + 1, :], ALU.mult, [blamv, bsc], [bsc])
            p.op("dve", lambda e, i=i: e.tensor_reduce(out=st[:, 1 + i:2 + i], in_=sc[:], axis=AX.X, op=ALU.add), [bsc, bst], [bst])
        ACT(p, st[:, 1:3], st[:, 1:3], AF.Exp, [bst], [bst])
        TT_(p, "dve", st[:, 3:4], st[:, 2:3], st[:, 1:2], ALU.subtract, [bst], [bst])
        TS(p, "dve", st[:, 3:4], st[:, 3:4], -LAM_INIT, None, ALU.add, None, [bst], [bst])
        for i in range(2):
            p.op("dve", lambda e, i=i: e.tensor_reduce(out=st[:, 4 + i:5 + i], in_=gn[:, i, :], axis=AX.X, op=ALU.max,
                                                       apply_absolute_value=True), [bgn, bst], [bst])
        TT_(p, "dve", st[:, 6:7], st[:, 4:5], st[:, 5:6], ALU.mult, [bst], [bst])
        TS(p, "dve", st[:, 6:7], st[:, 6:7], -8.0, None, ALU.mult, None, [bst], [bst])
        TS(p, "dve", st[:, 7:8], st[:, 0:1], 1.0 - LAM_INIT, None, ALU.mult, None, [bst], [bst])
        Q, bQ = LOAD(p, "sp", [128, 8, TX], BF16, "Q", dq)
        Kp = Pool_(p, "K", [128, NKEY], BF16, 2)
        Vp = Pool_(p, "V", [128, NKC, 128], BF16, 2)
        Pp = Pool_(p, "P", [128, 512], BF16, 6)
        psS = Pool_(p, "psS", [128, 512], F32, 4, psum=True)
        psO = [p.ps(f"psO{i}", [128, 512], F32) for i in range(4)]
        bO = [p.buf() for _ in range(4)]
        ep = Pool_(p, "ep", [128, 512], F32, 6)
        sqp = Pool_(p, "sqe", [128, 512], BF16, 2)
        obp = Pool_(p, "obe", [128, 512], BF16, 2)
        for hp in range(8):
            Kh, bK = Kp.get()
            p.dma("sp", Kh[:], dk[hp], writes=[bK])
            Vh, bV = Vp.get()
            p.dma("sp", Vh[:], dv[hp], writes=[bV])
            for qt in range(TX // 512):
                q0 = qt * 512

                def S(kc):
                    out = []
                    for s_ in range(2):
                        ps, bps = psS.get()
                        lo = 64 * s_
                        MM(p, ps[:, :], [(Kh[lo:lo + 64, kc * 128:(kc + 1) * 128], Q[lo:lo + 64, hp, q0:q0 + 512])], [bK, bQ], [bps])
                        out.append((ps, bps))
                    return out

                cur = S(0)
                for kc in range(NKC):
                    nxt = S(kc + 1) if kc + 1 < NKC else None
                    for s_ in range(2):
                        ps, bps = cur[s_]
                        P_, bP = Pp.get()
                        ACT(p, P_[:], ps[:, :], AF.Exp, [bps, bst], [bP], scale=0.125, bias=st[:, 6:7])
                        MMx(p, psO[2 * s_][:, :], Vh[:, kc, :], P_[:], kc == 0, kc == NKC - 1, [bV, bP], [bO[2 * s_]])
                        MMx(p, psO[2 * s_ + 1][:, :], ones[:], P_[:], kc == 0, kc == NKC - 1, [bones, bP], [bO[2 * s_ + 1]])
                    cur = nxt
                r0, br0 = ep.get()
                p.op("dve", lambda e, r0=r0: e.reciprocal(out=r0[:], in_=psO[1][:, :]), [bO[1]], [br0])
                t0, bt0 = ep.get()
                TT_(p, "dve", t0[:], psO[0][:, :], r0[:], ALU.mult, [bO[0], br0], [bt0])
                r1, br1 = ep.get()
                p.op("dve", lambda e, r1=r1: e.reciprocal(out=r1[:], in_=psO[3][:, :]), [bO[3]], [br1])
                t1, bt1 = ep.get()
                TT_(p, "dve", t1[:], psO[2][:, :], r1[:], ALU.mult, [bO[2], br1], [bt1])
                o_, bo_ = ep.get()
                STT(p, o_[:], t1[:], st[:, 3:4], t0[:], ALU.mult, ALU.add, [bt1, bt0, bst], [bo_])
                sq, bsq = sqp.get()
                ACT(p, sq[:], o_[:], AF.Square, [bo_], [bsq])
                pss, bpss = psS.get()
                MM(p, pss[:, :], [(ones[:], sq[:])], [bones, bsq], [bpss])
                rs, brs = ep.get()
                ACT(p, rs[:], pss[:, :], AF.Sqrt, [bpss], [brs], scale=1.0 / 128, bias=1e-5)
                p.op("dve", lambda e, rs=rs: e.reciprocal(out=rs[:], in_=rs[:]), [brs], [brs])
                ob, bob = obp.get()
                STT(p, ob[:], o_[:], st[:, 7:8], rs[:], ALU.mult, ALU.mult, [bo_, bst, brs], [bob])
                p.dma("sp", oo_[:, hp, q0:q0 + 512], ob[:], reads=[bob])
        p.emit()
    return nc


def run_L6(inp, q, k, v):
    nc = build_L6()
    kx = np.concatenate([a[:, :, :TX] for a in k], axis=2)
    kc = np.concatenate([a[:, :, TX:] for a in k], axis=2)
    kall = np.ascontiguousarray(np.concatenate([kc, kx], axis=2).transpose(1, 0, 2))
    vx = np.concatenate([a[:TX] for a in v], axis=0)
    vc = np.concatenate([a[TX:] for a in v], axis=0)
    vall = np.concatenate([vc, vx], axis=0).reshape(NKC, 128, 8, 128)
    vall = np.ascontiguousarray(vall.transpose(2, 1, 0, 3))
    lamv = np.ascontiguousarray(np.broadcast_to(np.stack([inp["da_lam_q1"][0], inp["da_lam_k1"][0], inp["da_lam_q2"][0],
                                                          inp["da_lam_k2"][0]])[None], (128, 4, 64)))
    gains = np.ascontiguousarray(np.broadcast_to(np.stack([inp["da_q_norm"][0], inp["da_k_norm"][0]])[None], (128, 2, 64)))
    sub = np.ascontiguousarray(inp["da_subln"][0].reshape(128, 1))
    maps = [{"qT": q[c], "kT": kall, "v": vall, "lamv": lamv, "gains": gains, "subln": sub, "ones": ONES_F32} for c in range(NCORE)]
    res = run(nc, maps)
    return [np.asarray(r["oT"]) for r in res]


def kernel(**inp):
    inp = {k: np.asarray(v) for k, v in inp.items()}
    x = inp["x"][0]
    ctx = inp["ctx"][0]
    T0 = TX + TC
    mods = run_L0(inp)
    xsl = shard_tokens(x, ctx)
    h = run_L1(xsl, mods, 0, inp["norm_mix"][0], T0)
    hx, hc = unshard_tokens([np.asarray(a) for a in h], True)
    u, uc = run_HA(inp, hx, hc)
    tabs = hc_tables()
    nc_hc = build_HC()
    zext, dext = hyena_pos_tables(16384)
    hid = run_LF(inp, zext)
    z = run_HC(inp, u, hid, dext, tabs, nc=nc_hc)
    zext_c, dext_c = hyena_pos_tables(256)
    hid_c = run_LF(inp, zext_c)
    zc = run_HC(inp, uc, hid_c, dext_c, tabs, nc=nc_hc)
    zsl = shard_tokens(np.asarray(z), np.asarray(zc))
    x1, h2, aff = run_L3(zsl, xsl, inp["hy_w_out"][0], inp["hy_b_out"][0], mods, 0, inp["norm_ffn"][0],
                         inp["moe_router"][0], T0, True)
    x2 = run_L4([np.asarray(a) for a in x1], [np.asarray(a) for a in h2], aff, mods, 0,
                inp["moe_w_gate"][0], inp["moe_w_up"][0], inp["moe_w_down"][0], T0, True)
    x2 = [np.asarray(a) for a in x2]
    q, k, v = run_L5(inp, x2, mods, T0)
    o = run_L6(inp, q, k, v)
    xs1 = [np.ascontiguousarray(a[:, :, :TX]) for a in x2]
    x3, h3, aff1 = run_L3(o, xs1, inp["da_w_out"][0], None, mods, 1, inp["norm_ffn"][1], inp["moe_router"][1], TX, False)
    x4 = run_L4([np.asarray(a) for a in x3], [np.asarray(a) for a in h3], aff1, mods, 1,
                inp["moe_w_gate"][1], inp["moe_w_up"][1], inp["moe_w_down"][1], TX, False)
    xo, _ = unshard_tokens([np.asarray(a) for a in x4], False)
    return np.ascontiguousarray(xo[None]).astype(np.float32)
```

```python
import numpy as np
from contextlib import ExitStack
import concourse.bass as bass
import concourse.mybir as mybir
from concourse.bass_utils import run_bass_kernel_spmd

F32 = mybir.dt.float32
BF16 = mybir.dt.bfloat16
I32 = mybir.dt.int32
ALU = mybir.AluOpType
AF = mybir.ActivationFunctionType
AX = mybir.AxisListType

EPOCH = 30000
N_DMA_SEMS = 24


class Buf:
    __slots__ = ("name", "w", "r")

    def __init__(self, name=""):
        self.name = name
        self.w = None
        self.r = {}


class Prog:
    ENGS = ("pe", "act", "dve", "pool", "sp")

    def __init__(self, nc, es):
        self.nc = nc
        self.es = es
        self.items = {e: [] for e in self.ENGS}
        self.cnt = {e: 0 for e in self.ENGS}
        self.esems = {e: [es.enter_context(nc.semaphore(f"s_{e}_0"))] for e in self.ENGS}
        self.seen = {e: {} for e in self.ENGS}
        self.dsems = {}
        self.dcount = {}
        self.drr = {}
        for q in ("sp", "pool", "act"):
            self.dsems[q] = [es.enter_context(nc.semaphore(f"d_{q}_{i}")) for i in range(N_DMA_SEMS)]
            self.dcount[q] = [0] * N_DMA_SEMS
            self.drr[q] = 0
        self.all_dma_tokens = []
        self.nbuf = 0

    def sb(self, name, shape, dt):
        t = self.es.enter_context(self.nc.sbuf_tensor("sb_" + name, list(shape), dt))
        return t

    def ps(self, name, shape, dt=F32):
        t = self.es.enter_context(self.nc.psum_tensor("ps_" + name, list(shape), dt))
        return t

    def buf(self, name=""):
        self.nbuf += 1
        return Buf(name or f"b{self.nbuf}")

    def _wait(self, eng, tok):
        if tok is None:
            return
        key, sem, val = tok
        if self.seen[eng].get(key, 0) >= val:
            return
        self.seen[eng][key] = val
        self.items[eng].append(("wait", sem, val))

    def _deps(self, eng, reads, writes):
        for b in reads:
            if b.w is not None:
                self._dep1(eng, b.w)
        for b in writes:
            if b.w is not None and b.w[0][0] != eng:
                self._dep1(eng, b.w)
            for t in b.r.values():
                if t[0][0] != eng:
                    self._dep1(eng, t)

    def _dep1(self, eng, tok):
        if eng == "pe" and tok[0][0] == "pe":
            return
        self._wait(eng, tok)

    def _mark(self, eng, tok, reads, writes):
        for b in reads:
            b.r[tok[0]] = tok
        for b in writes:
            b.w = tok
            b.r = {}

    def op(self, eng, fn, reads=(), writes=()):
        self._deps(eng, reads, writes)
        ep = self.cnt[eng] // EPOCH
        if ep >= len(self.esems[eng]):
            self.esems[eng].append(self.es.enter_context(self.nc.semaphore(f"s_{eng}_{ep}")))
        self.cnt[eng] += 1
        sem = self.esems[eng][ep]
        val = self.cnt[eng] - ep * EPOCH
        self.items[eng].append(("ins", fn, sem, 1))
        tok = ((eng, ep), sem, val)
        self._mark(eng, tok, reads, writes)
        return tok

    def group(self, eng, fns, reads=(), writes=()):
        self._deps(eng, reads, writes)
        tok = None
        for fn in fns:
            ep = self.cnt[eng] // EPOCH
            if ep >= len(self.esems[eng]):
                self.esems[eng].append(self.es.enter_context(self.nc.semaphore(f"s_{eng}_{ep}")))
            self.cnt[eng] += 1
            sem = self.esems[eng][ep]
            val = self.cnt[eng] - ep * EPOCH
            self.items[eng].append(("ins", fn, sem, 1))
            tok = ((eng, ep), sem, val)
        self._mark(eng, tok, reads, writes)
        return tok

    def dma(self, q, out, in_, reads=(), writes=(), **kw):
        self._deps(q, reads, writes)
        k = self.drr[q]
        self.drr[q] = (k + 1) % N_DMA_SEMS
        n = self.dcount[q][k]
        sem = self.dsems[q][k]
        if n > 0:
            self._wait(q, (("d", q, k), sem, 16 * n))
        self.dcount[q][k] = n + 1
        tok = (("d", q, k), sem, 16 * (n + 1))

        def fn(e, out=out, in_=in_, kw=kw):
            return e.dma_start(out=out, in_=in_, **kw)

        self.items[q].append(("ins", fn, sem, 16))
        self._mark(q, tok, reads, writes)
        self.all_dma_tokens.append(tok)
        return tok

    def coll(self, kind, in_ap, out_ap, reads=(), writes=(), groups=None):
        q = "pool"
        if not hasattr(self, "ccsem"):
            self.ccsem = self.es.enter_context(self.nc.semaphore("cc_sem"))
            self.ccn = 0
        self._deps(q, reads, writes)
        self.ccn += 1
        tok = (("cc",), self.ccsem, self.ccn)
        groups = groups or [list(range(8))]

        def fn(e):
            return e.collective_compute(kind, mybir.AluOpType.bypass, replica_groups=groups, ins=[in_ap.opt()], outs=[out_ap.opt()])

        self.items[q].append(("ins", fn, self.ccsem, 1))
        self._mark(q, tok, reads, writes)
        self.all_dma_tokens.append(tok)
        return tok

    def finish(self):
        last = {}
        for tok in self.all_dma_tokens:
            last[tok[0]] = tok
        for tok in last.values():
            self._wait("sp", tok)

    def emit(self):
        nc = self.nc
        self.finish()
        items = self.items
        with nc.Block() as block:
            def run(e, lst):
                for it in lst:
                    if it[0] == "wait":
                        e.wait_ge(it[1], it[2])
                    else:
                        it[1](e).then_inc(it[2], it[3])

            @block.tensor
            def _(e):
                run(e, items["pe"])

            @block.scalar
            def _(e):
                run(e, items["act"])

            @block.vector
            def _(e):
                run(e, items["dve"])

            @block.gpsimd
            def _(e):
                run(e, items["pool"])

            @block.sync
            def _(e):
                run(e, items["sp"])


class TT:
    def __init__(self, p, name, shape, dt, ntiles):
        self.t = p.sb(name, shape, dt)
        self.b = [p.buf(f"{name}_{i}") for i in range(ntiles)]


class Pool_:
    def __init__(self, p, name, shape, dt, n, psum=False):
        mk = p.ps if psum else p.sb
        self.ts = [mk(f"{name}{i}", shape, dt) for i in range(n)]
        self.bs = [p.buf(f"{name}{i}") for i in range(n)]
        self.i = 0

    def get(self):
        k = self.i
        self.i = (k + 1) % len(self.ts)
        return self.ts[k], self.bs[k]


def col_tiles(T, w=512):
    out = []
    c = 0
    while c < T:
        out.append((c, min(w, T - c)))
        c += w
    return out


def ACT(p, out, in_, func, reads, writes, **kw):
    return p.op("act", lambda e: e.activation(out=out, in_=in_, func=func, **kw), reads, writes)


def TT_(p, eng, out, in0, in1, op, reads, writes):
    return p.op(eng, lambda e: e.tensor_tensor(out=out, in0=in0, in1=in1, op=op), reads, writes)


def TS(p, eng, out, in0, s1, s2, op0, op1, reads, writes, **kw):
    if op1 is None:
        return p.op(eng, lambda e: e.tensor_scalar(out=out, in0=in0, scalar1=s1, scalar2=None, op0=op0, **kw), reads, writes)
    return p.op(eng, lambda e: e.tensor_scalar(out=out, in0=in0, scalar1=s1, scalar2=s2, op0=op0, op1=op1, **kw), reads, writes)


def STT(p, out, in0, scalar, in1, op0, op1, reads, writes):
    return p.op("dve", lambda e: e.scalar_tensor_tensor(out=out, in0=in0, scalar=scalar, in1=in1, op0=op0, op1=op1), reads, writes)


def CP(p, eng, out, in_, reads, writes):
    if eng == "act":
        return p.op("act", lambda e: e.activation(out=out, in_=in_, func=AF.Copy), reads, writes)
    return p.op(eng, lambda e: e.tensor_copy(out=out, in_=in_), reads, writes)


def MM(p, out, pairs, reads, writes):
    n = len(pairs)
    fns = []
    for i, (l, r) in enumerate(pairs):
        fns.append(lambda e, l=l, r=r, i=i: e.matmul(out, l, r, start=(i == 0), stop=(i == n - 1)))
    return p.group("pe", fns, reads, writes)


def LOAD(p, q, shape, dt, name, src, es=None):
    t = p.sb(name, shape, dt)
    b = p.buf(name)
    p.dma(q, t[:], src, writes=[b])
    return t, b


class Ctx:
    pass


def make_common(p, nc, ones_src):
    C = Ctx()
    C.ones, C.bones = LOAD(p, "pool", [128, 128], BF16, "ones", ones_src)
    C.sq = Pool_(p, "sq", [128, 8, 512], BF16, 2)
    C.ps = Pool_(p, "psA", [128, 512], F32, 4, psum=True)
    C.rs = Pool_(p, "rs", [128, 512], F32, 2)
    C.tmp = Pool_(p, "tmp", [128, 512], F32, 3)
    return C


def emit_gmod(p, name, g, bg, scale, bscale, ncol=8):
    gm = p.sb(name, [128, ncol], F32)
    bgm = p.buf(name)
    STT(p, gm[:], scale, 1.0, g, ALU.add, ALU.mult, [bg, bscale], [bgm])
    return gm, bgm


def emit_rms_mod(p, C, X, tiles, mods_of_tile, OUT, OUTF=None, eps=1e-6, D=1024):
    for ti, (c0, w) in enumerate(tiles):
        gm, bgm, sh, bsh = mods_of_tile(ti)
        sq, bsq = C.sq.get()
        ACT(p, sq[:, :, 0:w], X.t[:, :, c0:c0 + w], AF.Square, [X.b[ti]], [bsq])
        ps, bps = C.ps.get()
        MM(p, ps[:, 0:w], [(C.ones[:], sq[:, kc, 0:w]) for kc in range(8)], [bsq, C.bones], [bps])
        rs, brs = C.rs.get()
        ACT(p, rs[:, 0:w], ps[:, 0:w], AF.Sqrt, [bps], [brs], scale=1.0 / D, bias=eps)
        p.op("dve", lambda e, rs=rs, w=w: e.reciprocal(out=rs[:, 0:w], in_=rs[:, 0:w]), [brs], [brs])
        for kc in range(8):
            tmp, btmp = C.tmp.get()
            STT(p, tmp[:, 0:w], X.t[:, kc, c0:c0 + w], gm[:, kc:kc + 1], rs[:, 0:w], ALU.mult, ALU.mult,
                [X.b[ti], brs, bgm], [btmp])
            ACT(p, OUT.t[:, kc, c0:c0 + w], tmp[:, 0:w], AF.Identity, [btmp, bsh], [OUT.b[ti]],
                bias=sh[:, kc:kc + 1], scale=1.0)
            if OUTF is not None:
                ACT(p, OUTF.t[:, kc, c0:c0 + w], tmp[:, 0:w], AF.Identity, [btmp, bsh], [OUTF.b[ti]],
                    bias=sh[:, kc:kc + 1], scale=1.0)


def fm(a):
    T = a.shape[0]
    return np.ascontiguousarray(a.T.reshape(8, 128, T).transpose(1, 0, 2))


def unfm(a):
    T = a.shape[2]
    return np.ascontiguousarray(a.transpose(1, 0, 2).reshape(1024, T).T)


def vec_fm(v, n=8):
    return np.ascontiguousarray(v.reshape(n, 128).T)


NCORE = 8
TX = 2048
TC = 32
ONES_F32 = np.ones((128, 128), np.float32)


def run(nc, in_maps):
    res = run_bass_kernel_spmd(nc, in_maps, core_ids=list(range(NCORE)))
    return res.results


def build_L0():
    nc = bass.Bass("TRN2", target_bir_lowering=False)
    condT = nc.dram_tensor("condT", [128, 8, 2], F32, kind="ExternalInput").ap()
    w = nc.dram_tensor("w", [2, 128, 8, 768], F32, kind="ExternalInput").ap()
    b = nc.dram_tensor("b", [2, 2, 768], F32, kind="ExternalInput").ap()
    out = nc.dram_tensor("out", [2, 2, 768], F32, kind="ExternalOutput").ap()
    with ExitStack() as es:
        p = Prog(nc, es)
        ct, bct = LOAD(p, "sp", [128, 8, 2], F32, "ct", condT)
        sc = p.sb("sc", [128, 8, 2], F32); bsc = p.buf()
        ACT(p, sc[:], ct[:], AF.Silu, [bct], [bsc])
        psp = Pool_(p, "ps", [128, 512], F32, 2, psum=True)
        for l in range(2):
            wt, bwt = LOAD(p, "sp", [128, 8, 768], F32, f"w{l}", w[l])
            bt, bbt = LOAD(p, "sp", [2, 768], F32, f"b{l}", b[l])
            ot = p.sb(f"o{l}", [2, 768], F32); bot = p.buf()
            for h in range(2):
                ps, bps = psp.get()
                MM(p, ps[0:2, 0:384], [(sc[:, kc, :], wt[:, kc, h * 384:(h + 1) * 384]) for kc in range(8)],
                   [bsc, bwt], [bps])
                TT_(p, "dve", ot[:, h * 384:(h + 1) * 384], ps[0:2, 0:384], bt[:, h * 384:(h + 1) * 384], ALU.add,
                    [bps, bbt], [bot])
            p.dma("sp", out[l], ot[:], reads=[bot])
        p.emit()
    return nc


def run_L0(inp):
    nc = build_L0()
    cond = np.stack([inp["c"][0], inp["c_ctx"]], axis=1)
    condT = np.ascontiguousarray(cond.reshape(8, 128, 2).transpose(1, 0, 2))
    maps = []
    for c in range(NCORE):
        sl = slice(c * 768, (c + 1) * 768)
        w = np.ascontiguousarray(inp["ada_w"][:, :, sl].reshape(2, 8, 128, 768).transpose(0, 2, 1, 3))
        b = np.ascontiguousarray(np.broadcast_to(inp["ada_b"][:, None, sl], (2, 2, 768)))
        maps.append({"condT": condT, "w": w, "b": b})
    res = run(nc, maps)
    mods = np.concatenate([r["out"] for r in res], axis=2)
    return mods


def mods_fm(mods, l):
    m = mods[l].reshape(2, 6, 8, 128)
    return np.ascontiguousarray(m.transpose(3, 0, 1, 2))


def tiles_of(T):
    return col_tiles(T, 512)


def build_L1(T):
    nc = bass.Bass("TRN2", target_bir_lowering=False)
    xT = nc.dram_tensor("xT", [128, 8, T], F32, kind="ExternalInput").ap()
    md = nc.dram_tensor("md", [128, 2, 6, 8], F32, kind="ExternalInput").ap()
    g = nc.dram_tensor("g", [128, 8], F32, kind="ExternalInput").ap()
    ones = nc.dram_tensor("ones", [128, 128], F32, kind="ExternalInput").ap()
    hT = nc.dram_tensor("hT", [128, 8, T], BF16, kind="ExternalOutput").ap()
    tiles = tiles_of(T)
    with ExitStack() as es:
        p = Prog(nc, es)
        C = make_common(p, nc, ones)
        mdt, bmd = LOAD(p, "sp", [128, 2, 6, 8], F32, "md", md)
        gt, bg = LOAD(p, "sp", [128, 8], F32, "g", g)
        X = TT(p, "X", [128, 8, T], F32, len(tiles))
        H = TT(p, "H", [128, 8, T], BF16, len(tiles))
        for ti, (c0, w) in enumerate(tiles):
            p.dma("sp", X.t[:, :, c0:c0 + w], xT[:, :, c0:c0 + w], writes=[X.b[ti]])
        gmx, bgmx = emit_gmod(p, "gmx", gt[:], bg, mdt[:, 0, 1, :], bmd)
        gmc, bgmc = emit_gmod(p, "gmc", gt[:], bg, mdt[:, 1, 1, :], bmd)

        def mods_of_tile(ti):
            c0, w = tiles[ti]
            if c0 >= TX:
                return gmc, bgmc, mdt[:, 1, 0, :], bmd
            return gmx, bgmx, mdt[:, 0, 0, :], bmd

        emit_rms_mod(p, C, X, tiles, mods_of_tile, H)
        for ti, (c0, w) in enumerate(tiles):
            p.dma("sp", hT[:, :, c0:c0 + w], H.t[:, :, c0:c0 + w], reads=[H.b[ti]])
        p.emit()
    return nc


def shard_tokens(x, ctx):
    out = []
    for c in range(NCORE):
        parts = [x[c * TX:(c + 1) * TX]]
        if ctx is not None:
            parts.append(ctx[c * TC:(c + 1) * TC])
        out.append(fm(np.concatenate(parts, axis=0)))
    return out


def unshard_tokens(slabs, has_ctx):
    xs, cs = [], []
    for s in slabs:
        a = unfm(s)
        xs.append(a[:TX])
        if has_ctx:
            cs.append(a[TX:])
    return np.concatenate(xs, 0), (np.concatenate(cs, 0) if has_ctx else None)


def run_L1(xsl, mods, l, g, T):
    nc = build_L1(T)
    md = mods_fm(mods, l)
    maps = [{"xT": xsl[c], "md": md, "g": vec_fm(g), "ones": ONES_F32} for c in range(NCORE)]
    res = run(nc, maps)
    return [r["hT"] for r in res]


PI = float(np.pi)


def build_LF(P):
    nc = bass.Bass("TRN2", target_bir_lowering=False)
    zT = nc.dram_tensor("zT", [33, P], F32, kind="ExternalInput").ap()
    w1 = nc.dram_tensor("w1", [33, 64], F32, kind="ExternalInput").ap()
    w23 = nc.dram_tensor("w23", [64, 2, 64], F32, kind="ExternalInput").ap()
    bf = nc.dram_tensor("bf", [64, 4], F32, kind="ExternalInput").ap()
    hid = nc.dram_tensor("hid", [64, P], BF16, kind="ExternalOutput").ap()
    tiles = col_tiles(P)
    with ExitStack() as es:
        p = Prog(nc, es)
        w1t, bw1 = LOAD(p, "sp", [33, 64], F32, "w1", w1)
        w23t, bw23 = LOAD(p, "sp", [64, 2, 64], F32, "w23", w23)
        bft, bbf = LOAD(p, "sp", [64, 4], F32, "bf", bf)
        bfr = p.sb("bfr", [64, 3], F32); bbfr = p.buf()
        TS(p, "dve", bfr[:], bft[:, 0:3], bft[:, 3:4], None, ALU.mult, None, [bbf], [bbfr])
        zp = Pool_(p, "z", [33, 512], F32, 2)
        psp = Pool_(p, "ps", [64, 512], F32, 3, psum=True)
        ap_ = Pool_(p, "arg", [64, 512], F32, 3)
        hp = Pool_(p, "h", [64, 512], F32, 3)
        op_ = Pool_(p, "o", [64, 512], BF16, 2)
        wp = Pool_(p, "wr", [64, 512], F32, 2)
        for (c0, w) in tiles:
            zt, bz = zp.get()
            p.dma("sp", zt[:, 0:w], zT[:, c0:c0 + w], writes=[bz])
            cur, bcur = zt, bz
            for l in range(3):
                ps, bps = psp.get()
                lhsT = w1t[:] if l == 0 else w23t[:, l - 1, :]
                bl = bw1 if l == 0 else bw23
                MM(p, ps[:, 0:w], [(lhsT, cur[:, 0:w])], [bl, bcur], [bps])
                a, ba = ap_.get()
                ACT(p, a[:, 0:w], ps[:, 0:w], AF.Identity, [bps, bbf, bbfr], [ba], scale=bft[:, 3:4], bias=bfr[:, l:l + 1])
                wt_, bwt_ = wp.get()
                TS(p, "dve", wt_[:, 0:w], a[:, 0:w], -PI, 2 * PI, ALU.is_lt, ALU.mult, [ba], [bwt_])
                TT_(p, "dve", a[:, 0:w], a[:, 0:w], wt_[:, 0:w], ALU.add, [ba, bwt_], [ba])
                wt_, bwt_ = wp.get()
                TS(p, "dve", wt_[:, 0:w], a[:, 0:w], PI, -2 * PI, ALU.is_gt, ALU.mult, [ba], [bwt_])
                TT_(p, "dve", a[:, 0:w], a[:, 0:w], wt_[:, 0:w], ALU.add, [ba, bwt_], [ba])
                if l < 2:
                    h, bh = hp.get()
                else:
                    h, bh = op_.get()
                ACT(p, h[:, 0:w], a[:, 0:w], AF.Sin, [ba], [bh])
                cur, bcur = h, bh
            p.dma("sp", hid[:, c0:c0 + w], cur[:, 0:w], reads=[bcur])
        p.emit()
    return nc


def hyena_pos_tables(L):
    N = 32768
    t = np.linspace(0.0, 1.0, L, dtype=np.float32)[:, None]
    w = (2.0 * np.float32(np.pi) * np.arange(L, dtype=np.float32)[:, None] / np.float32(L)).astype(np.float32)
    bands = np.linspace(1e-4, 15, 16, dtype=np.float32)[None]
    z = np.concatenate([t, np.cos(w * bands), -np.sin(w * bands)], axis=-1).astype(np.float32)
    maxd = np.log(1e-2) / 0.3
    mind = np.log(1e-2) / 1.5
    deltas = np.linspace(mind, maxd, 1024, dtype=np.float32)
    decay = np.exp(-t * np.abs(deltas)).astype(np.float32)
    zext = np.zeros((N, 33), np.float32)
    dext = np.zeros((N, 1024), np.float32)
    zext[:L] = z
    dext[:L] = decay
    j = np.arange(1, L)
    zext[N - j] = z[j]
    dext[N - j] = decay[j]
    return zext, dext


def perm_pos(a):
    F_ = a.shape[1]
    return np.ascontiguousarray(a.reshape(256, 128, F_).transpose(2, 1, 0))


def run_LF(inp, zext):
    P = 32768 // NCORE
    nc = build_LF(P)
    zp = perm_pos(zext).reshape(33, 32768)
    w23 = np.ascontiguousarray(np.stack([inp["hy_f_w2"][0], inp["hy_f_w3"][0]], axis=1))
    bf = np.ascontiguousarray(np.stack([inp["hy_f_b1"][0], inp["hy_f_b2"][0], inp["hy_f_b3"][0], inp["hy_f_freq"][0]], axis=1))
    maps = [{"zT": np.ascontiguousarray(zp[:, c * P:(c + 1) * P]), "w1": inp["hy_f_w1"][0], "w23": w23, "bf": bf}
            for c in range(NCORE)]
    res = run(nc, maps)
    hid = np.concatenate([np.asarray(r["hid"]) for r in res], axis=1)
    return hid.reshape(64, 128, 256)


def build_HA(segs):
    Th = sum(n + 2 for _, n in segs)
    T = sum(n for _, n in segs)
    nc = bass.Bass("TRN2", target_bir_lowering=False)
    hT = nc.dram_tensor("hT", [128, 8, Th], BF16, kind="ExternalInput").ap()
    valid = nc.dram_tensor("valid", [1, Th], BF16, kind="ExternalInput").ap()
    win = nc.dram_tensor("win", [24, 128, 8, 128], F32, kind="ExternalInput").ap()
    cw = nc.dram_tensor("cw", [24, 128, 3, 128], F32, kind="ExternalInput").ap()
    brow = nc.dram_tensor("brow", [1, 4, 3072], F32, kind="ExternalInput").ap()
    cb = nc.dram_tensor("cb", [128, 24], F32, kind="ExternalInput").ap()
    uT = nc.dram_tensor("uT", [128, 24, T], F32, kind="ExternalOutput").ap()
    tiles = []
    o0 = 0
    for hb, n in segs:
        for (c0, w) in col_tiles(n):
            tiles.append((hb + c0, o0 + c0, w))
        o0 += n
    with ExitStack() as es:
        p = Prog(nc, es)
        H, bH = LOAD(p, "sp", [128, 8, Th], BF16, "H", hT)
        V, bV = LOAD(p, "sp", [1, Th], BF16, "V", valid)
        BR, bBR = LOAD(p, "sp", [1, 4, 3072], F32, "BR", brow)
        CB, bCB = LOAD(p, "sp", [128, 24], F32, "CB", cb)
        bk = p.sb("bk", [1, 3, 3072], BF16); bbk = p.buf()
        for k in range(3):
            TT_(p, "dve", bk[:, k, :], BR[:, 0, :], BR[:, 1 + k, :], ALU.mult, [bBR], [bbk])
        wp = Pool_(p, "w", [128, 8, 128], F32, 2)
        cp = Pool_(p, "c", [128, 3, 128], F32, 2)
        wkp = Pool_(p, "wk", [128, 3, 8, 128], BF16, 2)
        psp = Pool_(p, "ps", [128, 512], F32, 4, psum=True)
        op_ = Pool_(p, "o", [128, T], F32, 3)
        for m in range(24):
            wt, bw = wp.get()
            p.dma("sp", wt[:], win[m], writes=[bw])
            ct, bc = cp.get()
            p.dma("sp", ct[:], cw[m], writes=[bc])
            wk, bwk = wkp.get()
            for k in range(3):
                TT_(p, "dve" if k < 2 else "pool", wk[:, k, :, :], wt[:],
                    ct[:, k, :].unsqueeze(1).to_broadcast([128, 8, 128]), ALU.mult, [bw, bc], [bwk])
            ot, bo = op_.get()
            for (hb, ob, w) in tiles:
                ps, bps = psp.get()
                pairs = []
                for k in range(3):
                    for kc in range(8):
                        pairs.append((wk[:, k, kc, :], H[:, kc, hb + k:hb + k + w]))
                    pairs.append((bk[0:1, k, m * 128:(m + 1) * 128], V[0:1, hb + k:hb + k + w]))
                MM(p, ps[:, 0:w], pairs, [bwk, bH, bV, bbk], [bps])
                ACT(p, ot[:, ob:ob + w], ps[:, 0:w], AF.Identity, [bps, bCB], [bo], bias=CB[:, m:m + 1], scale=1.0)
            p.dma("sp", uT[:, m, :], ot[:], reads=[bo])
        p.emit()
    return nc


def halo_slabs(hx, hc):
    import ml_dtypes
    out = []
    for c in range(NCORE):
        parts, val = [], []
        for a, n in ((hx, TX), (hc, TC)):
            if a is None:
                continue
            L = a.shape[0]
            seg = np.zeros((n + 2, 1024), a.dtype)
            v = np.zeros((n + 2,), np.float32)
            lo, hi = c * n - 1, (c + 1) * n + 1
            slo, shi = max(lo, 0), min(hi, L)
            seg[slo - lo:shi - lo] = a[slo:shi]
            v[slo - lo:shi - lo] = 1.0
            parts.append(seg)
            val.append(v)
        out.append((fm(np.concatenate(parts, 0)), np.concatenate(val)[None].astype(ml_dtypes.bfloat16)))
    return out


def run_HA(inp, hx, hc):
    segs = [(0, TX)] + ([(TX + 2, TC)] if hc is not None else [])
    nc = build_HA(segs)
    W = inp["hy_w_in"][0]
    win = np.ascontiguousarray(W.reshape(8, 128, 24, 128).transpose(2, 1, 0, 3))
    cwv = inp["hy_conv_w"][0]
    cw = np.ascontiguousarray(np.broadcast_to(cwv.reshape(3, 24, 128).transpose(1, 0, 2)[:, None], (24, 128, 3, 128)))
    brow = np.ascontiguousarray(np.concatenate([inp["hy_b_in"][0][None], cwv], 0)[None])
    cb = vec_fm(inp["hy_conv_b"][0], 24)
    sl = halo_slabs(hx, hc)
    maps = [{"hT": sl[c][0], "valid": sl[c][1], "win": win, "cw": cw, "brow": brow, "cb": cb} for c in range(NCORE)]
    res = run(nc, maps)
    us, ucs = [], []
    for r in res:
        a = np.asarray(r["uT"]).transpose(1, 0, 2).reshape(3072, -1).T
        us.append(a[:TX])
        ucs.append(a[TX:])
    return np.concatenate(us, 0), (np.concatenate(ucs, 0) if hc is not None else None)


NG, GC = 8, 16


def hc_tables():
    import ml_dtypes
    bf = ml_dtypes.bfloat16
    N = 32768
    n1 = np.arange(128)[:, None].astype(np.float64)
    k1 = np.arange(256)[None].astype(np.float64)
    F1 = np.zeros((128, 2, 512), np.float64)
    for h in range(2):
        a = 2 * np.pi * (n1 + 128 * h) * k1 / 256
        F1[:, h, :256] = np.cos(a)
        F1[:, h, 256:] = -np.sin(a)
    phi = 2 * np.pi * np.arange(128)[:, None] * np.arange(256)[None] / N
    TW = np.stack([np.cos(phi), np.sin(phi)], 1)
    th = 2 * np.pi * np.arange(128)[:, None] * np.arange(128)[None] / 128
    F2 = np.stack([np.cos(th), np.sin(th), -np.sin(th)], 1)
    M3 = np.zeros((128, 2, 256), np.float64)
    M3[:, 0, :128] = np.cos(th); M3[:, 0, 128:] = np.sin(th)
    M3[:, 1, :128] = -np.sin(th); M3[:, 1, 128:] = np.cos(th)
    TWI = np.zeros((128, 2, 2, 128), np.float64)
    S4 = np.zeros((128, 2, 2, 128), np.float64)
    kp = np.arange(128)[:, None]
    for half in range(2):
        ph = 2 * np.pi * np.arange(128)[None] * (half * 128 + kp) / N
        TWI[:, half, 0] = np.cos(ph); TWI[:, half, 1] = np.sin(ph)
        ps_ = 2 * np.pi * np.arange(128)[None] * (half * 128 + kp) / 256
        S4[:, half, 0] = np.cos(ps_) / N; S4[:, half, 1] = -np.sin(ps_) / N
    return {"F1": F1.astype(bf), "TW": TW.astype(np.float32), "F2": F2.astype(bf), "M3": M3.astype(bf),
            "TWI": TWI.astype(np.float32), "S4": S4.astype(bf), "ones32": np.ones((128, 128), np.float32)}


HC_STOP = [0]


class _Stop(Exception):
    pass


def _chk(n):
    if HC_STOP[0] == n:
        raise _Stop()


def build_HC():
    nc = bass.Bass("TRN2", target_bir_lowering=False)
    dU = nc.dram_tensor("U", [NG, 128, 3, GC, 128], F32, kind="ExternalInput").ap()
    dhid = nc.dram_tensor("hid", [64, 128, 256], BF16, kind="ExternalInput").ap()
    dw4 = nc.dram_tensor("w4", [NG, 64, 2, 2 * GC], F32, kind="ExternalInput").ap()
    ddec = nc.dram_tensor("dec", [NG, 128, 2, 128, GC], F32, kind="ExternalInput").ap()
    dskip = nc.dram_tensor("skip", [128, NG, 2, GC], F32, kind="ExternalInput").ap()
    dF1 = nc.dram_tensor("F1", [128, 2, 512], BF16, kind="ExternalInput").ap()
    dTW = nc.dram_tensor("TW", [128, 2, 256], F32, kind="ExternalInput").ap()
    dF2 = nc.dram_tensor("F2", [128, 3, 128], BF16, kind="ExternalInput").ap()
    dM3 = nc.dram_tensor("M3", [128, 2, 256], BF16, kind="ExternalInput").ap()
    dTWI = nc.dram_tensor("TWI", [128, 2, 2, 128], F32, kind="ExternalInput").ap()
    dS4 = nc.dram_tensor("S4", [128, 2, 2, 128], BF16, kind="ExternalInput").ap()
    dones = nc.dram_tensor("ones32", [128, 128], F32, kind="ExternalInput").ap()
    dout = nc.dram_tensor("z2", [NG, 128, GC, 128], BF16, kind="ExternalOutput").ap()
    with ExitStack() as es:
        p = Prog(nc, es)
        F1, bF1 = LOAD(p, "sp", [128, 2, 512], BF16, "F1", dF1)
        TW, bTW = LOAD(p, "sp", [128, 2, 256], F32, "TW", dTW)
        F2, bF2 = LOAD(p, "sp", [128, 3, 128], BF16, "F2", dF2)
        M3, bM3 = LOAD(p, "sp", [128, 2, 256], BF16, "M3", dM3)
        TWI, bTWI = LOAD(p, "sp", [128, 2, 2, 128], F32, "TWI", dTWI)
        S4, bS4 = LOAD(p, "sp", [128, 2, 2, 128], BF16, "S4", dS4)
        ON, bON = LOAD(p, "sp", [128, 128], F32, "ON", dones)
        SK, bSK = LOAD(p, "sp", [128, NG, 2, GC], F32, "SK", dskip)
        cst = [bF1, bTW, bF2, bM3, bTWI, bS4]
        Up = Pool_(p, "U", [128, 3, GC, 128], F32, 1)
        Dp = Pool_(p, "D", [128, 2, 128, GC], F32, 1)
        W4p = Pool_(p, "W4", [64, 2, 2 * GC], BF16, 2)
        Hp = Pool_(p, "Hd", [64, 16, 256], BF16, 2)
        Kt = p.sb("Kt", [128, 2, GC, 2, 128], BF16); bKt = p.buf()
        Kr = p.sb("Kr", [128, 2, 128, 2, GC], BF16); bKr = p.buf()
        A = p.sb("A", [128, GC, 2, 256], BF16); bA = [p.buf() for _ in range(GC // 2)]
        Kf = p.sb("Kf", [128, GC, 2, 256], BF16); bKf = [p.buf() for _ in range(GC // 2)]
        G = p.sb("G", [128, GC, 2, 256], BF16); bG = [p.buf() for _ in range(GC // 2)]
        Bp = p.sb("Bp", [128, 2, 2, GC, 128], BF16); bBp = [p.buf() for _ in range(GC // 4)]
        Zb = p.sb("Zb", [128, GC, 128], BF16); bZb = [p.buf() for _ in range(GC // 4)]
        Z1 = p.sb("Z1", [128, GC, 128], F32); bZ1 = [p.buf() for _ in range(GC // 4)]
        Op = Pool_(p, "O", [128, GC, 128], BF16, 2)
        red = p.sb("red", [128, 2 * GC], F32); bred = p.buf()
        sN = p.sb("sN", [128, 2 * GC], F32); bsN = p.buf()
        psSp = Pool_(p, "psS", [128, 512], F32, 2, psum=True)
        ps1 = Pool_(p, "ps1", [128, 512], F32, 2, psum=True)
        psY = Pool_(p, "psY", [128, 512], F32, 3, psum=True)
        ps4 = Pool_(p, "ps4", [128, 512], F32, 1, psum=True)
        T1p = Pool_(p, "T1", [128, 2, 256], F32, 2)
        T2p = Pool_(p, "T2", [128, 2, 256], F32, 2)
        E1p = Pool_(p, "E1", [128, 4, 128], F32, 2)
        E2p = Pool_(p, "E2", [128, 4, 128], F32, 2)

        def fwd_twiddle(ps, bps, c, bdst):
            t1, b1 = T1p.get()
            t2, b2 = T2p.get()
            pv = ps[:, :].rearrange("p (r k) -> p r k", r=2)
            TT_(p, "dve", t1[:], pv, TW[:, 0, :].unsqueeze(1).to_broadcast([128, 2, 256]), ALU.mult, [bps, bTW], [b1])
            TT_(p, "dve", t2[:], pv, TW[:, 1, :].unsqueeze(1).to_broadcast([128, 2, 256]), ALU.mult, [bps, bTW], [b2])
            TT_(p, "pool", A[:, c, 0, :], t1[:, 0, :], t2[:, 1, :], ALU.add, [b1, b2], [bdst])
            TT_(p, "pool", A[:, c, 1, :], t1[:, 1, :], t2[:, 0, :], ALU.subtract, [b1, b2], [bdst])

        def stage2(pr):
            c0 = 2 * pr
            yr, byr = psY.get()
            yi, byi = psY.get()
            ar = A[:, c0:c0 + 2, 0, :]
            ai = A[:, c0:c0 + 2, 1, :]
            MM(p, yr[:, :], [(F2[:, 0, :], ar), (F2[:, 1, :], ai)], [bF2, bA[pr]], [byr])
            MM(p, yi[:, :], [(F2[:, 0, :], ai), (F2[:, 2, :], ar)], [bF2, bA[pr]], [byi])
            return yr, byr, yi, byi

        for g in range(NG):
          try:
            Ug, bU = Up.get()
            p.dma("sp", Ug[:], dU[g], writes=[bU])
            Dg, bD = Dp.get()
            p.dma("sp", Dg[:], ddec[g], writes=[bD])
            W4, bW4 = W4p.get()
            p.dma("pool", W4[:], dw4[g], writes=[bW4])
            _chk(10)
            for nb in range(8):
                Hd, bHd = Hp.get()
                p.dma("sp", Hd[:], dhid[:, nb * 16:(nb + 1) * 16, :], writes=[bHd])
                for jb in range(2):
                    bank, bbank = psSp.get()
                    fns = []
                    for jj in range(8):
                        j = jb * 8 + jj
                        for h in range(2):
                            k = jj * 2 + h
                            fns.append(lambda e, bank=bank, k=k, j=j, h=h, Hd=Hd, W4=W4: e.matmul(
                                bank[:, k * 32:(k + 1) * 32], Hd[:, j, h * 128:(h + 1) * 128], W4[:, h, :], start=True, stop=True))
                    p.group("pe", fns, [bHd, bW4], [bbank])
                    for jj in range(8):
                        j = jb * 8 + jj
                        n2 = nb * 16 + j
                        for h in range(2):
                            k = jj * 2 + h
                            TT_(p, "dve", Kr[:, h, n2, :, :], bank[:, k * 32:(k + 1) * 32].rearrange("p (o c) -> p o c", o=2),
                                Dg[:, h, n2, :].unsqueeze(1).to_broadcast([128, 2, GC]), ALU.mult, [bbank, bD], [bKr])
            for o_ in range(2):
                for c_ in range(GC):
                    CP(p, "act", Kt[:, o_, c_, :, :], Kr[:, :, :, o_, c_], [bKr], [bKt])
            _chk(1)
            p.op("dve", lambda e: e.tensor_reduce(out=red[:], in_=Kt[:].rearrange("p o c h n -> p (o c) (h n)"),
                                                   axis=AX.X, op=ALU.add, apply_absolute_value=True), [bKt], [bred])
            bank, bbank = psSp.get()
            sl = bank[:, 0:32]
            MM(p, sl, [(ON[:], red[:])], [bON, bred], [bbank])
            TS(p, "dve", sN[:], sl, 1e-6, None, ALU.add, None, [bbank], [bsN])
            p.op("dve", lambda e: e.reciprocal(out=sN[:], in_=sN[:]), [bsN], [bsN])
            _chk(2)
            Og, bO = Op.get()
            for o in range(2):
                for c in range(GC):
                    ps, bps = ps1.get()
                    MM(p, ps[:, :], [(Kt[:, o, c, 0, :], F1[:, 0, :]), (Kt[:, o, c, 1, :], F1[:, 1, :])], [bKt, bF1], [bps])
                    fwd_twiddle(ps, bps, c, bA[c // 2])
                for pr in range(GC // 2):
                    yr, byr, yi, byi = stage2(pr)
                    CP(p, "act", Kf[:, 2 * pr:2 * pr + 2, 0, :], yr[:, :].rearrange("p (c k) -> p c k", c=2), [byr], [bKf[pr]])
                    CP(p, "act", Kf[:, 2 * pr:2 * pr + 2, 1, :], yi[:, :].rearrange("p (c k) -> p c k", c=2), [byi], [bKf[pr]])
                _chk(3)
                if o == 0:
                    for q in range(GC // 4):
                        CP(p, "act", Zb[:, 4 * q:4 * q + 4, :], Ug[:, 0, 4 * q:4 * q + 4, :], [bU], [bZb[q]])
                for c in range(GC):
                    ps, bps = ps1.get()
                    MM(p, ps[:, :], [(Zb[:, c, :], F1[:, 0, :])], [bZb[c // 4], bF1], [bps])
                    fwd_twiddle(ps, bps, c, bA[c // 2])
                for pr in range(GC // 2):
                    c0 = 2 * pr
                    yr, byr, yi, byi = stage2(pr)
                    yrv = yr[:, :].rearrange("p (c k) -> p c k", c=2)
                    yiv = yi[:, :].rearrange("p (c k) -> p c k", c=2)
                    kr = Kf[:, c0:c0 + 2, 0, :]
                    ki = Kf[:, c0:c0 + 2, 1, :]
                    t1, b1 = T1p.get()
                    t2, b2 = T2p.get()
                    TT_(p, "dve", t1[:], yrv, kr, ALU.mult, [byr, bKf[pr]], [b1])
                    TT_(p, "dve", t2[:], yiv, ki, ALU.mult, [byi, bKf[pr]], [b2])
                    TT_(p, "pool", G[:, c0:c0 + 2, 0, :], t1[:], t2[:], ALU.subtract, [b1, b2], [bG[pr]])
                    t1, b1 = T1p.get()
                    t2, b2 = T2p.get()
                    TT_(p, "dve", t1[:], yrv, ki, ALU.mult, [byr, bKf[pr]], [b1])
                    TT_(p, "dve", t2[:], yiv, kr, ALU.mult, [byi, bKf[pr]], [b2])
                    TT_(p, "pool", G[:, c0:c0 + 2, 1, :], t1[:], t2[:], ALU.add, [b1, b2], [bG[pr]])
                _chk(4)
                for c in range(GC):
                    for half in range(2):
                        ps, bps = ps1.get()
                        MM(p, ps[:, 0:256], [(G[:, c, 0, half * 128:(half + 1) * 128], M3[:, 0, :]),
                                             (G[:, c, 1, half * 128:(half + 1) * 128], M3[:, 1, :])], [bG[c // 2], bM3], [bps])
                        t1, b1 = T1p.get()
                        t2, b2 = T2p.get()
                        pv = ps[:, 0:256].rearrange("p (r k) -> p r k", r=2)
                        TT_(p, "dve", t1[:, :, 0:128], pv, TWI[:, half, 0, :].unsqueeze(1).to_broadcast([128, 2, 128]), ALU.mult,
                            [bps, bTWI], [b1])
                        TT_(p, "dve", t2[:, :, 0:128], pv, TWI[:, half, 1, :].unsqueeze(1).to_broadcast([128, 2, 128]), ALU.mult,
                            [bps, bTWI], [b2])
                        TT_(p, "pool", Bp[:, half, 0, c, :], t1[:, 0, 0:128], t2[:, 1, 0:128], ALU.subtract, [b1, b2], [bBp[c // 4]])
                        TT_(p, "pool", Bp[:, half, 1, c, :], t2[:, 0, 0:128], t1[:, 1, 0:128], ALU.add, [b1, b2], [bBp[c // 4]])
                _chk(5)
                for q in range(GC // 4):
                    c0 = 4 * q
                    ps, bps = ps4.get()
                    MM(p, ps[:, :], [(S4[:, half, ri, :], Bp[:, half, ri, c0:c0 + 4, :]) for half in range(2) for ri in range(2)],
                       [bS4, bBp[q]], [bps])
                    e1, be1 = E1p.get()
                    e2, be2 = E2p.get()
                    pv = ps[:, :].rearrange("p (c n) -> p c n", c=4)
                    TT_(p, "dve", e1[:], pv, sN[:, o * GC + c0:o * GC + c0 + 4].unsqueeze(2).to_broadcast([128, 4, 128]), ALU.mult,
                        [bps, bsN], [be1])
                    if o == 0:
                        zc, bzc = Ug[:, 0, c0:c0 + 4, :], bU
                    else:
                        zc, bzc = Z1[:, c0:c0 + 4, :], bZ1[q]
                    TT_(p, "pool", e2[:], zc, SK[:, g, o, c0:c0 + 4].unsqueeze(2).to_broadcast([128, 4, 128]), ALU.mult,
                        [bzc, bSK], [be2])
                    TT_(p, "pool", e2[:], e2[:], e1[:], ALU.add, [be1, be2], [be2])
                    if o == 0:
                        TT_(p, "dve", Z1[:, c0:c0 + 4, :], e2[:], Ug[:, 1, c0:c0 + 4, :], ALU.mult, [be2, bU], [bZ1[q]])
                        CP(p, "act", Zb[:, c0:c0 + 4, :], Z1[:, c0:c0 + 4, :], [bZ1[q]], [bZb[q]])
                    else:
                        TT_(p, "dve", Og[:, c0:c0 + 4, :], e2[:], Ug[:, 2, c0:c0 + 4, :], ALU.mult, [be2, bU], [bO])
            p.dma("sp", dout[g], Og[:], reads=[bO])
          except _Stop:
            break
        p.emit()
    return nc


def run_HC(inp, u, hid, dext, tabs, nc=None):
    if nc is None:
        nc = build_HC()
    L = u.shape[0]
    up = np.zeros((16384, 3072), np.float32)
    up[:L] = u
    u4 = up.reshape(128, 128, 3, 1024)
    d4 = dext.reshape(2, 128, 128, 1024).transpose(1, 0, 2, 3)
    w4 = inp["hy_f_w4"][0].reshape(64, 2, 2, 1024)
    sk = inp["hy_skip"][0]
    maps = []
    for c in range(NCORE):
        ch = slice(c * 128, (c + 1) * 128)
        U = np.ascontiguousarray(u4[:, :, :, ch].reshape(128, 128, 3, NG, GC).transpose(3, 0, 2, 4, 1))
        D = np.ascontiguousarray(d4[:, :, :, ch].reshape(128, 2, 128, NG, GC).transpose(3, 0, 1, 2, 4))
        W = np.ascontiguousarray(w4[:, :, :, ch].reshape(64, 2, 2, NG, GC).transpose(3, 0, 2, 1, 4).reshape(NG, 64, 2, 2 * GC))
        S = np.ascontiguousarray(np.broadcast_to(sk[:, ch].reshape(2, NG, GC).transpose(1, 0, 2)[None], (128, NG, 2, GC)))
        m = {"U": U, "hid": hid, "w4": W, "dec": D, "skip": S}
        m.update(tabs)
        maps.append(m)
    res = run(nc, maps)
    zs = []
    for r in res:
        a = np.asarray(r["z2"])
        zs.append(a.transpose(1, 3, 0, 2).reshape(16384, 128))
    return np.concatenate(zs, axis=1)[:L]


def MMx(p, out, lhsT, rhs, start, stop, reads, writes):
    return p.group("pe", [lambda e: e.matmul(out, lhsT, rhs, start=start, stop=stop)], reads, writes)


def rms_mod_tile(p, C, xs, bx, w, gm, bgm, sh, bsh, obf, bobf, of=None, bof=None, eps=1e-6, D=1024):
    sq, bsq = C.sq.get()
    for kc in range(8):
        ACT(p, sq[:, kc, 0:w], xs(kc), AF.Square, [bx], [bsq])
    ps, bps = C.ps.get()
    MM(p, ps[:, 0:w], [(C.ones[:], sq[:, kc, 0:w]) for kc in range(8)], [bsq, C.bones], [bps])
    rs, brs = C.rs.get()
    ACT(p, rs[:, 0:w], ps[:, 0:w], AF.Sqrt, [bps], [brs], scale=1.0 / D, bias=eps)
    p.op("dve", lambda e: e.reciprocal(out=rs[:, 0:w], in_=rs[:, 0:w]), [brs], [brs])
    for kc in range(8):
        tmp, btmp = C.tmp.get()
        STT(p, tmp[:, 0:w], xs(kc), gm[:, kc:kc + 1], rs[:, 0:w], ALU.mult, ALU.mult, [bx, brs, bgm], [btmp])
        ACT(p, obf(kc), tmp[:, 0:w], AF.Identity, [btmp, bsh], [bobf], bias=sh[:, kc:kc + 1], scale=1.0)
        if of is not None:
            ACT(p, of(kc), tmp[:, 0:w], AF.Identity, [btmp, bsh], [bof], bias=sh[:, kc:kc + 1], scale=1.0)


def build_L3(T, has_bias):
    nc = bass.Bass("TRN2", target_bir_lowering=False)
    dz = nc.dram_tensor("zT", [128, 8, T], BF16, kind="ExternalInput").ap()
    dx = nc.dram_tensor("xT", [128, 8, T], F32, kind="ExternalInput").ap()
    dw = nc.dram_tensor("wout", [1024, 1024], F32, kind="ExternalInput").ap()
    db = nc.dram_tensor("bout", [128, 8], F32, kind="ExternalInput").ap()
    dmd = nc.dram_tensor("md", [128, 2, 6, 8], F32, kind="ExternalInput").ap()
    dg = nc.dram_tensor("g", [128, 8], F32, kind="ExternalInput").ap()
    dwr = nc.dram_tensor("wr", [128, 8, 16], F32, kind="ExternalInput").ap()
    dones = nc.dram_tensor("ones", [128, 128], F32, kind="ExternalInput").ap()
    ox = nc.dram_tensor("x1T", [128, 8, T], F32, kind="ExternalOutput").ap()
    oh = nc.dram_tensor("h2T", [128, 8, T], BF16, kind="ExternalOutput").ap()
    oa = nc.dram_tensor("aff", [T, 16], F32, kind="ExternalOutput").ap()
    tiles = tiles_of(T)
    with ExitStack() as es:
        p = Prog(nc, es)
        C = make_common(p, nc, dones)
        mdt, bmd = LOAD(p, "sp", [128, 2, 6, 8], F32, "md", dmd)
        gt, bg = LOAD(p, "sp", [128, 8], F32, "g", dg)
        bt, bb = LOAD(p, "sp", [128, 8], F32, "bo", db)
        wr, bwr = LOAD(p, "sp", [128, 8, 16], F32, "wr", dwr)
        W = p.sb("W", [128, 8, 1024], BF16); bW = p.buf()
        for kc in range(8):
            p.dma("pool", W[:, kc, :], dw[kc * 128:(kc + 1) * 128, :], writes=[bW])
        X = TT(p, "X", [128, 8, T], F32, len(tiles))
        Z = TT(p, "Z", [128, 8, T], BF16, len(tiles))
        for ti, (c0, w) in enumerate(tiles):
            p.dma("sp", Z.t[:, :, c0:c0 + w], dz[:, :, c0:c0 + w], writes=[Z.b[ti]])
            p.dma("sp", X.t[:, :, c0:c0 + w], dx[:, :, c0:c0 + w], writes=[X.b[ti]])
        gmx, bgmx = emit_gmod(p, "gmx", gt[:], bg, mdt[:, 0, 4, :], bmd)
        gmc, bgmc = emit_gmod(p, "gmc", gt[:], bg, mdt[:, 1, 4, :], bmd)
        Hb = Pool_(p, "Hb", [128, 8, 512], BF16, 2)
        Hf = Pool_(p, "Hf", [128, 8, 512], F32, 2)
        psL = Pool_(p, "psL", [128, 512], F32, 2, psum=True)
        sm = Pool_(p, "sm", [128, 40], F32, 3)
        for ti, (c0, w) in enumerate(tiles):
            cond = 1 if c0 >= TX else 0
            for m in range(8):
                ps, bps = C.ps.get()
                MM(p, ps[:, 0:w], [(W[:, kc, m * 128:(m + 1) * 128], Z.t[:, kc, c0:c0 + w]) for kc in range(8)], [bW, Z.b[ti]], [bps])
                if has_bias:
                    tmp, btmp = C.tmp.get()
                    ACT(p, tmp[:, 0:w], ps[:, 0:w], AF.Identity, [bps, bb], [btmp], bias=bt[:, m:m + 1], scale=1.0)
                    src, bsrc = tmp, btmp
                else:
                    src, bsrc = ps, bps
                STT(p, X.t[:, m, c0:c0 + w], src[:, 0:w], mdt[:, cond, 2, m:m + 1], X.t[:, m, c0:c0 + w], ALU.mult, ALU.add,
                    [bsrc, bmd, X.b[ti]], [X.b[ti]])
            p.dma("sp", ox[:, :, c0:c0 + w], X.t[:, :, c0:c0 + w], reads=[X.b[ti]])
            hb, bhb = Hb.get()
            hf, bhf = Hf.get()
            gm, bgm = (gmc, bgmc) if cond else (gmx, bgmx)
            rms_mod_tile(p, C, lambda kc: X.t[:, kc, c0:c0 + w], X.b[ti], w, gm, bgm, mdt[:, cond, 3, :], bmd,
                         lambda kc: hb[:, kc, 0:w], bhb, lambda kc: hf[:, kc, 0:w], bhf)
            p.dma("sp", oh[:, :, c0:c0 + w], hb[:, :, 0:w], reads=[bhb])
            for j0 in range(0, w, 128):
                tw = min(128, w - j0)
                pl, bpl = psL.get()
                MM(p, pl[0:tw, 0:16], [(hf[:, kc, j0:j0 + tw], wr[:, kc, :]) for kc in range(8)], [bhf, bwr], [bpl])
                s_, bs_ = sm.get()
                p.op("dve", lambda e, s_=s_, pl=pl, tw=tw: e.tensor_reduce(out=s_[0:tw, 32:33], in_=pl[0:tw, 0:16], axis=AX.X, op=ALU.max),
                     [bpl], [bs_])
                TS(p, "dve", s_[0:tw, 33:34], s_[0:tw, 32:33], -1.0, None, ALU.mult, None, [bs_], [bs_])
                ACT(p, s_[0:tw, 0:16], pl[0:tw, 0:16], AF.Exp, [bpl, bs_], [bs_], bias=s_[0:tw, 33:34], scale=1.0,
                    accum_out=s_[0:tw, 34:35])
                p.op("dve", lambda e, s_=s_, tw=tw: e.reciprocal(out=s_[0:tw, 35:36], in_=s_[0:tw, 34:35]), [bs_], [bs_])
                TS(p, "dve", s_[0:tw, 16:32], s_[0:tw, 0:16], s_[0:tw, 35:36], None, ALU.mult, None, [bs_], [bs_])
                p.dma("sp", oa[c0 + j0:c0 + j0 + tw, :], s_[0:tw, 16:32], reads=[bs_])
        p.emit()
    return nc


def run_L3(zsl, xsl, wout, bout, mods, l, g, wr, T, has_bias):
    nc = build_L3(T, has_bias)
    md = mods_fm(mods, l)
    wrl = np.ascontiguousarray(wr.reshape(8, 128, 16).transpose(1, 0, 2))
    bo = vec_fm(bout) if bout is not None else np.zeros((128, 8), np.float32)
    maps = [{"zT": zsl[c], "xT": xsl[c], "wout": wout, "bout": bo, "md": md, "g": vec_fm(g), "wr": wrl, "ones": ONES_F32}
            for c in range(NCORE)]
    res = run(nc, maps)
    return [r["x1T"] for r in res], [r["h2T"] for r in res], [np.asarray(r["aff"]) for r in res]


def emit_bisect(p, name, a, ba, n, cap, Gm, bGm, psb, iters=30):
    st = p.sb(name + "_st", [128, 8], F32); bst = p.buf()
    cmp_ = p.sb(name + "_cmp", [128, n], BF16); bcmp = p.buf()
    p.op("dve", lambda e: e.memset(st[:, 0:1], 0.0), [], [bst])
    p.op("dve", lambda e: e.memset(st[:, 1:2], 1.0), [bst], [bst])
    p.op("dve", lambda e: e.memset(st[:, 2:3], 0.5), [bst], [bst])
    p.op("dve", lambda e: e.memset(st[:, 3:5], 0.0), [bst], [bst])
    for it in range(iters):
        p.op("dve", lambda e: e.tensor_scalar(out=cmp_[:], in0=a, scalar1=st[:, 2:3], scalar2=0.0, op0=ALU.is_ge, op1=ALU.add,
                                               accum_out=st[:, 3:4]), [ba, bst], [bcmp, bst])
        ps, bps = psb
        MM(p, ps[:, 0:2], [(Gm, st[:, 3:5])], [bGm, bst], [bps])
        TS(p, "dve", st[:, 5:6], ps[:, 0:1], float(cap) - 0.5, None, ALU.is_ge, None, [bps, bst], [bst])
        TT_(p, "dve", st[:, 6:7], st[:, 2:3], st[:, 0:1], ALU.subtract, [bst], [bst])
        STT(p, st[:, 0:1], st[:, 6:7], st[:, 5:6], st[:, 0:1], ALU.mult, ALU.add, [bst], [bst])
        TT_(p, "dve", st[:, 6:7], st[:, 1:2], st[:, 2:3], ALU.subtract, [bst], [bst])
        STT(p, st[:, 1:2], st[:, 6:7], st[:, 5:6], st[:, 2:3], ALU.mult, ALU.add, [bst], [bst])
        TT_(p, "dve", st[:, 6:7], st[:, 0:1], st[:, 1:2], ALU.add, [bst], [bst])
        TS(p, "dve", st[:, 2:3], st[:, 6:7], 0.5, None, ALU.mult, None, [bst], [bst])
    return st[:, 0:1], bst


def moe_consts():
    Gm = np.zeros((128, 128), np.float32)
    for k in range(128):
        Gm[k, (k // 8) * 8:(k // 8) * 8 + 8] = 1.0
    Sel = np.zeros((128, 16, 128), np.float32)
    for e in range(16):
        Sel[e * 8, e, :] = 1.0
    return Gm, Sel


def build_L4(T, has_ctx):
    nc = bass.Bass("TRN2", target_bir_lowering=False)
    dx = nc.dram_tensor("xT", [128, 8, T], F32, kind="ExternalInput").ap()
    dh = nc.dram_tensor("hT", [128, 8, T], BF16, kind="ExternalInput").ap()
    daf = nc.dram_tensor("afull", [128, 2048], F32, kind="ExternalInput").ap()
    dac = nc.dram_tensor("acfull", [128, 32], F32, kind="ExternalInput").ap()
    dao = nc.dram_tensor("aown", [128, T], F32, kind="ExternalInput").ap()
    dmd = nc.dram_tensor("md", [128, 2, 6, 8], F32, kind="ExternalInput").ap()
    dGm = nc.dram_tensor("Gm", [128, 128], F32, kind="ExternalInput").ap()
    dSel = nc.dram_tensor("Sel", [128, 16, 128], F32, kind="ExternalInput").ap()
    dwg = nc.dram_tensor("wg", [16, 1024, 1024], F32, kind="ExternalInput").ap()
    dwu = nc.dram_tensor("wu", [16, 1024, 1024], F32, kind="ExternalInput").ap()
    dwd = nc.dram_tensor("wd", [16, 1024, 1024], F32, kind="ExternalInput").ap()
    ox = nc.dram_tensor("x2T", [128, 8, T], F32, kind="ExternalOutput").ap()
    tiles = tiles_of(T)
    with ExitStack() as es:
        p = Prog(nc, es)
        mdt, bmd = LOAD(p, "sp", [128, 2, 6, 8], F32, "md", dmd)
        Gm, bGm = LOAD(p, "sp", [128, 128], F32, "Gm", dGm)
        Sel, bSel = LOAD(p, "sp", [128, 16, 128], F32, "Sel", dSel)
        af, baf = LOAD(p, "sp", [128, 2048], F32, "af", daf)
        ao, bao = LOAD(p, "sp", [128, T], F32, "ao", dao)
        X = TT(p, "X", [128, 8, T], F32, len(tiles))
        H = TT(p, "H", [128, 8, T], BF16, len(tiles))
        for ti, (c0, w) in enumerate(tiles):
            p.dma("sp", H.t[:, :, c0:c0 + w], dh[:, :, c0:c0 + w], writes=[H.b[ti]])
            p.dma("sp", X.t[:, :, c0:c0 + w], dx[:, :, c0:c0 + w], writes=[X.b[ti]])
        psb = Pool_(p, "psb", [128, 512], F32, 1, psum=True)
        tau, btau = emit_bisect(p, "bx", af[:], baf, 2048, 2048, Gm[:], bGm, (psb.ts[0], psb.bs[0]))
        gw, bgw = ao, bao
        STT(p, gw[:, 0:TX], ao[:, 0:TX], tau, ao[:, 0:TX], ALU.is_ge, ALU.mult, [bao, btau], [bgw])
        if has_ctx:
            ac, bac = LOAD(p, "sp", [128, 32], F32, "ac", dac)
            tauc, btauc = emit_bisect(p, "bc", ac[:], bac, 32, 32, Gm[:], bGm, (psb.ts[0], psb.bs[0]))
            STT(p, gw[:, TX:T], ao[:, TX:T], tauc, ao[:, TX:T], ALU.is_ge, ALU.mult, [bao, btauc, bgw], [bgw])
        Wp = Pool_(p, "W", [128, 8, 1024], BF16, 4)
        Ap = Pool_(p, "A", [128, 8, 512], BF16, 1)
        sgp = Pool_(p, "sg", [128, 512], BF16, 2)
        atp = Pool_(p, "at", [128, 512], BF16, 2)
        gbp = Pool_(p, "gb", [128, 512], BF16, 2)
        psG = Pool_(p, "psG", [128, 512], F32, 4, psum=True)
        psD = Pool_(p, "psD", [128, 512], F32, 2, psum=True)
        psB = Pool_(p, "psB", [128, 512], F32, 1, psum=True)

        def loadw(src):
            wt, bw = Wp.get()
            for kc in range(8):
                p.dma("pool", wt[:, kc, :], src[kc * 128:(kc + 1) * 128, :], writes=[bw])
            return wt, bw

        for e in range(16):
            Wg, bWg = loadw(dwg[e])
            Wu, bWu = loadw(dwu[e])
            Wd, bWd = loadw(dwd[e])
            for ti, (c0, w) in enumerate(tiles):
                cond = 1 if c0 >= TX else 0
                pb, bpb = psB.get()
                MM(p, pb[:, 0:w], [(Sel[:, e, :], gw[:, c0:c0 + w])], [bSel, bgw], [bpb])
                gb, bgb = gbp.get()
                CP(p, "act", gb[:, 0:w], pb[:, 0:w], [bpb], [bgb])
                A_, bA = Ap.get()
                for fc in range(8):
                    pg, bpg = psG.get()
                    MM(p, pg[:, 0:w], [(Wg[:, kc, fc * 128:(fc + 1) * 128], H.t[:, kc, c0:c0 + w]) for kc in range(8)],
                       [bWg, H.b[ti]], [bpg])
                    sg, bsg = sgp.get()
                    ACT(p, sg[:, 0:w], pg[:, 0:w], AF.Silu, [bpg], [bsg])
                    pu, bpu = psG.get()
                    MM(p, pu[:, 0:w], [(Wu[:, kc, fc * 128:(fc + 1) * 128], H.t[:, kc, c0:c0 + w]) for kc in range(8)],
                       [bWu, H.b[ti]], [bpu])
                    at, bat = atp.get()
                    TT_(p, "dve", at[:, 0:w], pu[:, 0:w], sg[:, 0:w], ALU.mult, [bpu, bsg], [bat])
                    TT_(p, "dve", A_[:, fc, 0:w], at[:, 0:w], gb[:, 0:w], ALU.mult, [bat, bgb], [bA])
                for dc in range(8):
                    pd, bpd = psD.get()
                    MM(p, pd[:, 0:w], [(Wd[:, fc, dc * 128:(dc + 1) * 128], A_[:, fc, 0:w]) for fc in range(8)], [bWd, bA], [bpd])
                    STT(p, X.t[:, dc, c0:c0 + w], pd[:, 0:w], mdt[:, cond, 5, dc:dc + 1], X.t[:, dc, c0:c0 + w], ALU.mult, ALU.add,
                        [bpd, bmd, X.b[ti]], [X.b[ti]])
        for ti, (c0, w) in enumerate(tiles):
            p.dma("sp", ox[:, :, c0:c0 + w], X.t[:, :, c0:c0 + w], reads=[X.b[ti]])
        p.emit()
    return nc


def grp_layout(aT):
    n = aT.shape[1]
    return np.ascontiguousarray(aT.reshape(16, 8, n // 8).reshape(128, n // 8))


def run_L4(xsl, hsl, affs, mods, l, wg, wu, wd, T, has_ctx, nc=None):
    if nc is None:
        nc = build_L4(T, has_ctx)
    md = mods_fm(mods, l)
    Gm, Sel = moe_consts()
    ax = np.concatenate([a[:TX] for a in affs], 0)
    afull = grp_layout(np.ascontiguousarray(ax.T))
    if has_ctx:
        ac = np.concatenate([a[TX:] for a in affs], 0)
        acfull = grp_layout(np.ascontiguousarray(ac.T))
    else:
        acfull = np.zeros((128, 32), np.float32)
    maps = []
    for c in range(NCORE):
        aown = np.ascontiguousarray(np.repeat(affs[c].T, 8, axis=0))
        maps.append({"xT": xsl[c], "hT": hsl[c], "afull": afull, "acfull": acfull, "aown": aown, "md": md, "Gm": Gm, "Sel": Sel,
                     "wg": wg, "wu": wu, "wd": wd})
    res = run(nc, maps)
    return [r["x2T"] for r in res]


def rope_consts(core):
    t = core * TX + np.arange(TX)
    row = (t // 64).astype(np.float32)
    col = (t % 64).astype(np.float32)
    inv = (10000.0 ** (-np.arange(0, 32, 2, dtype=np.float32) / 32)).astype(np.float32)
    C = np.zeros((128, TX), np.float32)
    S = np.zeros((128, TX), np.float32)
    for p_ in range(128):
        d = p_ % 64
        pos = row if d < 32 else col
        i = (d % 32) % 16
        ang = pos * inv[i]
        C[p_] = np.cos(ang)
        S[p_] = np.sin(ang)
    PT = np.zeros((128, 128), np.float32)
    for m in range(128):
        if (m % 32) < 16:
            PT[m + 16, m] = -1.0
        else:
            PT[m - 16, m] = 1.0
    BD = np.zeros((128, 128), np.float32)
    BD[:64, :64] = 1.0
    BD[64:, 64:] = 1.0
    return C, S, PT, BD


def build_L5(T):
    nc = bass.Bass("TRN2", target_bir_lowering=False)
    dx = nc.dram_tensor("xT", [128, 8, T], F32, kind="ExternalInput").ap()
    dmd = nc.dram_tensor("md", [128, 2, 6, 8], F32, kind="ExternalInput").ap()
    dg = nc.dram_tensor("g", [128, 8], F32, kind="ExternalInput").ap()
    dw = nc.dram_tensor("wqkv", [1024, 3072], F32, kind="ExternalInput").ap()
    dqk = nc.dram_tensor("qkg", [128, 2], F32, kind="ExternalInput").ap()
    dC = nc.dram_tensor("ropeC", [128, TX], F32, kind="ExternalInput").ap()
    dS = nc.dram_tensor("ropeS", [128, TX], F32, kind="ExternalInput").ap()
    dPT = nc.dram_tensor("PT", [128, 128], F32, kind="ExternalInput").ap()
    dBD = nc.dram_tensor("BD", [128, 128], F32, kind="ExternalInput").ap()
    dones = nc.dram_tensor("ones", [128, 128], F32, kind="ExternalInput").ap()
    oq = nc.dram_tensor("qT", [128, 8, TX], BF16, kind="ExternalOutput").ap()
    ok = nc.dram_tensor("kT", [128, 8, T], BF16, kind="ExternalOutput").ap()
    ov = nc.dram_tensor("v", [T, 1024], BF16, kind="ExternalOutput").ap()
    tiles = tiles_of(T)
    with ExitStack() as es:
        p = Prog(nc, es)
        C = make_common(p, nc, dones)
        mdt, bmd = LOAD(p, "sp", [128, 2, 6, 8], F32, "md", dmd)
        gt, bg = LOAD(p, "sp", [128, 8], F32, "g", dg)
        qkg, bqkg = LOAD(p, "sp", [128, 2], F32, "qkg", dqk)
        RC, bRC = LOAD(p, "sp", [128, TX], F32, "RC", dC)
        RS, bRS = LOAD(p, "sp", [128, TX], F32, "RS", dS)
        PT, bPT = LOAD(p, "sp", [128, 128], F32, "PT", dPT)
        BD, bBD = LOAD(p, "pool", [128, 128], BF16, "BD", dBD)
        X = TT(p, "X", [128, 8, T], F32, len(tiles))
        H = TT(p, "H", [128, 8, T], BF16, len(tiles))
        for ti, (c0, w) in enumerate(tiles):
            p.dma("sp", X.t[:, :, c0:c0 + w], dx[:, :, c0:c0 + w], writes=[X.b[ti]])
        gmx, bgmx = emit_gmod(p, "gmx", gt[:], bg, mdt[:, 0, 1, :], bmd)
        gmc, bgmc = emit_gmod(p, "gmc", gt[:], bg, mdt[:, 1, 1, :], bmd)
        for ti, (c0, w) in enumerate(tiles):
            cond = 1 if c0 >= TX else 0
            gm, bgm = (gmc, bgmc) if cond else (gmx, bgmx)
            rms_mod_tile(p, C, lambda kc: X.t[:, kc, c0:c0 + w], X.b[ti], w, gm, bgm, mdt[:, cond, 0, :], bmd,
                         lambda kc: H.t[:, kc, c0:c0 + w], H.b[ti])
        Wm = Pool_(p, "Wm", [128, 8, 128], BF16, 3)
        qr = Pool_(p, "qr", [128, 512], F32, 2)
        sqp = Pool_(p, "sq1", [128, 512], BF16, 2)
        qn = Pool_(p, "qn", [128, 512], F32, 2)
        t1p = Pool_(p, "t1", [128, 512], F32, 2)
        t2p = Pool_(p, "t2", [128, 512], F32, 2)
        ob = Pool_(p, "ob", [128, 512], BF16, 3)
        ps2 = Pool_(p, "ps2", [128, 512], F32, 4, psum=True)
        for m in range(16):
            isq = m < 8
            wt, bw = Wm.get()
            p.dma("pool", wt[:], dw[:, m * 128:(m + 1) * 128].rearrange("(kc p) n -> p kc n", p=128), writes=[bw])
            gcol = qkg[:, 0:1] if isq else qkg[:, 1:2]
            for ti, (c0, w) in enumerate(tiles):
                isctx = c0 >= TX
                if isq and isctx:
                    continue
                ps, bps = ps2.get()
                MM(p, ps[:, 0:w], [(wt[:, kc, :], H.t[:, kc, c0:c0 + w]) for kc in range(8)], [bw, H.b[ti]], [bps])
                q_, bq_ = qr.get()
                CP(p, "act", q_[:, 0:w], ps[:, 0:w], [bps], [bq_])
                sq, bsq = sqp.get()
                ACT(p, sq[:, 0:w], ps[:, 0:w], AF.Square, [bps], [bsq])
                pss, bpss = ps2.get()
                MM(p, pss[:, 0:w], [(BD[:], sq[:, 0:w])], [bBD, bsq], [bpss])
                rs, brs = C.rs.get()
                ACT(p, rs[:, 0:w], pss[:, 0:w], AF.Sqrt, [bpss], [brs], scale=1.0 / 64, bias=1e-6)
                p.op("dve", lambda e, rs=rs, w=w: e.reciprocal(out=rs[:, 0:w], in_=rs[:, 0:w]), [brs], [brs])
                n_, bn_ = qn.get()
                STT(p, n_[:, 0:w], q_[:, 0:w], gcol, rs[:, 0:w], ALU.mult, ALU.mult, [bq_, bqkg, brs], [bn_])
                o_, bo_ = ob.get()
                if not isctx:
                    pr, bpr = ps2.get()
                    MM(p, pr[:, 0:w], [(PT[:], n_[:, 0:w])], [bPT, bn_], [bpr])
                    t1, b1 = t1p.get()
                    t2, b2 = t2p.get()
                    TT_(p, "pool", t1[:, 0:w], n_[:, 0:w], RC[:, c0:c0 + w], ALU.mult, [bn_, bRC], [b1])
                    TT_(p, "dve", t2[:, 0:w], pr[:, 0:w], RS[:, c0:c0 + w], ALU.mult, [bpr, bRS], [b2])
                    TT_(p, "pool", o_[:, 0:w], t1[:, 0:w], t2[:, 0:w], ALU.add, [b1, b2], [bo_])
                else:
                    CP(p, "act", o_[:, 0:w], n_[:, 0:w], [bn_], [bo_])
                if isq:
                    p.dma("sp", oq[:, m, c0:c0 + w], o_[:, 0:w], reads=[bo_])
                else:
                    p.dma("sp", ok[:, m - 8, c0:c0 + w], o_[:, 0:w], reads=[bo_])
        Wv = p.sb("Wv", [128, 8, 1024], BF16); bWv = p.buf()
        for kc in range(8):
            p.dma("pool", Wv[:, kc, :], dw[kc * 128:(kc + 1) * 128, 2048:3072], writes=[bWv])
        vb = Pool_(p, "vb", [128, 512], BF16, 3)
        for ti, (c0, w) in enumerate(tiles):
            for j0 in range(0, w, 128):
                tw = min(128, w - j0)
                for hh in range(2):
                    ps, bps = ps2.get()
                    MM(p, ps[0:tw, :], [(H.t[:, kc, c0 + j0:c0 + j0 + tw], Wv[:, kc, hh * 512:(hh + 1) * 512]) for kc in range(8)],
                       [H.b[ti], bWv], [bps])
                    v_, bv_ = vb.get()
                    CP(p, "act", v_[0:tw, :], ps[0:tw, :], [bps], [bv_])
                    p.dma("sp", ov[c0 + j0:c0 + j0 + tw, hh * 512:(hh + 1) * 512], v_[0:tw, :], reads=[bv_])
        p.emit()
    return nc


def run_L5(inp, xsl, mods, T):
    nc = build_L5(T)
    md = mods_fm(mods, 1)
    qkg = np.ascontiguousarray(np.stack([np.tile(inp["da_q_norm"][0], 2), np.tile(inp["da_k_norm"][0], 2)], axis=1))
    maps = []
    for c in range(NCORE):
        C_, S_, PT, BD = rope_consts(c)
        maps.append({"xT": xsl[c], "md": md, "g": vec_fm(inp["norm_mix"][1]), "wqkv": inp["da_w_qkv"][0], "qkg": qkg,
                     "ropeC": C_, "ropeS": S_, "PT": PT, "BD": BD, "ones": ONES_F32})
    res = run(nc, maps)
    return [np.asarray(r["qT"]) for r in res], [np.asarray(r["kT"]) for r in res], [np.asarray(r["v"]) for r in res]


LAM_INIT = 0.8 - 0.6 * float(np.exp(-0.3 * 1))
NKEY = 16384 + 256
NKC = NKEY // 128


def build_L6():
    nc = bass.Bass("TRN2", target_bir_lowering=False)
    dq = nc.dram_tensor("qT", [128, 8, TX], BF16, kind="ExternalInput").ap()
    dk = nc.dram_tensor("kT", [8, 128, NKEY], BF16, kind="ExternalInput").ap()
    dv = nc.dram_tensor("v", [8, 128, NKC, 128], BF16, kind="ExternalInput").ap()
    dlam = nc.dram_tensor("lamv", [128, 4, 64], F32, kind="ExternalInput").ap()
    dgn = nc.dram_tensor("gains", [128, 2, 64], F32, kind="ExternalInput").ap()
    dsub = nc.dram_tensor("subln", [128, 1], F32, kind="ExternalInput").ap()
    dones = nc.dram_tensor("ones", [128, 128], F32, kind="ExternalInput").ap()
    oo_ = nc.dram_tensor("oT", [128, 8, TX], BF16, kind="ExternalOutput").ap()
    with ExitStack() as es:
        p = Prog(nc, es)
        ones, bones = LOAD(p, "pool", [128, 128], BF16, "ones", dones)
        lamv, blamv = LOAD(p, "sp", [128, 4, 64], F32, "lamv", dlam)
        gn, bgn = LOAD(p, "sp", [128, 2, 64], F32, "gn", dgn)
        st = p.sb("st", [128, 16], F32); bst = p.buf()
        p.dma("sp", st[:, 0:1], dsub, writes=[bst])
        sc = p.sb("scr", [128, 64], F32); bsc = p.buf()
        for i in range(2):
            TT_(p, "dve", sc[:], lamv[:, 2 * i, :], lamv[:, 2 * i + 1, :], ALU.mult, [blamv, bsc], [bsc])
            p.op("dve", lambda e, i=i: e.tensor_reduce(out=st[:, 1 + i:2 + i], in_=sc[:], axis=AX.X, op=ALU.add), [bsc, bst], [bst])
        ACT(p, st[:, 1:3], st[:, 1:3], AF.Exp, [bst], [bst])
        TT_(p, "dve", st[:, 3:4], st[:, 2:3], st[:, 1:2], ALU.subtract, [bst], [bst])
        TS(p, "dve", st[:, 3:4], st[:, 3:4], -LAM_INIT, None, ALU.add, None, [bst], [bst])
        for i in range(2):
            p.op("dve", lambda e, i=i: e.tensor_reduce(out=st[:, 4 + i:5 + i], in_=gn[:, i, :], axis=AX.X, op=ALU.max,
                                                       apply_absolute_value=True), [bgn, bst], [bst])
        TT_(p, "dve", st[:, 6:7], st[:, 4:5], st[:, 5:6], ALU.mult, [bst], [bst])
        TS(p, "dve", st[:, 6:7], st[:, 6:7], -8.0, None, ALU.mult, None, [bst], [bst])
        TS(p, "dve", st[:, 7:8], st[:, 0:1], 1.0 - LAM_INIT, None, ALU.mult, None, [bst], [bst])
        Q, bQ = LOAD(p, "sp", [128, 8, TX], BF16, "Q", dq)
        Kp = Pool_(p, "K", [128, NKEY], BF16, 2)
        Vp = Pool_(p, "V", [128, NKC, 128], BF16, 2)
        Pp = Pool_(p, "P", [128, 512], BF16, 6)
        psS = Pool_(p, "psS", [128, 512], F32, 4, psum=True)
        psO = [p.ps(f"psO{i}", [128, 512], F32) for i in range(4)]
        bO = [p.buf() for _ in range(4)]
        ep = Pool_(p, "ep", [128, 512], F32, 6)
        sqp = Pool_(p, "sqe", [128, 512], BF16, 2)
        obp = Pool_(p, "obe", [128, 512], BF16, 2)
        for hp in range(8):
            Kh, bK = Kp.get()
            p.dma("sp", Kh[:], dk[hp], writes=[bK])
            Vh, bV = Vp.get()
            p.dma("sp", Vh[:], dv[hp], writes=[bV])
            for qt in range(TX // 512):
                q0 = qt * 512

                def S(kc):
                    out = []
                    for s_ in range(2):
                        ps, bps = psS.get()
                        lo = 64 * s_
                        MM(p, ps[:, :], [(Kh[lo:lo + 64, kc * 128:(kc + 1) * 128], Q[lo:lo + 64, hp, q0:q0 + 512])], [bK, bQ], [bps])
                        out.append((ps, bps))
                    return out

                cur = S(0)
                for kc in range(NKC):
                    nxt = S(kc + 1) if kc + 1 < NKC else None
                    for s_ in range(2):
                        ps, bps = cur[s_]
                        P_, bP = Pp.get()
                        ACT(p, P_[:], ps[:, :], AF.Exp, [bps, bst], [bP], scale=0.125, bias=st[:, 6:7])
                        MMx(p, psO[2 * s_][:, :], Vh[:, kc, :], P_[:], kc == 0, kc == NKC - 1, [bV, bP], [bO[2 * s_]])
                        MMx(p, psO[2 * s_ + 1][:, :], ones[:], P_[:], kc == 0, kc == NKC - 1, [bones, bP], [bO[2 * s_ + 1]])
                    cur = nxt
                r0, br0 = ep.get()
                p.op("dve", lambda e, r0=r0: e.reciprocal(out=r0[:], in_=psO[1][:, :]), [bO[1]], [br0])
                t0, bt0 = ep.get()
                TT_(p, "dve", t0[:], psO[0][:, :], r0[:], ALU.mult, [bO[0], br0], [bt0])
                r1, br1 = ep.get()
                p.op("dve", lambda e, r1=r1: e.reciprocal(out=r1[:], in_=psO[3][:, :]), [bO[3]], [br1])
                t1, bt1 = ep.get()
                TT_(p, "dve", t1[:], psO[2][:, :], r1[:], ALU.mult, [bO[2], br1], [bt1])
                o_, bo_ = ep.get()
                STT(p, o_[:], t1[:], st[:, 3:4], t0[:], ALU.mult, ALU.add, [bt1, bt0, bst], [bo_])
                sq, bsq = sqp.get()
                ACT(p, sq[:], o_[:], AF.Square, [bo_], [bsq])
                pss, bpss = psS.get()
                MM(p, pss[:, :], [(ones[:], sq[:])], [bones, bsq], [bpss])
                rs, brs = ep.get()
                ACT(p, rs[:], pss[:, :], AF.Sqrt, [bpss], [brs], scale=1.0 / 128, bias=1e-5)
                p.op("dve", lambda e, rs=rs: e.reciprocal(out=rs[:], in_=rs[:]), [brs], [brs])
                ob, bob = obp.get()
                STT(p, ob[:], o_[:], st[:, 7:8], rs[:], ALU.mult, ALU.mult, [bo_, bst, brs], [bob])
                p.dma("sp", oo_[:, hp, q0:q0 + 512], ob[:], reads=[bob])
        p.emit()
    return nc


def run_L6(inp, q, k, v):
    nc = build_L6()
    kx = np.concatenate([a[:, :, :TX] for a in k], axis=2)
    kc = np.concatenate([a[:, :, TX:] for a in k], axis=2)
    kall = np.ascontiguousarray(np.concatenate([kc, kx], axis=2).transpose(1, 0, 2))
    vx = np.concatenate([a[:TX] for a in v], axis=0)
    vc = np.concatenate([a[TX:] for a in v], axis=0)
    vall = np.concatenate([vc, vx], axis=0).reshape(NKC, 128, 8, 128)
    vall = np.ascontiguousarray(vall.transpose(2, 1, 0, 3))
    lamv = np.ascontiguousarray(np.broadcast_to(np.stack([inp["da_lam_q1"][0], inp["da_lam_k1"][0], inp["da_lam_q2"][0],
                                                          inp["da_lam_k2"][0]])[None], (128, 4, 64)))
    gains = np.ascontiguousarray(np.broadcast_to(np.stack([inp["da_q_norm"][0], inp["da_k_norm"][0]])[None], (128, 2, 64)))
    sub = np.ascontiguousarray(inp["da_subln"][0].reshape(128, 1))
    maps = [{"qT": q[c], "kT": kall, "v": vall, "lamv": lamv, "gains": gains, "subln": sub, "ones": ONES_F32} for c in range(NCORE)]
    res = run(nc, maps)
    return [np.asarray(r["oT"]) for r in res]


def ctx_pos_tables():
    L, N = 256, 512
    t = np.linspace(0.0, 1.0, L, dtype=np.float32)[:, None]
    w = (2.0 * np.float32(np.pi) * np.arange(L, dtype=np.float32)[:, None] / np.float32(L)).astype(np.float32)
    bands = np.linspace(1e-4, 15, 16, dtype=np.float32)[None]
    z = np.concatenate([t, np.cos(w * bands), -np.sin(w * bands)], axis=-1).astype(np.float32)
    maxd = np.log(1e-2) / 0.3
    mind = np.log(1e-2) / 1.5
    deltas = np.linspace(mind, maxd, 1024, dtype=np.float32)
    decay = np.exp(-t * np.abs(deltas)).astype(np.float32)
    zext = np.zeros((N, 33), np.float32)
    dext = np.zeros((N, 1024), np.float32)
    zext[:L] = z
    dext[:L] = decay
    j = np.arange(1, L)
    zext[N - j] = z[j]
    dext[N - j] = decay[j]
    return zext, dext


def hcc_tables():
    import ml_dtypes
    bf = ml_dtypes.bfloat16
    n = (np.arange(4)[None, :, None] * 128 + np.arange(128)[:, None, None]).astype(np.float64)
    k = np.arange(512)[None, None, :].astype(np.float64)
    th = 2 * np.pi * n * k / 512
    return {"COS": np.cos(th).astype(bf), "SIN": np.sin(th).astype(bf), "NSIN": (-np.sin(th)).astype(bf),
            "ones32": np.ones((128, 128), np.float32)}


def build_HCc():
    nc = bass.Bass("TRN2", target_bir_lowering=False)
    dU = nc.dram_tensor("U", [128, 2, 3, 128], F32, kind="ExternalInput").ap()
    dhid = nc.dram_tensor("hid", [64, 512], BF16, kind="ExternalInput").ap()
    dw4 = nc.dram_tensor("w4", [64, 2, 256], F32, kind="ExternalInput").ap()
    ddec = nc.dram_tensor("dec", [128, 4, 128], F32, kind="ExternalInput").ap()
    dskip = nc.dram_tensor("skip", [128, 2, 128], F32, kind="ExternalInput").ap()
    dC = nc.dram_tensor("COS", [128, 4, 512], BF16, kind="ExternalInput").ap()
    dS = nc.dram_tensor("SIN", [128, 4, 512], BF16, kind="ExternalInput").ap()
    dNS = nc.dram_tensor("NSIN", [128, 4, 512], BF16, kind="ExternalInput").ap()
    dones = nc.dram_tensor("ones32", [128, 128], F32, kind="ExternalInput").ap()
    dout = nc.dram_tensor("z2", [128, 2, 128], BF16, kind="ExternalOutput").ap()
    with ExitStack() as es:
        p = Prog(nc, es)
        U, bU = LOAD(p, "sp", [128, 2, 3, 128], F32, "U", dU)
        Hd, bHd = LOAD(p, "sp", [64, 512], BF16, "Hd", dhid)
        W4, bW4 = LOAD(p, "pool", [64, 2, 256], BF16, "W4", dw4)
        Dc, bDc = LOAD(p, "sp", [128, 4, 128], F32, "Dc", ddec)
        SK, bSK = LOAD(p, "sp", [128, 2, 128], F32, "SK", dskip)
        COS, bC = LOAD(p, "sp", [128, 4, 512], BF16, "COS", dC)
        SIN, bS = LOAD(p, "sp", [128, 4, 512], BF16, "SIN", dS)
        NSIN, bNS = LOAD(p, "sp", [128, 4, 512], BF16, "NSIN", dNS)
        ON, bON = LOAD(p, "sp", [128, 128], F32, "ON", dones)
        psp = Pool_(p, "ps", [128, 512], F32, 6, psum=True)
        Kt = p.sb("Kt", [128, 4, 256], F32); bKt = p.buf()
        Kb = p.sb("Kb", [128, 4, 256], BF16); bKb = p.buf()
        for j in range(4):
            ps, bps = psp.get()
            MM(p, ps[:, 0:256], [(Hd[:, j * 128:(j + 1) * 128], W4[:, j // 2, :])], [bHd, bW4], [bps])
            TT_(p, "dve", Kt[:, j, :].rearrange("p (o c) -> p o c", o=2), ps[:, 0:256].rearrange("p (o c) -> p o c", o=2),
                Dc[:, j, :].unsqueeze(1).to_broadcast([128, 2, 128]), ALU.mult, [bps, bDc], [bKt])
        CP(p, "act", Kb[:], Kt[:], [bKt], [bKb])
        red = p.sb("red", [128, 256], F32); bred = p.buf()
        p.op("dve", lambda e: e.tensor_reduce(out=red[:], in_=Kt[:].rearrange("p j oc -> p oc j"), axis=AX.X, op=ALU.add,
                                               apply_absolute_value=True), [bKt], [bred])
        sN = p.sb("sN", [128, 256], F32); bsN = p.buf()
        ps, bps = psp.get()
        MM(p, ps[:, 0:256], [(ON[:], red[:])], [bON, bred], [bps])
        TS(p, "dve", sN[:], ps[:, 0:256], 1e-6, None, ALU.add, None, [bps], [bsN])
        p.op("dve", lambda e: e.reciprocal(out=sN[:], in_=sN[:]), [bsN], [bsN])
        TS(p, "dve", sN[:], sN[:], 1.0 / 512, None, ALU.mult, None, [bsN], [bsN])
        Kf = p.sb("Kf", [128, 4, 2, 256], F32); bKf = p.buf()
        for kc in range(4):
            for ri, TB, bTB in ((0, COS, bC), (1, NSIN, bNS)):
                ps, bps = psp.get()
                MM(p, ps[:, 0:256], [(TB[:, j, kc * 128:(kc + 1) * 128], Kb[:, j, :]) for j in range(4)], [bTB, bKb], [bps])
                CP(p, "act", Kf[:, kc, ri, :], ps[:, 0:256], [bps], [bKf])
        Zf = p.sb("Zf", [128, 2, 128], F32); bZf = p.buf()
        Zb = p.sb("Zb", [128, 2, 128], BF16); bZb = p.buf()
        G = p.sb("G", [128, 4, 2, 128], BF16); bG = p.buf()
        O = p.sb("O", [128, 2, 128], BF16); bO = p.buf()
        tp = Pool_(p, "t", [128, 128], F32, 4)
        CP(p, "act", Zf[:], U[:, :, 0, :], [bU], [bZf])
        for o in range(2):
            CP(p, "act", Zb[:], Zf[:], [bZf], [bZb])
            for kc in range(4):
                pr, bpr = psp.get()
                MM(p, pr[:, 0:128], [(COS[:, j, kc * 128:(kc + 1) * 128], Zb[:, j, :]) for j in range(2)], [bC, bZb], [bpr])
                pi, bpi = psp.get()
                MM(p, pi[:, 0:128], [(NSIN[:, j, kc * 128:(kc + 1) * 128], Zb[:, j, :]) for j in range(2)], [bNS, bZb], [bpi])
                kr = Kf[:, kc, 0, o * 128:(o + 1) * 128]
                ki = Kf[:, kc, 1, o * 128:(o + 1) * 128]
                t1, b1 = tp.get()
                t2, b2 = tp.get()
                TT_(p, "dve", t1[:], pr[:, 0:128], kr, ALU.mult, [bpr, bKf], [b1])
                TT_(p, "dve", t2[:], pi[:, 0:128], ki, ALU.mult, [bpi, bKf], [b2])
                TT_(p, "pool", G[:, kc, 0, :], t1[:], t2[:], ALU.subtract, [b1, b2], [bG])
                t1, b1 = tp.get()
                t2, b2 = tp.get()
                TT_(p, "dve", t1[:], pr[:, 0:128], ki, ALU.mult, [bpr, bKf], [b1])
                TT_(p, "dve", t2[:], pi[:, 0:128], kr, ALU.mult, [bpi, bKf], [b2])
                TT_(p, "pool", G[:, kc, 1, :], t1[:], t2[:], ALU.add, [b1, b2], [bG])
            for i in range(2):
                py, bpy = psp.get()
                pairs = []
                for j in range(4):
                    pairs.append((COS[:, j, i * 128:(i + 1) * 128], G[:, j, 0, :]))
                    pairs.append((NSIN[:, j, i * 128:(i + 1) * 128], G[:, j, 1, :]))
                MM(p, py[:, 0:128], pairs, [bC, bNS, bG], [bpy])
                t1, b1 = tp.get()
                t2, b2 = tp.get()
                TT_(p, "dve", t1[:], py[:, 0:128], sN[:, o * 128:(o + 1) * 128], ALU.mult, [bpy, bsN], [b1])
                TT_(p, "pool", t2[:], Zf[:, i, :], SK[:, o, :], ALU.mult, [bZf, bSK], [b2])
                TT_(p, "pool", t2[:], t2[:], t1[:], ALU.add, [b1, b2], [b2])
                if o == 0:
                    TT_(p, "dve", Zf[:, i, :], t2[:], U[:, i, 1, :], ALU.mult, [b2, bU, bZb], [bZf])
                else:
                    TT_(p, "dve", O[:, i, :], t2[:], U[:, i, 2, :], ALU.mult, [b2, bU], [bO])
        p.dma("sp", dout, O[:], reads=[bO])
        p.emit()
    return nc


def run_LFc(inp, zext512):
    P = 512 // NCORE
    nc = build_LF(P)
    zp = np.ascontiguousarray(zext512.T)
    w23 = np.ascontiguousarray(np.stack([inp["hy_f_w2"][0], inp["hy_f_w3"][0]], axis=1))
    bf = np.ascontiguousarray(np.stack([inp["hy_f_b1"][0], inp["hy_f_b2"][0], inp["hy_f_b3"][0], inp["hy_f_freq"][0]], axis=1))
    maps = [{"zT": np.ascontiguousarray(zp[:, c * P:(c + 1) * P]), "w1": inp["hy_f_w1"][0], "w23": w23, "bf": bf}
            for c in range(NCORE)]
    res = run(nc, maps)
    return np.concatenate([np.asarray(r["hid"]) for r in res], axis=1)


def run_HCc(inp, uc, hid512, dext512):
    nc = build_HCc()
    tabs = hcc_tables()
    w4 = inp["hy_f_w4"][0].reshape(64, 2, 2, 1024)
    sk = inp["hy_skip"][0]
    maps = []
    for c in range(NCORE):
        ch = slice(c * 128, (c + 1) * 128)
        U = np.ascontiguousarray(uc.reshape(2, 128, 3, 1024)[:, :, :, ch].transpose(1, 0, 2, 3))
        W = np.ascontiguousarray(w4[:, :, :, ch].transpose(0, 2, 1, 3).reshape(64, 2, 256))
        D = np.ascontiguousarray(dext512[:, ch].reshape(4, 128, 128).transpose(1, 0, 2))
        S = np.ascontiguousarray(np.broadcast_to(sk[:, ch][None], (128, 2, 128)))
        m = {"U": U, "hid": hid512, "w4": W, "dec": D, "skip": S}
        m.update(tabs)
        maps.append(m)
    res = run(nc, maps)
    zs = [np.asarray(r["z2"]).transpose(1, 0, 2).reshape(256, 128) for r in res]
    return np.concatenate(zs, axis=1)


def kernel(**inp):
    inp = {k: np.asarray(v) for k, v in inp.items()}
    x = inp["x"][0]
    ctx = inp["ctx"][0]
    T0 = TX + TC
    mods = run_L0(inp)
    xsl = shard_tokens(x, ctx)
    h = run_L1(xsl, mods, 0, inp["norm_mix"][0], T0)
    hx, hc = unshard_tokens([np.asarray(a) for a in h], True)
    u, uc = run_HA(inp, hx, hc)
    tabs = hc_tables()
    nc_hc = build_HC()
    zext, dext = hyena_pos_tables(16384)
    hid = run_LF(inp, zext)
    z = run_HC(inp, u, hid, dext, tabs, nc=nc_hc)
    zext_c, dext_c = ctx_pos_tables()
    hid_c = run_LFc(inp, zext_c)
    zc = run_HCc(inp, uc, hid_c, dext_c)
    zsl = shard_tokens(np.asarray(z), np.asarray(zc))
    x1, h2, aff = run_L3(zsl, xsl, inp["hy_w_out"][0], inp["hy_b_out"][0], mods, 0, inp["norm_ffn"][0],
                         inp["moe_router"][0], T0, True)
    x2 = run_L4([np.asarray(a) for a in x1], [np.asarray(a) for a in h2], aff, mods, 0,
                inp["moe_w_gate"][0], inp["moe_w_up"][0], inp["moe_w_down"][0], T0, True)
    x2 = [np.asarray(a) for a in x2]
    q, k, v = run_L5(inp, x2, mods, T0)
    o = run_L6(inp, q, k, v)
    xs1 = [np.ascontiguousarray(a[:, :, :TX]) for a in x2]
    x3, h3, aff1 = run_L3(o, xs1, inp["da_w_out"][0], None, mods, 1, inp["norm_ffn"][1], inp["moe_router"][1], TX, False)
    x4 = run_L4([np.asarray(a) for a in x3], [np.asarray(a) for a in h3], aff1, mods, 1,
                inp["moe_w_gate"][1], inp["moe_w_up"][1], inp["moe_w_down"][1], TX, False)
    xo, _ = unshard_tokens([np.asarray(a) for a in x4], False)
    return np.ascontiguousarray(xo[None]).astype(np.float32)
```

```python
import numpy as np
from contextlib import ExitStack
import concourse.bass as bass
import concourse.mybir as mybir
from concourse.bass_utils import run_bass_kernel_spmd

F32 = mybir.dt.float32
BF16 = mybir.dt.bfloat16
I32 = mybir.dt.int32
ALU = mybir.AluOpType
AF = mybir.ActivationFunctionType
AX = mybir.AxisListType

EPOCH = 30000
N_DMA_SEMS = 24


class Buf:
    __slots__ = ("name", "w", "r")

    def __init__(self, name=""):
        self.name = name
        self.w = None
        self.r = {}


class Prog:
    ENGS = ("pe", "act", "dve", "pool", "sp")

    def __init__(self, nc, es):
        self.nc = nc
        self.es = es
        self.items = {e: [] for e in self.ENGS}
        self.cnt = {e: 0 for e in self.ENGS}
        self.esems = {e: [es.enter_context(nc.semaphore(f"s_{e}_0"))] for e in self.ENGS}
        self.seen = {e: {} for e in self.ENGS}
        self.dsems = {}
        self.dcount = {}
        self.drr = {}
        for q in ("sp", "pool", "act"):
            self.dsems[q] = [es.enter_context(nc.semaphore(f"d_{q}_{i}")) for i in range(N_DMA_SEMS)]
            self.dcount[q] = [0] * N_DMA_SEMS
            self.drr[q] = 0
        self.all_dma_tokens = []
        self.nbuf = 0

    def sb(self, name, shape, dt):
        t = self.es.enter_context(self.nc.sbuf_tensor("sb_" + name, list(shape), dt))
        return t

    def ps(self, name, shape, dt=F32):
        t = self.es.enter_context(self.nc.psum_tensor("ps_" + name, list(shape), dt))
        return t

    def buf(self, name=""):
        self.nbuf += 1
        return Buf(name or f"b{self.nbuf}")

    def _wait(self, eng, tok):
        if tok is None:
            return
        key, sem, val = tok
        if self.seen[eng].get(key, 0) >= val:
            return
        self.seen[eng][key] = val
        self.items[eng].append(("wait", sem, val))

    def _deps(self, eng, reads, writes):
        for b in reads:
            if b.w is not None:
                self._dep1(eng, b.w)
        for b in writes:
            if b.w is not None and b.w[0][0] != eng:
                self._dep1(eng, b.w)
            for t in b.r.values():
                if t[0][0] != eng:
                    self._dep1(eng, t)

    def _dep1(self, eng, tok):
        if eng == "pe" and tok[0][0] == "pe":
            return
        self._wait(eng, tok)

    def _mark(self, eng, tok, reads, writes):
        for b in reads:
            b.r[tok[0]] = tok
        for b in writes:
            b.w = tok
            b.r = {}

    def op(self, eng, fn, reads=(), writes=()):
        self._deps(eng, reads, writes)
        ep = self.cnt[eng] // EPOCH
        if ep >= len(self.esems[eng]):
            self.esems[eng].append(self.es.enter_context(self.nc.semaphore(f"s_{eng}_{ep}")))
        self.cnt[eng] += 1
        sem = self.esems[eng][ep]
        val = self.cnt[eng] - ep * EPOCH
        self.items[eng].append(("ins", fn, sem, 1))
        tok = ((eng, ep), sem, val)
        self._mark(eng, tok, reads, writes)
        return tok

    def group(self, eng, fns, reads=(), writes=()):
        self._deps(eng, reads, writes)
        tok = None
        for fn in fns:
            ep = self.cnt[eng] // EPOCH
            if ep >= len(self.esems[eng]):
                self.esems[eng].append(self.es.enter_context(self.nc.semaphore(f"s_{eng}_{ep}")))
            self.cnt[eng] += 1
            sem = self.esems[eng][ep]
            val = self.cnt[eng] - ep * EPOCH
            self.items[eng].append(("ins", fn, sem, 1))
            tok = ((eng, ep), sem, val)
        self._mark(eng, tok, reads, writes)
        return tok

    def dma(self, q, out, in_, reads=(), writes=(), **kw):
        self._deps(q, reads, writes)
        k = self.drr[q]
        self.drr[q] = (k + 1) % N_DMA_SEMS
        n = self.dcount[q][k]
        sem = self.dsems[q][k]
        if n > 0:
            self._wait(q, (("d", q, k), sem, 16 * n))
        self.dcount[q][k] = n + 1
        tok = (("d", q, k), sem, 16 * (n + 1))

        def fn(e, out=out, in_=in_, kw=kw):
            return e.dma_start(out=out, in_=in_, **kw)

        self.items[q].append(("ins", fn, sem, 16))
        self._mark(q, tok, reads, writes)
        self.all_dma_tokens.append(tok)
        return tok

    def coll(self, kind, in_ap, out_ap, reads=(), writes=(), groups=None):
        q = "pool"
        if not hasattr(self, "ccsem"):
            self.ccsem = self.es.enter_context(self.nc.semaphore("cc_sem"))
            self.ccn = 0
        self._deps(q, reads, writes)
        self.ccn += 1
        tok = (("cc",), self.ccsem, self.ccn)
        groups = groups or [list(range(8))]

        def fn(e):
            return e.collective_compute(kind, mybir.AluOpType.bypass, replica_groups=groups, ins=[in_ap.opt()], outs=[out_ap.opt()])

        self.items[q].append(("ins", fn, self.ccsem, 1))
        self._mark(q, tok, reads, writes)
        self.all_dma_tokens.append(tok)
        return tok

    def finish(self):
        last = {}
        for tok in self.all_dma_tokens:
            last[tok[0]] = tok
        for tok in last.values():
            self._wait("sp", tok)

    def emit(self):
        nc = self.nc
        self.finish()
        items = self.items
        with nc.Block() as block:
            def run(e, lst):
                for it in lst:
                    if it[0] == "wait":
                        e.wait_ge(it[1], it[2])
                    else:
                        it[1](e).then_inc(it[2], it[3])

            @block.tensor
            def _(e):
                run(e, items["pe"])

            @block.scalar
            def _(e):
                run(e, items["act"])

            @block.vector
            def _(e):
                run(e, items["dve"])

            @block.gpsimd
            def _(e):
                run(e, items["pool"])

            @block.sync
            def _(e):
                run(e, items["sp"])


class TT:
    def __init__(self, p, name, shape, dt, ntiles):
        self.t = p.sb(name, shape, dt)
        self.b = [p.buf(f"{name}_{i}") for i in range(ntiles)]


class Pool_:
    def __init__(self, p, name, shape, dt, n, psum=False):
        mk = p.ps if psum else p.sb
        self.ts = [mk(f"{name}{i}", shape, dt) for i in range(n)]
        self.bs = [p.buf(f"{name}{i}") for i in range(n)]
        self.i = 0

    def get(self):
        k = self.i
        self.i = (k + 1) % len(self.ts)
        return self.ts[k], self.bs[k]


def col_tiles(T, w=512):
    out = []
    c = 0
    while c < T:
        out.append((c, min(w, T - c)))
        c += w
    return out


def ACT(p, out, in_, func, reads, writes, **kw):
    return p.op("act", lambda e: e.activation(out=out, in_=in_, func=func, **kw), reads, writes)


def TT_(p, eng, out, in0, in1, op, reads, writes):
    return p.op(eng, lambda e: e.tensor_tensor(out=out, in0=in0, in1=in1, op=op), reads, writes)


def TS(p, eng, out, in0, s1, s2, op0, op1, reads, writes, **kw):
    if op1 is None:
        return p.op(eng, lambda e: e.tensor_scalar(out=out, in0=in0, scalar1=s1, scalar2=None, op0=op0, **kw), reads, writes)
    return p.op(eng, lambda e: e.tensor_scalar(out=out, in0=in0, scalar1=s1, scalar2=s2, op0=op0, op1=op1, **kw), reads, writes)


def STT(p, out, in0, scalar, in1, op0, op1, reads, writes):
    return p.op("dve", lambda e: e.scalar_tensor_tensor(out=out, in0=in0, scalar=scalar, in1=in1, op0=op0, op1=op1), reads, writes)


def CP(p, eng, out, in_, reads, writes):
    if eng == "act":
        return p.op("act", lambda e: e.activation(out=out, in_=in_, func=AF.Copy), reads, writes)
    return p.op(eng, lambda e: e.tensor_copy(out=out, in_=in_), reads, writes)


def MM(p, out, pairs, reads, writes):
    n = len(pairs)
    fns = []
    for i, (l, r) in enumerate(pairs):
        fns.append(lambda e, l=l, r=r, i=i: e.matmul(out, l, r, start=(i == 0), stop=(i == n - 1)))
    return p.group("pe", fns, reads, writes)


def LOAD(p, q, shape, dt, name, src, es=None):
    t = p.sb(name, shape, dt)
    b = p.buf(name)
    p.dma(q, t[:], src, writes=[b])
    return t, b


class Ctx:
    pass


def make_common(p, nc, ones_src):
    C = Ctx()
    C.ones, C.bones = LOAD(p, "pool", [128, 128], BF16, "ones", ones_src)
    C.sq = Pool_(p, "sq", [128, 8, 512], BF16, 2)
    C.ps = Pool_(p, "psA", [128, 512], F32, 4, psum=True)
    C.rs = Pool_(p, "rs", [128, 512], F32, 2)
    C.tmp = Pool_(p, "tmp", [128, 512], F32, 3)
    return C


def emit_gmod(p, name, g, bg, scale, bscale, ncol=8):
    gm = p.sb(name, [128, ncol], F32)
    bgm = p.buf(name)
    STT(p, gm[:], scale, 1.0, g, ALU.add, ALU.mult, [bg, bscale], [bgm])
    return gm, bgm


def emit_rms_mod(p, C, X, tiles, mods_of_tile, OUT, OUTF=None, eps=1e-6, D=1024):
    for ti, (c0, w) in enumerate(tiles):
        gm, bgm, sh, bsh = mods_of_tile(ti)
        sq, bsq = C.sq.get()
        ACT(p, sq[:, :, 0:w], X.t[:, :, c0:c0 + w], AF.Square, [X.b[ti]], [bsq])
        ps, bps = C.ps.get()
        MM(p, ps[:, 0:w], [(C.ones[:], sq[:, kc, 0:w]) for kc in range(8)], [bsq, C.bones], [bps])
        rs, brs = C.rs.get()
        ACT(p, rs[:, 0:w], ps[:, 0:w], AF.Sqrt, [bps], [brs], scale=1.0 / D, bias=eps)
        p.op("dve", lambda e, rs=rs, w=w: e.reciprocal(out=rs[:, 0:w], in_=rs[:, 0:w]), [brs], [brs])
        for kc in range(8):
            tmp, btmp = C.tmp.get()
            STT(p, tmp[:, 0:w], X.t[:, kc, c0:c0 + w], gm[:, kc:kc + 1], rs[:, 0:w], ALU.mult, ALU.mult,
                [X.b[ti], brs, bgm], [btmp])
            ACT(p, OUT.t[:, kc, c0:c0 + w], tmp[:, 0:w], AF.Identity, [btmp, bsh], [OUT.b[ti]],
                bias=sh[:, kc:kc + 1], scale=1.0)
            if OUTF is not None:
                ACT(p, OUTF.t[:, kc, c0:c0 + w], tmp[:, 0:w], AF.Identity, [btmp, bsh], [OUTF.b[ti]],
                    bias=sh[:, kc:kc + 1], scale=1.0)


def fm(a):
    T = a.shape[0]
    return np.ascontiguousarray(a.T.reshape(8, 128, T).transpose(1, 0, 2))


def unfm(a):
    T = a.shape[2]
    return np.ascontiguousarray(a.transpose(1, 0, 2).reshape(1024, T).T)


def vec_fm(v, n=8):
    return np.ascontiguousarray(v.reshape(n, 128).T)


NCORE = 8
TX = 2048
TC = 32
ONES_F32 = np.ones((128, 128), np.float32)


def run(nc, in_maps):
    res = run_bass_kernel_spmd(nc, in_maps, core_ids=list(range(NCORE)))
    return res.results


def build_L0():
    nc = bass.Bass("TRN2", target_bir_lowering=False)
    condT = nc.dram_tensor("condT", [128, 8, 2], F32, kind="ExternalInput").ap()
    w = nc.dram_tensor("w", [2, 128, 8, 768], F32, kind="ExternalInput").ap()
    b = nc.dram_tensor("b", [2, 2, 768], F32, kind="ExternalInput").ap()
    out = nc.dram_tensor("out", [2, 2, 768], F32, kind="ExternalOutput").ap()
    with ExitStack() as es:
        p = Prog(nc, es)
        ct, bct = LOAD(p, "sp", [128, 8, 2], F32, "ct", condT)
        sc = p.sb("sc", [128, 8, 2], F32); bsc = p.buf()
        ACT(p, sc[:], ct[:], AF.Silu, [bct], [bsc])
        psp = Pool_(p, "ps", [128, 512], F32, 2, psum=True)
        for l in range(2):
            wt, bwt = LOAD(p, "sp", [128, 8, 768], F32, f"w{l}", w[l])
            bt, bbt = LOAD(p, "sp", [2, 768], F32, f"b{l}", b[l])
            ot = p.sb(f"o{l}", [2, 768], F32); bot = p.buf()
            for h in range(2):
                ps, bps = psp.get()
                MM(p, ps[0:2, 0:384], [(sc[:, kc, :], wt[:, kc, h * 384:(h + 1) * 384]) for kc in range(8)],
                   [bsc, bwt], [bps])
                TT_(p, "dve", ot[:, h * 384:(h + 1) * 384], ps[0:2, 0:384], bt[:, h * 384:(h + 1) * 384], ALU.add,
                    [bps, bbt], [bot])
            p.dma("sp", out[l], ot[:], reads=[bot])
        p.emit()
    return nc


def run_L0(inp):
    nc = build_L0()
    cond = np.stack([inp["c"][0], inp["c_ctx"]], axis=1)
    condT = np.ascontiguousarray(cond.reshape(8, 128, 2).transpose(1, 0, 2))
    maps = []
    for c in range(NCORE):
        sl = slice(c * 768, (c + 1) * 768)
        w = np.ascontiguousarray(inp["ada_w"][:, :, sl].reshape(2, 8, 128, 768).transpose(0, 2, 1, 3))
        b = np.ascontiguousarray(np.broadcast_to(inp["ada_b"][:, None, sl], (2, 2, 768)))
        maps.append({"condT": condT, "w": w, "b": b})
    res = run(nc, maps)
    mods = np.concatenate([r["out"] for r in res], axis=2)
    return mods


def mods_fm(mods, l):
    m = mods[l].reshape(2, 6, 8, 128)
    return np.ascontiguousarray(m.transpose(3, 0, 1, 2))


def tiles_of(T):
    return col_tiles(T, 512)


def build_L1(T):
    nc = bass.Bass("TRN2", target_bir_lowering=False)
    xT = nc.dram_tensor("xT", [128, 8, T], F32, kind="ExternalInput").ap()
    md = nc.dram_tensor("md", [128, 2, 6, 8], F32, kind="ExternalInput").ap()
    g = nc.dram_tensor("g", [128, 8], F32, kind="ExternalInput").ap()
    ones = nc.dram_tensor("ones", [128, 128], F32, kind="ExternalInput").ap()
    hT = nc.dram_tensor("hT", [128, 8, T], BF16, kind="ExternalOutput").ap()
    tiles = tiles_of(T)
    with ExitStack() as es:
        p = Prog(nc, es)
        C = make_common(p, nc, ones)
        mdt, bmd = LOAD(p, "sp", [128, 2, 6, 8], F32, "md", md)
        gt, bg = LOAD(p, "sp", [128, 8], F32, "g", g)
        X = TT(p, "X", [128, 8, T], F32, len(tiles))
        H = TT(p, "H", [128, 8, T], BF16, len(tiles))
        for ti, (c0, w) in enumerate(tiles):
            p.dma("sp", X.t[:, :, c0:c0 + w], xT[:, :, c0:c0 + w], writes=[X.b[ti]])
        gmx, bgmx = emit_gmod(p, "gmx", gt[:], bg, mdt[:, 0, 1, :], bmd)
        gmc, bgmc = emit_gmod(p, "gmc", gt[:], bg, mdt[:, 1, 1, :], bmd)

        def mods_of_tile(ti):
            c0, w = tiles[ti]
            if c0 >= TX:
                return gmc, bgmc, mdt[:, 1, 0, :], bmd
            return gmx, bgmx, mdt[:, 0, 0, :], bmd

        emit_rms_mod(p, C, X, tiles, mods_of_tile, H)
        for ti, (c0, w) in enumerate(tiles):
            p.dma("sp", hT[:, :, c0:c0 + w], H.t[:, :, c0:c0 + w], reads=[H.b[ti]])
        p.emit()
    return nc


def shard_tokens(x, ctx):
    out = []
    for c in range(NCORE):
        parts = [x[c * TX:(c + 1) * TX]]
        if ctx is not None:
            parts.append(ctx[c * TC:(c + 1) * TC])
        out.append(fm(np.concatenate(parts, axis=0)))
    return out


def unshard_tokens(slabs, has_ctx):
    xs, cs = [], []
    for s in slabs:
        a = unfm(s)
        xs.append(a[:TX])
        if has_ctx:
            cs.append(a[TX:])
    return np.concatenate(xs, 0), (np.concatenate(cs, 0) if has_ctx else None)


def run_L1(xsl, mods, l, g, T):
    nc = build_L1(T)
    md = mods_fm(mods, l)
    maps = [{"xT": xsl[c], "md": md, "g": vec_fm(g), "ones": ONES_F32} for c in range(NCORE)]
    res = run(nc, maps)
    return [r["hT"] for r in res]


PI = float(np.pi)


def build_LF(P):
    nc = bass.Bass("TRN2", target_bir_lowering=False)
    zT = nc.dram_tensor("zT", [33, P], F32, kind="ExternalInput").ap()
    w1 = nc.dram_tensor("w1", [33, 64], F32, kind="ExternalInput").ap()
    w23 = nc.dram_tensor("w23", [64, 2, 64], F32, kind="ExternalInput").ap()
    bf = nc.dram_tensor("bf", [64, 4], F32, kind="ExternalInput").ap()
    hid = nc.dram_tensor("hid", [64, P], BF16, kind="ExternalOutput").ap()
    tiles = col_tiles(P)
    with ExitStack() as es:
        p = Prog(nc, es)
        w1t, bw1 = LOAD(p, "sp", [33, 64], F32, "w1", w1)
        w23t, bw23 = LOAD(p, "sp", [64, 2, 64], F32, "w23", w23)
        bft, bbf = LOAD(p, "sp", [64, 4], F32, "bf", bf)
        bfr = p.sb("bfr", [64, 3], F32); bbfr = p.buf()
        TS(p, "dve", bfr[:], bft[:, 0:3], bft[:, 3:4], None, ALU.mult, None, [bbf], [bbfr])
        zp = Pool_(p, "z", [33, 512], F32, 2)
        psp = Pool_(p, "ps", [64, 512], F32, 3, psum=True)
        ap_ = Pool_(p, "arg", [64, 512], F32, 3)
        hp = Pool_(p, "h", [64, 512], F32, 3)
        op_ = Pool_(p, "o", [64, 512], BF16, 2)
        wp = Pool_(p, "wr", [64, 512], F32, 2)
        for (c0, w) in tiles:
            zt, bz = zp.get()
            p.dma("sp", zt[:, 0:w], zT[:, c0:c0 + w], writes=[bz])
            cur, bcur = zt, bz
            for l in range(3):
                ps, bps = psp.get()
                lhsT = w1t[:] if l == 0 else w23t[:, l - 1, :]
                bl = bw1 if l == 0 else bw23
                MM(p, ps[:, 0:w], [(lhsT, cur[:, 0:w])], [bl, bcur], [bps])
                a, ba = ap_.get()
                ACT(p, a[:, 0:w], ps[:, 0:w], AF.Identity, [bps, bbf, bbfr], [ba], scale=bft[:, 3:4], bias=bfr[:, l:l + 1])
                wt_, bwt_ = wp.get()
                TS(p, "dve", wt_[:, 0:w], a[:, 0:w], -PI, 2 * PI, ALU.is_lt, ALU.mult, [ba], [bwt_])
                TT_(p, "dve", a[:, 0:w], a[:, 0:w], wt_[:, 0:w], ALU.add, [ba, bwt_], [ba])
                wt_, bwt_ = wp.get()
                TS(p, "dve", wt_[:, 0:w], a[:, 0:w], PI, -2 * PI, ALU.is_gt, ALU.mult, [ba], [bwt_])
                TT_(p, "dve", a[:, 0:w], a[:, 0:w], wt_[:, 0:w], ALU.add, [ba, bwt_], [ba])
                if l < 2:
                    h, bh = hp.get()
                else:
                    h, bh = op_.get()
                ACT(p, h[:, 0:w], a[:, 0:w], AF.Sin, [ba], [bh])
                cur, bcur = h, bh
            p.dma("sp", hid[:, c0:c0 + w], cur[:, 0:w], reads=[bcur])
        p.emit()
    return nc


def hyena_pos_tables(L):
    N = 32768
    t = np.linspace(0.0, 1.0, L, dtype=np.float32)[:, None]
    w = (2.0 * np.float32(np.pi) * np.arange(L, dtype=np.float32)[:, None] / np.float32(L)).astype(np.float32)
    bands = np.linspace(1e-4, 15, 16, dtype=np.float32)[None]
    z = np.concatenate([t, np.cos(w * bands), -np.sin(w * bands)], axis=-1).astype(np.float32)
    maxd = np.log(1e-2) / 0.3
    mind = np.log(1e-2) / 1.5
    deltas = np.linspace(mind, maxd, 1024, dtype=np.float32)
    decay = np.exp(-t * np.abs(deltas)).astype(np.float32)
    zext = np.zeros((N, 33), np.float32)
    dext = np.zeros((N, 1024), np.float32)
    zext[:L] = z
    dext[:L] = decay
    j = np.arange(1, L)
    zext[N - j] = z[j]
    dext[N - j] = decay[j]
    return zext, dext


def perm_pos(a):
    F_ = a.shape[1]
    return np.ascontiguousarray(a.reshape(256, 128, F_).transpose(2, 1, 0))


def run_LF(inp, zext):
    P = 32768 // NCORE
    nc = build_LF(P)
    zp = perm_pos(zext).reshape(33, 32768)
    w23 = np.ascontiguousarray(np.stack([inp["hy_f_w2"][0], inp["hy_f_w3"][0]], axis=1))
    bf = np.ascontiguousarray(np.stack([inp["hy_f_b1"][0], inp["hy_f_b2"][0], inp["hy_f_b3"][0], inp["hy_f_freq"][0]], axis=1))
    maps = [{"zT": np.ascontiguousarray(zp[:, c * P:(c + 1) * P]), "w1": inp["hy_f_w1"][0], "w23": w23, "bf": bf}
            for c in range(NCORE)]
    res = run(nc, maps)
    hid = np.concatenate([np.asarray(r["hid"]) for r in res], axis=1)
    return hid.reshape(64, 128, 256)


def build_HA(segs):
    Th = sum(n + 2 for _, n in segs)
    T = sum(n for _, n in segs)
    nc = bass.Bass("TRN2", target_bir_lowering=False)
    hT = nc.dram_tensor("hT", [128, 8, Th], BF16, kind="ExternalInput").ap()
    valid = nc.dram_tensor("valid", [1, Th], BF16, kind="ExternalInput").ap()
    win = nc.dram_tensor("win", [24, 128, 8, 128], F32, kind="ExternalInput").ap()
    cw = nc.dram_tensor("cw", [128, 24, 3], F32, kind="ExternalInput").ap()
    brow = nc.dram_tensor("brow", [1, 3072], F32, kind="ExternalInput").ap()
    cb = nc.dram_tensor("cb", [128, 24], F32, kind="ExternalInput").ap()
    uT = nc.dram_tensor("uT", [128, 24, T], F32, kind="ExternalOutput").ap()
    tiles = []
    o0 = 0
    for hb, n in segs:
        for (c0, w) in col_tiles(n, 510):
            tiles.append((hb + c0, o0 + c0, w))
        o0 += n
    with ExitStack() as es:
        p = Prog(nc, es)
        H, bH = LOAD(p, "sp", [128, 8, Th], BF16, "H", hT)
        V, bV = LOAD(p, "sp", [1, Th], BF16, "V", valid)
        BR, bBR = LOAD(p, "pool", [1, 3072], BF16, "BR", brow)
        CB, bCB = LOAD(p, "sp", [128, 24], F32, "CB", cb)
        CW, bCW = LOAD(p, "sp", [128, 24, 3], F32, "CW", cw)
        wp = Pool_(p, "w", [128, 8, 128], BF16, 3)
        psp = Pool_(p, "ps", [128, 512], F32, 6, psum=True)
        op_ = Pool_(p, "o", [128, T], F32, 3)
        for m in range(24):
            wt, bw = wp.get()
            p.dma("pool", wt[:], win[m], writes=[bw])
            ot, bo = op_.get()
            for (hb, ob, w) in tiles:
                ps, bps = psp.get()
                pairs = [(wt[:, kc, :], H[:, kc, hb:hb + w + 2]) for kc in range(8)]
                pairs.append((BR[0:1, m * 128:(m + 1) * 128], V[0:1, hb:hb + w + 2]))
                MM(p, ps[:, 0:w + 2], pairs, [bw, bH, bV, bBR], [bps])
                ACT(p, ot[:, ob:ob + w], ps[:, 1:w + 1], AF.Identity, [bps, bCB, bCW], [bo], bias=CB[:, m:m + 1], scale=CW[:, m, 1:2])
                STT(p, ot[:, ob:ob + w], ps[:, 0:w], CW[:, m, 0:1], ot[:, ob:ob + w], ALU.mult, ALU.add, [bps, bCW, bo], [bo])
                STT(p, ot[:, ob:ob + w], ps[:, 2:w + 2], CW[:, m, 2:3], ot[:, ob:ob + w], ALU.mult, ALU.add, [bps, bCW, bo], [bo])
            p.dma("sp", uT[:, m, :], ot[:], reads=[bo])
        p.emit()
    return nc


def halo_slabs(hx, hc):
    import ml_dtypes
    out = []
    for c in range(NCORE):
        parts, val = [], []
        for a, n in ((hx, TX), (hc, TC)):
            if a is None:
                continue
            L = a.shape[0]
            seg = np.zeros((n + 2, 1024), a.dtype)
            v = np.zeros((n + 2,), np.float32)
            lo, hi = c * n - 1, (c + 1) * n + 1
            slo, shi = max(lo, 0), min(hi, L)
            seg[slo - lo:shi - lo] = a[slo:shi]
            v[slo - lo:shi - lo] = 1.0
            parts.append(seg)
            val.append(v)
        out.append((fm(np.concatenate(parts, 0)), np.concatenate(val)[None].astype(ml_dtypes.bfloat16)))
    return out


def run_HA(inp, hx, hc):
    segs = [(0, TX)] + ([(TX + 2, TC)] if hc is not None else [])
    nc = build_HA(segs)
    W = inp["hy_w_in"][0]
    win = np.ascontiguousarray(W.reshape(8, 128, 24, 128).transpose(2, 1, 0, 3))
    cwv = inp["hy_conv_w"][0]
    cw = np.ascontiguousarray(cwv.reshape(3, 24, 128).transpose(2, 1, 0))
    brow = np.ascontiguousarray(inp["hy_b_in"][0][None])
    cb = vec_fm(inp["hy_conv_b"][0], 24)
    sl = halo_slabs(hx, hc)
    maps = [{"hT": sl[c][0], "valid": sl[c][1], "win": win, "cw": cw, "brow": brow, "cb": cb} for c in range(NCORE)]
    res = run(nc, maps)
    us, ucs = [], []
    for r in res:
        a = np.asarray(r["uT"]).transpose(1, 0, 2).reshape(3072, -1).T
        us.append(a[:TX])
        ucs.append(a[TX:])
    return np.concatenate(us, 0), (np.concatenate(ucs, 0) if hc is not None else None)


NG, GC = 8, 16


def hc_tables():
    import ml_dtypes
    bf = ml_dtypes.bfloat16
    N = 32768
    n1 = np.arange(128)[:, None].astype(np.float64)
    k1 = np.arange(256)[None].astype(np.float64)
    F1 = np.zeros((128, 2, 512), np.float64)
    for h in range(2):
        a = 2 * np.pi * (n1 + 128 * h) * k1 / 256
        F1[:, h, :256] = np.cos(a)
        F1[:, h, 256:] = -np.sin(a)
    phi = 2 * np.pi * np.arange(128)[:, None] * np.arange(256)[None] / N
    TW = np.stack([np.cos(phi), np.sin(phi)], 1)
    th = 2 * np.pi * np.arange(128)[:, None] * np.arange(128)[None] / 128
    F2 = np.stack([np.cos(th), np.sin(th), -np.sin(th)], 1)
    M3 = np.zeros((128, 2, 256), np.float64)
    M3[:, 0, :128] = np.cos(th); M3[:, 0, 128:] = np.sin(th)
    M3[:, 1, :128] = -np.sin(th); M3[:, 1, 128:] = np.cos(th)
    TWI = np.zeros((128, 2, 2, 128), np.float64)
    S4 = np.zeros((128, 2, 2, 128), np.float64)
    kp = np.arange(128)[:, None]
    for half in range(2):
        ph = 2 * np.pi * np.arange(128)[None] * (half * 128 + kp) / N
        TWI[:, half, 0] = np.cos(ph); TWI[:, half, 1] = np.sin(ph)
        ps_ = 2 * np.pi * np.arange(128)[None] * (half * 128 + kp) / 256
        S4[:, half, 0] = np.cos(ps_) / N; S4[:, half, 1] = -np.sin(ps_) / N
    return {"F1": F1.astype(bf), "TW": TW.astype(np.float32), "F2": F2.astype(bf), "M3": M3.astype(bf),
            "TWI": TWI.astype(np.float32), "S4": S4.astype(bf), "ones32": np.ones((128, 128), np.float32)}


HC_STOP = [0]


class _Stop(Exception):
    pass


def _chk(n):
    if HC_STOP[0] == n:
        raise _Stop()


def build_HC():
    nc = bass.Bass("TRN2", target_bir_lowering=False)
    dU = nc.dram_tensor("U", [NG, 128, 3, GC, 128], F32, kind="ExternalInput").ap()
    dhid = nc.dram_tensor("hid", [64, 128, 256], BF16, kind="ExternalInput").ap()
    dw4 = nc.dram_tensor("w4", [NG, 64, 2, 2 * GC], F32, kind="ExternalInput").ap()
    ddec = nc.dram_tensor("dec", [NG, 128, 2, 128, GC], F32, kind="ExternalInput").ap()
    dskip = nc.dram_tensor("skip", [128, NG, 2, GC], F32, kind="ExternalInput").ap()
    dF1 = nc.dram_tensor("F1", [128, 2, 512], BF16, kind="ExternalInput").ap()
    dTW = nc.dram_tensor("TW", [128, 2, 256], F32, kind="ExternalInput").ap()
    dF2 = nc.dram_tensor("F2", [128, 3, 128], BF16, kind="ExternalInput").ap()
    dM3 = nc.dram_tensor("M3", [128, 2, 256], BF16, kind="ExternalInput").ap()
    dTWI = nc.dram_tensor("TWI", [128, 2, 2, 128], F32, kind="ExternalInput").ap()
    dS4 = nc.dram_tensor("S4", [128, 2, 2, 128], BF16, kind="ExternalInput").ap()
    dones = nc.dram_tensor("ones32", [128, 128], F32, kind="ExternalInput").ap()
    dout = nc.dram_tensor("z2", [NG, 128, GC, 128], BF16, kind="ExternalOutput").ap()
    with ExitStack() as es:
        p = Prog(nc, es)
        F1, bF1 = LOAD(p, "sp", [128, 2, 512], BF16, "F1", dF1)
        TW, bTW = LOAD(p, "sp", [128, 2, 256], F32, "TW", dTW)
        F2, bF2 = LOAD(p, "sp", [128, 3, 128], BF16, "F2", dF2)
        M3, bM3 = LOAD(p, "sp", [128, 2, 256], BF16, "M3", dM3)
        TWI, bTWI = LOAD(p, "sp", [128, 2, 2, 128], F32, "TWI", dTWI)
        S4, bS4 = LOAD(p, "sp", [128, 2, 2, 128], BF16, "S4", dS4)
        ON, bON = LOAD(p, "sp", [128, 128], F32, "ON", dones)
        SK, bSK = LOAD(p, "sp", [128, NG, 2, GC], F32, "SK", dskip)
        cst = [bF1, bTW, bF2, bM3, bTWI, bS4]
        Up = Pool_(p, "U", [128, 3, GC, 128], F32, 1)
        Dp = Pool_(p, "D", [128, 2, 128, GC], F32, 1)
        W4p = Pool_(p, "W4", [64, 2, 2 * GC], BF16, 2)
        Hp = Pool_(p, "Hd", [64, 16, 256], BF16, 2)
        Kt = p.sb("Kt", [128, 2, GC, 2, 128], BF16); bKt = p.buf()
        Kr = p.sb("Kr", [128, 2, 128, 2, GC], BF16); bKr = [p.buf(), p.buf()]
        Sgp = Pool_(p, "Sg", [128, 512], F32, 2)
        A = p.sb("A", [128, GC, 2, 256], BF16); bA = [p.buf() for _ in range(GC // 2)]
        Kf = p.sb("Kf", [128, GC, 2, 256], BF16); bKf = [p.buf() for _ in range(GC // 2)]
        G = p.sb("G", [128, GC, 2, 256], BF16); bG = [p.buf() for _ in range(GC // 2)]
        Bp = p.sb("Bp", [128, 2, 2, GC, 128], BF16); bBp = [p.buf() for _ in range(GC // 4)]
        Zb = p.sb("Zb", [128, GC, 128], BF16); bZb = [p.buf() for _ in range(GC // 4)]
        Z1 = p.sb("Z1", [128, GC, 128], F32); bZ1 = [p.buf() for _ in range(GC // 4)]
        Op = Pool_(p, "O", [128, GC, 128], BF16, 2)
        red = p.sb("red", [128, 2 * GC], F32); bred = p.buf()
        sN = p.sb("sN", [128, 2 * GC], F32); bsN = p.buf()
        psSp = Pool_(p, "psS", [128, 512], F32, 2, psum=True)
        ps1 = Pool_(p, "ps1", [128, 512], F32, 2, psum=True)
        psY = Pool_(p, "psY", [128, 512], F32, 3, psum=True)
        ps4 = Pool_(p, "ps4", [128, 512], F32, 1, psum=True)
        T1p = Pool_(p, "T1", [128, 2, 256], F32, 2)
        T2p = Pool_(p, "T2", [128, 2, 256], F32, 2)
        E1p = Pool_(p, "E1", [128, 4, 128], F32, 2)
        E2p = Pool_(p, "E2", [128, 4, 128], F32, 2)

        def fwd_twiddle(ps, bps, c, bdst):
            t1, b1 = T1p.get()
            t2, b2 = T2p.get()
            pv = ps[:, :].rearrange("p (r k) -> p r k", r=2)
            TT_(p, "dve", t1[:], pv, TW[:, 0, :].unsqueeze(1).to_broadcast([128, 2, 256]), ALU.mult, [bps, bTW], [b1])
            TT_(p, "dve", t2[:], pv, TW[:, 1, :].unsqueeze(1).to_broadcast([128, 2, 256]), ALU.mult, [bps, bTW], [b2])
            TT_(p, "pool", A[:, c, 0, :], t1[:, 0, :], t2[:, 1, :], ALU.add, [b1, b2], [bdst])
            TT_(p, "pool", A[:, c, 1, :], t1[:, 1, :], t2[:, 0, :], ALU.subtract, [b1, b2], [bdst])

        def stage2(pr):
            c0 = 2 * pr
            yr, byr = psY.get()
            yi, byi = psY.get()
            ar = A[:, c0:c0 + 2, 0, :]
            ai = A[:, c0:c0 + 2, 1, :]
            MM(p, yr[:, :], [(F2[:, 0, :], ar), (F2[:, 1, :], ai)], [bF2, bA[pr]], [byr])
            MM(p, yi[:, :], [(F2[:, 0, :], ai), (F2[:, 2, :], ar)], [bF2, bA[pr]], [byi])
            return yr, byr, yi, byi

        for g in range(NG):
          try:
            Ug, bU = Up.get()
            p.dma("sp", Ug[:], dU[g], writes=[bU])
            Dg, bD = Dp.get()
            p.dma("sp", Dg[:], ddec[g], writes=[bD])
            W4, bW4 = W4p.get()
            p.dma("pool", W4[:], dw4[g], writes=[bW4])
            _chk(10)
            for nb in range(8):
                Hd, bHd = Hp.get()
                p.dma("sp", Hd[:], dhid[:, nb * 16:(nb + 1) * 16, :], writes=[bHd])
                for jb in range(2):
                    bank, bbank = psSp.get()
                    fns = []
                    for jj in range(8):
                        j = jb * 8 + jj
                        for h in range(2):
                            k = jj * 2 + h
                            fns.append(lambda e, bank=bank, k=k, j=j, h=h, Hd=Hd, W4=W4: e.matmul(
                                bank[:, k * 32:(k + 1) * 32], Hd[:, j, h * 128:(h + 1) * 128], W4[:, h, :], start=True, stop=True))
                    p.group("pe", fns, [bHd, bW4], [bbank])
                    stg, bstg = Sgp.get()
                    CP(p, "act", stg[:], bank[:, :], [bbank], [bstg])
                    for jj in range(8):
                        j = jb * 8 + jj
                        n2 = nb * 16 + j
                        for h in range(2):
                            k = jj * 2 + h
                            TT_(p, "dve" if h == 0 else "pool", Kr[:, h, n2, :, :],
                                stg[:, k * 32:(k + 1) * 32].rearrange("p (o c) -> p o c", o=2),
                                Dg[:, h, n2, :].unsqueeze(1).to_broadcast([128, 2, GC]), ALU.mult, [bstg, bD], [bKr[h]])
            for o_ in range(2):
                for c_ in range(GC):
                    CP(p, "act", Kt[:, o_, c_, :, :], Kr[:, :, :, o_, c_], bKr, [bKt])
            _chk(1)
            p.op("dve", lambda e: e.tensor_reduce(out=red[:], in_=Kt[:].rearrange("p o c h n -> p (o c) (h n)"),
                                                   axis=AX.X, op=ALU.add, apply_absolute_value=True), [bKt], [bred])
            bank, bbank = psSp.get()
            sl = bank[:, 0:32]
            MM(p, sl, [(ON[:], red[:])], [bON, bred], [bbank])
            TS(p, "dve", sN[:], sl, 1e-6, None, ALU.add, None, [bbank], [bsN])
            p.op("dve", lambda e: e.reciprocal(out=sN[:], in_=sN[:]), [bsN], [bsN])
            _chk(2)
            Og, bO = Op.get()
            for o in range(2):
                for c in range(GC):
                    ps, bps = ps1.get()
                    MM(p, ps[:, :], [(Kt[:, o, c, 0, :], F1[:, 0, :]), (Kt[:, o, c, 1, :], F1[:, 1, :])], [bKt, bF1], [bps])
                    fwd_twiddle(ps, bps, c, bA[c // 2])
                for pr in range(GC // 2):
                    yr, byr, yi, byi = stage2(pr)
                    CP(p, "act", Kf[:, 2 * pr:2 * pr + 2, 0, :], yr[:, :].rearrange("p (c k) -> p c k", c=2), [byr], [bKf[pr]])
                    CP(p, "act", Kf[:, 2 * pr:2 * pr + 2, 1, :], yi[:, :].rearrange("p (c k) -> p c k", c=2), [byi], [bKf[pr]])
                _chk(3)
                if o == 0:
                    for q in range(GC // 4):
                        CP(p, "act", Zb[:, 4 * q:4 * q + 4, :], Ug[:, 0, 4 * q:4 * q + 4, :], [bU], [bZb[q]])
                for c in range(GC):
                    ps, bps = ps1.get()
                    MM(p, ps[:, :], [(Zb[:, c, :], F1[:, 0, :])], [bZb[c // 4], bF1], [bps])
                    fwd_twiddle(ps, bps, c, bA[c // 2])
                for pr in range(GC // 2):
                    c0 = 2 * pr
                    yr, byr, yi, byi = stage2(pr)
                    yrv = yr[:, :].rearrange("p (c k) -> p c k", c=2)
                    yiv = yi[:, :].rearrange("p (c k) -> p c k", c=2)
                    kr = Kf[:, c0:c0 + 2, 0, :]
                    ki = Kf[:, c0:c0 + 2, 1, :]
                    t1, b1 = T1p.get()
                    t2, b2 = T2p.get()
                    TT_(p, "dve", t1[:], yrv, kr, ALU.mult, [byr, bKf[pr]], [b1])
                    TT_(p, "dve", t2[:], yiv, ki, ALU.mult, [byi, bKf[pr]], [b2])
                    TT_(p, "pool", G[:, c0:c0 + 2, 0, :], t1[:], t2[:], ALU.subtract, [b1, b2], [bG[pr]])
                    t1, b1 = T1p.get()
                    t2, b2 = T2p.get()
                    TT_(p, "dve", t1[:], yrv, ki, ALU.mult, [byr, bKf[pr]], [b1])
                    TT_(p, "dve", t2[:], yiv, kr, ALU.mult, [byi, bKf[pr]], [b2])
                    TT_(p, "pool", G[:, c0:c0 + 2, 1, :], t1[:], t2[:], ALU.add, [b1, b2], [bG[pr]])
                _chk(4)
                for c in range(GC):
                    for half in range(2):
                        ps, bps = ps1.get()
                        MM(p, ps[:, 0:256], [(G[:, c, 0, half * 128:(half + 1) * 128], M3[:, 0, :]),
                                             (G[:, c, 1, half * 128:(half + 1) * 128], M3[:, 1, :])], [bG[c // 2], bM3], [bps])
                        t1, b1 = T1p.get()
                        t2, b2 = T2p.get()
                        pv = ps[:, 0:256].rearrange("p (r k) -> p r k", r=2)
                        TT_(p, "dve", t1[:, :, 0:128], pv, TWI[:, half, 0, :].unsqueeze(1).to_broadcast([128, 2, 128]), ALU.mult,
                            [bps, bTWI], [b1])
                        TT_(p, "dve", t2[:, :, 0:128], pv, TWI[:, half, 1, :].unsqueeze(1).to_broadcast([128, 2, 128]), ALU.mult,
                            [bps, bTWI], [b2])
                        TT_(p, "pool", Bp[:, half, 0, c, :], t1[:, 0, 0:128], t2[:, 1, 0:128], ALU.subtract, [b1, b2], [bBp[c // 4]])
                        TT_(p, "pool", Bp[:, half, 1, c, :], t2[:, 0, 0:128], t1[:, 1, 0:128], ALU.add, [b1, b2], [bBp[c // 4]])
                _chk(5)
                for q in range(GC // 4):
                    c0 = 4 * q
                    ps, bps = ps4.get()
                    MM(p, ps[:, :], [(S4[:, half, ri, :], Bp[:, half, ri, c0:c0 + 4, :]) for half in range(2) for ri in range(2)],
                       [bS4, bBp[q]], [bps])
                    e1, be1 = E1p.get()
                    e2, be2 = E2p.get()
                    pv = ps[:, :].rearrange("p (c n) -> p c n", c=4)
                    TT_(p, "dve", e1[:], pv, sN[:, o * GC + c0:o * GC + c0 + 4].unsqueeze(2).to_broadcast([128, 4, 128]), ALU.mult,
                        [bps, bsN], [be1])
                    if o == 0:
                        zc, bzc = Ug[:, 0, c0:c0 + 4, :], bU
                    else:
                        zc, bzc = Z1[:, c0:c0 + 4, :], bZ1[q]
                    TT_(p, "pool", e2[:], zc, SK[:, g, o, c0:c0 + 4].unsqueeze(2).to_broadcast([128, 4, 128]), ALU.mult,
                        [bzc, bSK], [be2])
                    TT_(p, "pool", e2[:], e2[:], e1[:], ALU.add, [be1, be2], [be2])
                    if o == 0:
                        TT_(p, "dve", Z1[:, c0:c0 + 4, :], e2[:], Ug[:, 1, c0:c0 + 4, :], ALU.mult, [be2, bU], [bZ1[q]])
                        CP(p, "act", Zb[:, c0:c0 + 4, :], Z1[:, c0:c0 + 4, :], [bZ1[q]], [bZb[q]])
                    else:
                        TT_(p, "dve", Og[:, c0:c0 + 4, :], e2[:], Ug[:, 2, c0:c0 + 4, :], ALU.mult, [be2, bU], [bO])
            p.dma("sp", dout[g], Og[:], reads=[bO])
          except _Stop:
            break
        p.emit()
    return nc


def run_HC(inp, u, hid, dext, tabs, nc=None):
    if nc is None:
        nc = build_HC()
    L = u.shape[0]
    up = np.zeros((16384, 3072), np.float32)
    up[:L] = u
    u4 = up.reshape(128, 128, 3, 1024)
    d4 = dext.reshape(2, 128, 128, 1024).transpose(1, 0, 2, 3)
    w4 = inp["hy_f_w4"][0].reshape(64, 2, 2, 1024)
    sk = inp["hy_skip"][0]
    maps = []
    for c in range(NCORE):
        ch = slice(c * 128, (c + 1) * 128)
        U = np.ascontiguousarray(u4[:, :, :, ch].reshape(128, 128, 3, NG, GC).transpose(3, 0, 2, 4, 1))
        D = np.ascontiguousarray(d4[:, :, :, ch].reshape(128, 2, 128, NG, GC).transpose(3, 0, 1, 2, 4))
        W = np.ascontiguousarray(w4[:, :, :, ch].reshape(64, 2, 2, NG, GC).transpose(3, 0, 2, 1, 4).reshape(NG, 64, 2, 2 * GC))
        S = np.ascontiguousarray(np.broadcast_to(sk[:, ch].reshape(2, NG, GC).transpose(1, 0, 2)[None], (128, NG, 2, GC)))
        m = {"U": U, "hid": hid, "w4": W, "dec": D, "skip": S}
        m.update(tabs)
        maps.append(m)
    res = run(nc, maps)
    zs = []
    for r in res:
        a = np.asarray(r["z2"])
        zs.append(a.transpose(1, 3, 0, 2).reshape(16384, 128))
    return np.concatenate(zs, axis=1)[:L]


def MMx(p, out, lhsT, rhs, start, stop, reads, writes):
    return p.group("pe", [lambda e: e.matmul(out, lhsT, rhs, start=start, stop=stop)], reads, writes)


def rms_mod_tile(p, C, xs, bx, w, gm, bgm, sh, bsh, obf, bobf, of=None, bof=None, eps=1e-6, D=1024):
    sq, bsq = C.sq.get()
    for kc in range(8):
        ACT(p, sq[:, kc, 0:w], xs(kc), AF.Square, [bx], [bsq])
    ps, bps = C.ps.get()
    MM(p, ps[:, 0:w], [(C.ones[:], sq[:, kc, 0:w]) for kc in range(8)], [bsq, C.bones], [bps])
    rs, brs = C.rs.get()
    ACT(p, rs[:, 0:w], ps[:, 0:w], AF.Sqrt, [bps], [brs], scale=1.0 / D, bias=eps)
    p.op("dve", lambda e: e.reciprocal(out=rs[:, 0:w], in_=rs[:, 0:w]), [brs], [brs])
    for kc in range(8):
        tmp, btmp = C.tmp.get()
        STT(p, tmp[:, 0:w], xs(kc), gm[:, kc:kc + 1], rs[:, 0:w], ALU.mult, ALU.mult, [bx, brs, bgm], [btmp])
        ACT(p, obf(kc), tmp[:, 0:w], AF.Identity, [btmp, bsh], [bobf], bias=sh[:, kc:kc + 1], scale=1.0)
        if of is not None:
            ACT(p, of(kc), tmp[:, 0:w], AF.Identity, [btmp, bsh], [bof], bias=sh[:, kc:kc + 1], scale=1.0)


def build_L3(T, has_bias):
    nc = bass.Bass("TRN2", target_bir_lowering=False)
    dz = nc.dram_tensor("zT", [128, 8, T], BF16, kind="ExternalInput").ap()
    dx = nc.dram_tensor("xT", [128, 8, T], F32, kind="ExternalInput").ap()
    dw = nc.dram_tensor("wout", [1024, 1024], F32, kind="ExternalInput").ap()
    db = nc.dram_tensor("bout", [128, 8], F32, kind="ExternalInput").ap()
    dmd = nc.dram_tensor("md", [128, 2, 6, 8], F32, kind="ExternalInput").ap()
    dg = nc.dram_tensor("g", [128, 8], F32, kind="ExternalInput").ap()
    dwr = nc.dram_tensor("wr", [128, 8, 16], F32, kind="ExternalInput").ap()
    dones = nc.dram_tensor("ones", [128, 128], F32, kind="ExternalInput").ap()
    ox = nc.dram_tensor("x1T", [128, 8, T], F32, kind="ExternalOutput").ap()
    oh = nc.dram_tensor("h2T", [128, 8, T], BF16, kind="ExternalOutput").ap()
    oa = nc.dram_tensor("aff", [T, 16], F32, kind="ExternalOutput").ap()
    tiles = tiles_of(T)
    with ExitStack() as es:
        p = Prog(nc, es)
        C = make_common(p, nc, dones)
        mdt, bmd = LOAD(p, "sp", [128, 2, 6, 8], F32, "md", dmd)
        gt, bg = LOAD(p, "sp", [128, 8], F32, "g", dg)
        bt, bb = LOAD(p, "sp", [128, 8], F32, "bo", db)
        wr, bwr = LOAD(p, "sp", [128, 8, 16], F32, "wr", dwr)
        W = p.sb("W", [128, 8, 1024], BF16); bW = p.buf()
        for kc in range(8):
            p.dma("pool", W[:, kc, :], dw[kc * 128:(kc + 1) * 128, :], writes=[bW])
        X = TT(p, "X", [128, 8, T], F32, len(tiles))
        Z = TT(p, "Z", [128, 8, T], BF16, len(tiles))
        for ti, (c0, w) in enumerate(tiles):
            p.dma("sp", Z.t[:, :, c0:c0 + w], dz[:, :, c0:c0 + w], writes=[Z.b[ti]])
            p.dma("sp", X.t[:, :, c0:c0 + w], dx[:, :, c0:c0 + w], writes=[X.b[ti]])
        gmx, bgmx = emit_gmod(p, "gmx", gt[:], bg, mdt[:, 0, 4, :], bmd)
        gmc, bgmc = emit_gmod(p, "gmc", gt[:], bg, mdt[:, 1, 4, :], bmd)
        Hb = Pool_(p, "Hb", [128, 8, 512], BF16, 2)
        Hf = Pool_(p, "Hf", [128, 8, 512], F32, 2)
        psL = Pool_(p, "psL", [128, 512], F32, 2, psum=True)
        sm = Pool_(p, "sm", [128, 40], F32, 3)
        for ti, (c0, w) in enumerate(tiles):
            cond = 1 if c0 >= TX else 0
            for m in range(8):
                ps, bps = C.ps.get()
                MM(p, ps[:, 0:w], [(W[:, kc, m * 128:(m + 1) * 128], Z.t[:, kc, c0:c0 + w]) for kc in range(8)], [bW, Z.b[ti]], [bps])
                if has_bias:
                    tmp, btmp = C.tmp.get()
                    ACT(p, tmp[:, 0:w], ps[:, 0:w], AF.Identity, [bps, bb], [btmp], bias=bt[:, m:m + 1], scale=1.0)
                    src, bsrc = tmp, btmp
                else:
                    src, bsrc = ps, bps
                STT(p, X.t[:, m, c0:c0 + w], src[:, 0:w], mdt[:, cond, 2, m:m + 1], X.t[:, m, c0:c0 + w], ALU.mult, ALU.add,
                    [bsrc, bmd, X.b[ti]], [X.b[ti]])
            p.dma("sp", ox[:, :, c0:c0 + w], X.t[:, :, c0:c0 + w], reads=[X.b[ti]])
            hb, bhb = Hb.get()
            hf, bhf = Hf.get()
            gm, bgm = (gmc, bgmc) if cond else (gmx, bgmx)
            rms_mod_tile(p, C, lambda kc: X.t[:, kc, c0:c0 + w], X.b[ti], w, gm, bgm, mdt[:, cond, 3, :], bmd,
                         lambda kc: hb[:, kc, 0:w], bhb, lambda kc: hf[:, kc, 0:w], bhf)
            p.dma("sp", oh[:, :, c0:c0 + w], hb[:, :, 0:w], reads=[bhb])
            for j0 in range(0, w, 128):
                tw = min(128, w - j0)
                pl, bpl = psL.get()
                MM(p, pl[0:tw, 0:16], [(hf[:, kc, j0:j0 + tw], wr[:, kc, :]) for kc in range(8)], [bhf, bwr], [bpl])
                s_, bs_ = sm.get()
                p.op("dve", lambda e, s_=s_, pl=pl, tw=tw: e.tensor_reduce(out=s_[0:tw, 32:33], in_=pl[0:tw, 0:16], axis=AX.X, op=ALU.max),
                     [bpl], [bs_])
                TS(p, "dve", s_[0:tw, 33:34], s_[0:tw, 32:33], -1.0, None, ALU.mult, None, [bs_], [bs_])
                ACT(p, s_[0:tw, 0:16], pl[0:tw, 0:16], AF.Exp, [bpl, bs_], [bs_], bias=s_[0:tw, 33:34], scale=1.0,
                    accum_out=s_[0:tw, 34:35])
                p.op("dve", lambda e, s_=s_, tw=tw: e.reciprocal(out=s_[0:tw, 35:36], in_=s_[0:tw, 34:35]), [bs_], [bs_])
                TS(p, "dve", s_[0:tw, 16:32], s_[0:tw, 0:16], s_[0:tw, 35:36], None, ALU.mult, None, [bs_], [bs_])
                p.dma("sp", oa[c0 + j0:c0 + j0 + tw, :], s_[0:tw, 16:32], reads=[bs_])
        p.emit()
    return nc


def run_L3(zsl, xsl, wout, bout, mods, l, g, wr, T, has_bias):
    nc = build_L3(T, has_bias)
    md = mods_fm(mods, l)
    wrl = np.ascontiguousarray(wr.reshape(8, 128, 16).transpose(1, 0, 2))
    bo = vec_fm(bout) if bout is not None else np.zeros((128, 8), np.float32)
    maps = [{"zT": zsl[c], "xT": xsl[c], "wout": wout, "bout": bo, "md": md, "g": vec_fm(g), "wr": wrl, "ones": ONES_F32}
            for c in range(NCORE)]
    res = run(nc, maps)
    return [r["x1T"] for r in res], [r["h2T"] for r in res], [np.asarray(r["aff"]) for r in res]


def emit_bisect(p, name, a, ba, n, cap, Gm, bGm, psb, iters=30):
    st = p.sb(name + "_st", [128, 8], F32); bst = p.buf()
    cmp_ = p.sb(name + "_cmp", [128, n], BF16); bcmp = p.buf()
    p.op("dve", lambda e: e.memset(st[:, 0:1], 0.0), [], [bst])
    p.op("dve", lambda e: e.memset(st[:, 1:2], 1.0), [bst], [bst])
    p.op("dve", lambda e: e.memset(st[:, 2:3], 0.5), [bst], [bst])
    p.op("dve", lambda e: e.memset(st[:, 3:5], 0.0), [bst], [bst])
    for it in range(iters):
        p.op("dve", lambda e: e.tensor_scalar(out=cmp_[:], in0=a, scalar1=st[:, 2:3], scalar2=0.0, op0=ALU.is_ge, op1=ALU.add,
                                               accum_out=st[:, 3:4]), [ba, bst], [bcmp, bst])
        ps, bps = psb
        MM(p, ps[:, 0:2], [(Gm, st[:, 3:5])], [bGm, bst], [bps])
        TS(p, "dve", st[:, 5:6], ps[:, 0:1], float(cap) - 0.5, None, ALU.is_ge, None, [bps, bst], [bst])
        TT_(p, "dve", st[:, 6:7], st[:, 2:3], st[:, 0:1], ALU.subtract, [bst], [bst])
        STT(p, st[:, 0:1], st[:, 6:7], st[:, 5:6], st[:, 0:1], ALU.mult, ALU.add, [bst], [bst])
        TT_(p, "dve", st[:, 6:7], st[:, 1:2], st[:, 2:3], ALU.subtract, [bst], [bst])
        STT(p, st[:, 1:2], st[:, 6:7], st[:, 5:6], st[:, 2:3], ALU.mult, ALU.add, [bst], [bst])
        TT_(p, "dve", st[:, 6:7], st[:, 0:1], st[:, 1:2], ALU.add, [bst], [bst])
        TS(p, "dve", st[:, 2:3], st[:, 6:7], 0.5, None, ALU.mult, None, [bst], [bst])
    return st[:, 0:1], bst


def moe_consts():
    Gm = np.zeros((128, 128), np.float32)
    for k in range(128):
        Gm[k, (k // 8) * 8:(k // 8) * 8 + 8] = 1.0
    Sel = np.zeros((128, 16, 128), np.float32)
    for e in range(16):
        Sel[e * 8, e, :] = 1.0
    return Gm, Sel


def build_L4(T, has_ctx):
    nc = bass.Bass("TRN2", target_bir_lowering=False)
    dx = nc.dram_tensor("xT", [128, 8, T], F32, kind="ExternalInput").ap()
    dh = nc.dram_tensor("hT", [128, 8, T], BF16, kind="ExternalInput").ap()
    daf = nc.dram_tensor("afull", [128, 2048], F32, kind="ExternalInput").ap()
    dac = nc.dram_tensor("acfull", [128, 32], F32, kind="ExternalInput").ap()
    dao = nc.dram_tensor("aown", [128, T], F32, kind="ExternalInput").ap()
    dmd = nc.dram_tensor("md", [128, 2, 6, 8], F32, kind="ExternalInput").ap()
    dGm = nc.dram_tensor("Gm", [128, 128], F32, kind="ExternalInput").ap()
    dSel = nc.dram_tensor("Sel", [128, 16, 128], F32, kind="ExternalInput").ap()
    dwg = nc.dram_tensor("wg", [16, 1024, 1024], F32, kind="ExternalInput").ap()
    dwu = nc.dram_tensor("wu", [16, 1024, 1024], F32, kind="ExternalInput").ap()
    dwd = nc.dram_tensor("wd", [16, 1024, 1024], F32, kind="ExternalInput").ap()
    ox = nc.dram_tensor("x2T", [128, 8, T], F32, kind="ExternalOutput").ap()
    tiles = tiles_of(T)
    with ExitStack() as es:
        p = Prog(nc, es)
        mdt, bmd = LOAD(p, "sp", [128, 2, 6, 8], F32, "md", dmd)
        Gm, bGm = LOAD(p, "sp", [128, 128], F32, "Gm", dGm)
        Sel, bSel = LOAD(p, "sp", [128, 16, 128], F32, "Sel", dSel)
        af, baf = LOAD(p, "sp", [128, 2048], F32, "af", daf)
        ao, bao = LOAD(p, "sp", [128, T], F32, "ao", dao)
        X = TT(p, "X", [128, 8, T], F32, len(tiles))
        H = TT(p, "H", [128, 8, T], BF16, len(tiles))
        for ti, (c0, w) in enumerate(tiles):
            p.dma("sp", H.t[:, :, c0:c0 + w], dh[:, :, c0:c0 + w], writes=[H.b[ti]])
            p.dma("sp", X.t[:, :, c0:c0 + w], dx[:, :, c0:c0 + w], writes=[X.b[ti]])
        psb = Pool_(p, "psb", [128, 512], F32, 1, psum=True)
        tau, btau = emit_bisect(p, "bx", af[:], baf, 2048, 2048, Gm[:], bGm, (psb.ts[0], psb.bs[0]))
        gw, bgw = ao, bao
        STT(p, gw[:, 0:TX], ao[:, 0:TX], tau, ao[:, 0:TX], ALU.is_ge, ALU.mult, [bao, btau], [bgw])
        if has_ctx:
            ac, bac = LOAD(p, "sp", [128, 32], F32, "ac", dac)
            tauc, btauc = emit_bisect(p, "bc", ac[:], bac, 32, 32, Gm[:], bGm, (psb.ts[0], psb.bs[0]))
            STT(p, gw[:, TX:T], ao[:, TX:T], tauc, ao[:, TX:T], ALU.is_ge, ALU.mult, [bao, btauc, bgw], [bgw])
        Wp = Pool_(p, "W", [128, 8, 1024], BF16, 4)
        Ap = Pool_(p, "A", [128, 8, 512], BF16, 1)
        sgp = Pool_(p, "sg", [128, 512], BF16, 2)
        atp = Pool_(p, "at", [128, 512], BF16, 2)
        gbp = Pool_(p, "gb", [128, 512], BF16, 2)
        psG = Pool_(p, "psG", [128, 512], F32, 4, psum=True)
        psD = Pool_(p, "psD", [128, 512], F32, 2, psum=True)
        psB = Pool_(p, "psB", [128, 512], F32, 1, psum=True)

        def loadw(src):
            wt, bw = Wp.get()
            for kc in range(8):
                p.dma("pool", wt[:, kc, :], src[kc * 128:(kc + 1) * 128, :], writes=[bw])
            return wt, bw

        for e in range(16):
            Wg, bWg = loadw(dwg[e])
            Wu, bWu = loadw(dwu[e])
            Wd, bWd = loadw(dwd[e])
            for ti, (c0, w) in enumerate(tiles):
                cond = 1 if c0 >= TX else 0
                pb, bpb = psB.get()
                MM(p, pb[:, 0:w], [(Sel[:, e, :], gw[:, c0:c0 + w])], [bSel, bgw], [bpb])
                gb, bgb = gbp.get()
                CP(p, "act", gb[:, 0:w], pb[:, 0:w], [bpb], [bgb])
                A_, bA = Ap.get()
                for fc in range(8):
                    pg, bpg = psG.get()
                    MM(p, pg[:, 0:w], [(Wg[:, kc, fc * 128:(fc + 1) * 128], H.t[:, kc, c0:c0 + w]) for kc in range(8)],
                       [bWg, H.b[ti]], [bpg])
                    sg, bsg = sgp.get()
                    ACT(p, sg[:, 0:w], pg[:, 0:w], AF.Silu, [bpg], [bsg])
                    pu, bpu = psG.get()
                    MM(p, pu[:, 0:w], [(Wu[:, kc, fc * 128:(fc + 1) * 128], H.t[:, kc, c0:c0 + w]) for kc in range(8)],
                       [bWu, H.b[ti]], [bpu])
                    at, bat = atp.get()
                    TT_(p, "dve", at[:, 0:w], pu[:, 0:w], sg[:, 0:w], ALU.mult, [bpu, bsg], [bat])
                    TT_(p, "dve", A_[:, fc, 0:w], at[:, 0:w], gb[:, 0:w], ALU.mult, [bat, bgb], [bA])
                for dc in range(8):
                    pd, bpd = psD.get()
                    MM(p, pd[:, 0:w], [(Wd[:, fc, dc * 128:(dc + 1) * 128], A_[:, fc, 0:w]) for fc in range(8)], [bWd, bA], [bpd])
                    STT(p, X.t[:, dc, c0:c0 + w], pd[:, 0:w], mdt[:, cond, 5, dc:dc + 1], X.t[:, dc, c0:c0 + w], ALU.mult, ALU.add,
                        [bpd, bmd, X.b[ti]], [X.b[ti]])
        for ti, (c0, w) in enumerate(tiles):
            p.dma("sp", ox[:, :, c0:c0 + w], X.t[:, :, c0:c0 + w], reads=[X.b[ti]])
        p.emit()
    return nc


def grp_layout(aT):
    n = aT.shape[1]
    return np.ascontiguousarray(aT.reshape(16, 8, n // 8).reshape(128, n // 8))


def run_L4(xsl, hsl, affs, mods, l, wg, wu, wd, T, has_ctx, nc=None):
    if nc is None:
        nc = build_L4(T, has_ctx)
    md = mods_fm(mods, l)
    Gm, Sel = moe_consts()
    ax = np.concatenate([a[:TX] for a in affs], 0)
    afull = grp_layout(np.ascontiguousarray(ax.T))
    if has_ctx:
        ac = np.concatenate([a[TX:] for a in affs], 0)
        acfull = grp_layout(np.ascontiguousarray(ac.T))
    else:
        acfull = np.zeros((128, 32), np.float32)
    maps = []
    for c in range(NCORE):
        aown = np.ascontiguousarray(np.repeat(affs[c].T, 8, axis=0))
        maps.append({"xT": xsl[c], "hT": hsl[c], "afull": afull, "acfull": acfull, "aown": aown, "md": md, "Gm": Gm, "Sel": Sel,
                     "wg": wg, "wu": wu, "wd": wd})
    res = run(nc, maps)
    return [r["x2T"] for r in res]


def rope_consts(core):
    t = core * TX + np.arange(TX)
    row = (t // 64).astype(np.float32)
    col = (t % 64).astype(np.float32)
    inv = (10000.0 ** (-np.arange(0, 32, 2, dtype=np.float32) / 32)).astype(np.float32)
    C = np.zeros((128, TX), np.float32)
    S = np.zeros((128, TX), np.float32)
    for p_ in range(128):
        d = p_ % 64
        pos = row if d < 32 else col
        i = (d % 32) % 16
        ang = pos * inv[i]
        C[p_] = np.cos(ang)
        S[p_] = np.sin(ang)
    PT = np.zeros((128, 128), np.float32)
    for m in range(128):
        if (m % 32) < 16:
            PT[m + 16, m] = -1.0
        else:
            PT[m - 16, m] = 1.0
    BD = np.zeros((128, 128), np.float32)
    BD[:64, :64] = 1.0
    BD[64:, 64:] = 1.0
    return C, S, PT, BD


def build_L5(T):
    nc = bass.Bass("TRN2", target_bir_lowering=False)
    dx = nc.dram_tensor("xT", [128, 8, T], F32, kind="ExternalInput").ap()
    dmd = nc.dram_tensor("md", [128, 2, 6, 8], F32, kind="ExternalInput").ap()
    dg = nc.dram_tensor("g", [128, 8], F32, kind="ExternalInput").ap()
    dw = nc.dram_tensor("wqkv", [1024, 3072], F32, kind="ExternalInput").ap()
    dqk = nc.dram_tensor("qkg", [128, 2], F32, kind="ExternalInput").ap()
    dC = nc.dram_tensor("ropeC", [128, TX], F32, kind="ExternalInput").ap()
    dS = nc.dram_tensor("ropeS", [128, TX], F32, kind="ExternalInput").ap()
    dPT = nc.dram_tensor("PT", [128, 128], F32, kind="ExternalInput").ap()
    dBD = nc.dram_tensor("BD", [128, 128], F32, kind="ExternalInput").ap()
    dones = nc.dram_tensor("ones", [128, 128], F32, kind="ExternalInput").ap()
    oq = nc.dram_tensor("qT", [128, 8, TX], BF16, kind="ExternalOutput").ap()
    ok = nc.dram_tensor("kT", [128, 8, T], BF16, kind="ExternalOutput").ap()
    ov = nc.dram_tensor("v", [T, 1024], BF16, kind="ExternalOutput").ap()
    tiles = tiles_of(T)
    with ExitStack() as es:
        p = Prog(nc, es)
        C = make_common(p, nc, dones)
        mdt, bmd = LOAD(p, "sp", [128, 2, 6, 8], F32, "md", dmd)
        gt, bg = LOAD(p, "sp", [128, 8], F32, "g", dg)
        qkg, bqkg = LOAD(p, "sp", [128, 2], F32, "qkg", dqk)
        RC, bRC = LOAD(p, "sp", [128, TX], F32, "RC", dC)
        RS, bRS = LOAD(p, "sp", [128, TX], F32, "RS", dS)
        PT, bPT = LOAD(p, "sp", [128, 128], F32, "PT", dPT)
        BD, bBD = LOAD(p, "pool", [128, 128], BF16, "BD", dBD)
        X = TT(p, "X", [128, 8, T], F32, len(tiles))
        H = TT(p, "H", [128, 8, T], BF16, len(tiles))
        for ti, (c0, w) in enumerate(tiles):
            p.dma("sp", X.t[:, :, c0:c0 + w], dx[:, :, c0:c0 + w], writes=[X.b[ti]])
        gmx, bgmx = emit_gmod(p, "gmx", gt[:], bg, mdt[:, 0, 1, :], bmd)
        gmc, bgmc = emit_gmod(p, "gmc", gt[:], bg, mdt[:, 1, 1, :], bmd)
        for ti, (c0, w) in enumerate(tiles):
            cond = 1 if c0 >= TX else 0
            gm, bgm = (gmc, bgmc) if cond else (gmx, bgmx)
            rms_mod_tile(p, C, lambda kc: X.t[:, kc, c0:c0 + w], X.b[ti], w, gm, bgm, mdt[:, cond, 0, :], bmd,
                         lambda kc: H.t[:, kc, c0:c0 + w], H.b[ti])
        Wm = Pool_(p, "Wm", [128, 8, 128], BF16, 3)
        qr = Pool_(p, "qr", [128, 512], F32, 4)
        sqp = Pool_(p, "sq1", [128, 512], BF16, 4)
        qn = Pool_(p, "qn", [128, 512], F32, 4)
        t1p = Pool_(p, "t1", [128, 512], F32, 4)
        t2p = Pool_(p, "t2", [128, 512], F32, 4)
        ob = Pool_(p, "ob", [128, 512], BF16, 3)
        ps2 = Pool_(p, "ps2", [128, 512], F32, 4, psum=True)
        for m in range(16):
            isq = m < 8
            wt, bw = Wm.get()
            p.dma("pool", wt[:], dw[:, m * 128:(m + 1) * 128].rearrange("(kc p) n -> p kc n", p=128), writes=[bw])
            gcol = qkg[:, 0:1] if isq else qkg[:, 1:2]
            for ti, (c0, w) in enumerate(tiles):
                isctx = c0 >= TX
                if isq and isctx:
                    continue
                ps, bps = ps2.get()
                MM(p, ps[:, 0:w], [(wt[:, kc, :], H.t[:, kc, c0:c0 + w]) for kc in range(8)], [bw, H.b[ti]], [bps])
                q_, bq_ = qr.get()
                CP(p, "act", q_[:, 0:w], ps[:, 0:w], [bps], [bq_])
                sq, bsq = sqp.get()
                ACT(p, sq[:, 0:w], ps[:, 0:w], AF.Square, [bps], [bsq])
                pss, bpss = ps2.get()
                MM(p, pss[:, 0:w], [(BD[:], sq[:, 0:w])], [bBD, bsq], [bpss])
                rs, brs = C.rs.get()
                ACT(p, rs[:, 0:w], pss[:, 0:w], AF.Sqrt, [bpss], [brs], scale=1.0 / 64, bias=1e-6)
                p.op("dve", lambda e, rs=rs, w=w: e.reciprocal(out=rs[:, 0:w], in_=rs[:, 0:w]), [brs], [brs])
                n_, bn_ = qn.get()
                STT(p, n_[:, 0:w], q_[:, 0:w], gcol, rs[:, 0:w], ALU.mult, ALU.mult, [bq_, bqkg, brs], [bn_])
                o_, bo_ = ob.get()
                if not isctx:
                    pr, bpr = ps2.get()
                    MM(p, pr[:, 0:w], [(PT[:], n_[:, 0:w])], [bPT, bn_], [bpr])
                    t1, b1 = t1p.get()
                    t2, b2 = t2p.get()
                    TT_(p, "pool", t1[:, 0:w], n_[:, 0:w], RC[:, c0:c0 + w], ALU.mult, [bn_, bRC], [b1])
                    TT_(p, "dve", t2[:, 0:w], pr[:, 0:w], RS[:, c0:c0 + w], ALU.mult, [bpr, bRS], [b2])
                    TT_(p, "pool", o_[:, 0:w], t1[:, 0:w], t2[:, 0:w], ALU.add, [b1, b2], [bo_])
                else:
                    CP(p, "act", o_[:, 0:w], n_[:, 0:w], [bn_], [bo_])
                if isq:
                    p.dma("sp", oq[:, m, c0:c0 + w], o_[:, 0:w], reads=[bo_])
                else:
                    p.dma("sp", ok[:, m - 8, c0:c0 + w], o_[:, 0:w], reads=[bo_])
        Wv = p.sb("Wv", [128, 8, 1024], BF16); bWv = p.buf()
        for kc in range(8):
            p.dma("pool", Wv[:, kc, :], dw[kc * 128:(kc + 1) * 128, 2048:3072], writes=[bWv])
        vb = Pool_(p, "vb", [128, 512], BF16, 3)
        for ti, (c0, w) in enumerate(tiles):
            for j0 in range(0, w, 128):
                tw = min(128, w - j0)
                for hh in range(2):
                    ps, bps = ps2.get()
                    MM(p, ps[0:tw, :], [(H.t[:, kc, c0 + j0:c0 + j0 + tw], Wv[:, kc, hh * 512:(hh + 1) * 512]) for kc in range(8)],
                       [H.b[ti], bWv], [bps])
                    v_, bv_ = vb.get()
                    CP(p, "act", v_[0:tw, :], ps[0:tw, :], [bps], [bv_])
                    p.dma("sp", ov[c0 + j0:c0 + j0 + tw, hh * 512:(hh + 1) * 512], v_[0:tw, :], reads=[bv_])
        p.emit()
    return nc


def run_L5(inp, xsl, mods, T):
    nc = build_L5(T)
    md = mods_fm(mods, 1)
    qkg = np.ascontiguousarray(np.stack([np.tile(inp["da_q_norm"][0], 2), np.tile(inp["da_k_norm"][0], 2)], axis=1))
    maps = []
    for c in range(NCORE):
        C_, S_, PT, BD = rope_consts(c)
        maps.append({"xT": xsl[c], "md": md, "g": vec_fm(inp["norm_mix"][1]), "wqkv": inp["da_w_qkv"][0], "qkg": qkg,
                     "ropeC": C_, "ropeS": S_, "PT": PT, "BD": BD, "ones": ONES_F32})
    res = run(nc, maps)
    return [np.asarray(r["qT"]) for r in res], [np.asarray(r["kT"]) for r in res], [np.asarray(r["v"]) for r in res]


LAM_INIT = 0.8 - 0.6 * float(np.exp(-0.3 * 1))
NKEY = 16384 + 256
NKC = NKEY // 128


def build_L6():
    nc = bass.Bass("TRN2", target_bir_lowering=False)
    dq = nc.dram_tensor("qT", [128, 8, TX], BF16, kind="ExternalInput").ap()
    dk = nc.dram_tensor("kT", [8, 128, NKEY], BF16, kind="ExternalInput").ap()
    dv = nc.dram_tensor("v", [8, 128, NKC, 128], BF16, kind="ExternalInput").ap()
    dlam = nc.dram_tensor("lamv", [128, 4, 64], F32, kind="ExternalInput").ap()
    dgn = nc.dram_tensor("gains", [128, 2, 64], F32, kind="ExternalInput").ap()
    dsub = nc.dram_tensor("subln", [128, 1], F32, kind="ExternalInput").ap()
    dones = nc.dram_tensor("ones", [128, 128], F32, kind="ExternalInput").ap()
    oo_ = nc.dram_tensor("oT", [128, 8, TX], BF16, kind="ExternalOutput").ap()
    with ExitStack() as es:
        p = Prog(nc, es)
        ones, bones = LOAD(p, "pool", [128, 128], BF16, "ones", dones)
        lamv, blamv = LOAD(p, "sp", [128, 4, 64], F32, "lamv", dlam)
        gn, bgn = LOAD(p, "sp", [128, 2, 64], F32, "gn", dgn)
        st = p.sb("st", [128, 16], F32); bst = p.buf()
        p.dma("sp", st[:, 0:1], dsub, writes=[bst])
        sc = p.sb("scr", [128, 64], F32); bsc = p.buf()
        for i in range(2):
            TT_(p, "dve", sc[:], lamv[:, 2 * i, :], lamv[:, 2 * i + 1, :], ALU.mult, [blamv, bsc], [bsc])
            p.op("dve", lambda e, i=i: e.tensor_reduce(out=st[:, 1 + i:2 + i], in_=sc[:], axis=AX.X, op=ALU.add), [bsc, bst], [bst])
        ACT(p, st[:, 1:3], st[:, 1:3], AF.Exp, [bst], [bst])
        TT_(p, "dve", st[:, 3:4], st[:, 2:3], st[:, 1:2], ALU.subtract, [bst], [bst])
        TS(p, "dve", st[:, 3:4], st[:, 3:4], -LAM_INIT, None, ALU.add, None, [bst], [bst])
        for i in range(2):
            p.op("dve", lambda e, i=i: e.tensor_reduce(out=st[:, 4 + i:5 + i], in_=gn[:, i, :], axis=AX.X, op=ALU.max,
                                                       apply_absolute_value=True), [bgn, bst], [bst])
        TT_(p, "dve", st[:, 6:7], st[:, 4:5], st[:, 5:6], ALU.mult, [bst], [bst])
        TS(p, "dve", st[:, 6:7], st[:, 6:7], -8.0, None, ALU.mult, None, [bst], [bst])
        TS(p, "dve", st[:, 7:8], st[:, 0:1], 1.0 - LAM_INIT, None, ALU.mult, None, [bst], [bst])
        Q, bQ = LOAD(p, "sp", [128, 8, TX], BF16, "Q", dq)
        ones32, bones32 = LOAD(p, "sp", [128, 128], F32, "ones32", dones)
        Kp = Pool_(p, "K", [128, NKEY], BF16, 2)
        Vp = Pool_(p, "V", [128, NKC, 128], BF16, 2)
        Pp = Pool_(p, "P", [128, 1024], BF16, 3)
        psS = Pool_(p, "psS", [128, 1024], F32, 2, psum=True)
        psO = [p.ps(f"psO{i}", [128, 512], F32) for i in range(3)]
        bO = [p.buf() for _ in range(3)]
        psD = Pool_(p, "psD", [128, 512], F32, 1, psum=True)
        Dacc = p.sb("Dacc", [128, 512], F32)
        bDa = p.buf()
        ep = Pool_(p, "ep", [128, 512], F32, 6)
        sqp = Pool_(p, "sqe", [128, 512], BF16, 2)
        obp = Pool_(p, "obe", [128, 512], BF16, 2)
        for hp in range(8):
            Kh, bK = Kp.get()
            p.dma("sp", Kh[:], dk[hp], writes=[bK])
            Vh, bV = Vp.get()
            p.dma("sp", Vh[:], dv[hp], writes=[bV])
            for qt in range(TX // 512):
                q0 = qt * 512

                def S(kc):
                    ps, bps = psS.get()
                    for s_ in range(2):
                        lo = 64 * s_
                        MM(p, ps[:, 512 * s_:512 * s_ + 512],
                           [(Kh[lo:lo + 64, kc * 128:(kc + 1) * 128], Q[lo:lo + 64, hp, q0:q0 + 512])], [bK, bQ], [bps])
                    return ps, bps

                cur = S(0)
                for kc in range(NKC):
                    nxt = S(kc + 1) if kc + 1 < NKC else None
                    ps, bps = cur
                    P_, bP = Pp.get()
                    ACT(p, P_[:], ps[:, :], AF.Exp, [bps, bst], [bP], scale=0.125, bias=st[:, 6:7])
                    for s_ in range(2):
                        MMx(p, psO[s_][:, :], Vh[:, kc, :], P_[:, 512 * s_:512 * s_ + 512], kc == 0, kc == NKC - 1, [bV, bP], [bO[s_]])
                    MMx(p, psO[2][:, :], ones[:], P_[:, 0:512], kc == 0, kc == NKC - 1, [bones, bP], [bO[2]])
                    if kc == 0:
                        CP(p, "dve", Dacc[:], P_[:, 512:1024], [bP], [bDa])
                    else:
                        TT_(p, "dve", Dacc[:], Dacc[:], P_[:, 512:1024], ALU.add, [bP, bDa], [bDa])
                    cur = nxt
                ts_ = []
                for s_ in range(2):
                    if s_ == 0:
                        pd, bpd = psO[2], bO[2]
                    else:
                        pd, bpd = psD.get()
                        MM(p, pd[:, :], [(ones32[:], Dacc[:])], [bones32, bDa], [bpd])
                    r_, br_ = ep.get()
                    p.op("dve", lambda e, r_=r_, pd=pd: e.reciprocal(out=r_[:], in_=pd[:, :]), [bpd], [br_])
                    t_, bt_ = ep.get()
                    TT_(p, "dve", t_[:], psO[s_][:, :], r_[:], ALU.mult, [bO[s_], br_], [bt_])
                    ts_.append((t_, bt_))
                (t0, bt0), (t1, bt1) = ts_
                o_, bo_ = ep.get()
                STT(p, o_[:], t1[:], st[:, 3:4], t0[:], ALU.mult, ALU.add, [bt1, bt0, bst], [bo_])
                sq, bsq = sqp.get()
                ACT(p, sq[:], o_[:], AF.Square, [bo_], [bsq])
                pss, bpss = psD.get()
                MM(p, pss[:, :], [(ones[:], sq[:])], [bones, bsq], [bpss])
                rs, brs = ep.get()
                ACT(p, rs[:], pss[:, :], AF.Sqrt, [bpss], [brs], scale=1.0 / 128, bias=1e-5)
                p.op("dve", lambda e, rs=rs: e.reciprocal(out=rs[:], in_=rs[:]), [brs], [brs])
                ob, bob = obp.get()
                STT(p, ob[:], o_[:], st[:, 7:8], rs[:], ALU.mult, ALU.mult, [bo_, bst, brs], [bob])
                p.dma("sp", oo_[:, hp, q0:q0 + 512], ob[:], reads=[bob])
        p.emit()
    return nc


def run_L6(inp, q, k, v):
    nc = build_L6()
    kx = np.concatenate([a[:, :, :TX] for a in k], axis=2)
    kc = np.concatenate([a[:, :, TX:] for a in k], axis=2)
    kall = np.ascontiguousarray(np.concatenate([kc, kx], axis=2).transpose(1, 0, 2))
    vx = np.concatenate([a[:TX] for a in v], axis=0)
    vc = np.concatenate([a[TX:] for a in v], axis=0)
    vall = np.concatenate([vc, vx], axis=0).reshape(NKC, 128, 8, 128)
    vall = np.ascontiguousarray(vall.transpose(2, 1, 0, 3))
    lamv = np.ascontiguousarray(np.broadcast_to(np.stack([inp["da_lam_q1"][0], inp["da_lam_k1"][0], inp["da_lam_q2"][0],
                                                          inp["da_lam_k2"][0]])[None], (128, 4, 64)))
    gains = np.ascontiguousarray(np.broadcast_to(np.stack([inp["da_q_norm"][0], inp["da_k_norm"][0]])[None], (128, 2, 64)))
    sub = np.ascontiguousarray(inp["da_subln"][0].reshape(128, 1))
    maps = [{"qT": q[c], "kT": kall, "v": vall, "lamv": lamv, "gains": gains, "subln": sub, "ones": ONES_F32} for c in range(NCORE)]
    res = run(nc, maps)
    return [np.asarray(r["oT"]) for r in res]


def ctx_pos_tables():
    L, N = 256, 512
    t = np.linspace(0.0, 1.0, L, dtype=np.float32)[:, None]
    w = (2.0 * np.float32(np.pi) * np.arange(L, dtype=np.float32)[:, None] / np.float32(L)).astype(np.float32)
    bands = np.linspace(1e-4, 15, 16, dtype=np.float32)[None]
    z = np.concatenate([t, np.cos(w * bands), -np.sin(w * bands)], axis=-1).astype(np.float32)
    maxd = np.log(1e-2) / 0.3
    mind = np.log(1e-2) / 1.5
    deltas = np.linspace(mind, maxd, 1024, dtype=np.float32)
    decay = np.exp(-t * np.abs(deltas)).astype(np.float32)
    zext = np.zeros((N, 33), np.float32)
    dext = np.zeros((N, 1024), np.float32)
    zext[:L] = z
    dext[:L] = decay
    j = np.arange(1, L)
    zext[N - j] = z[j]
    dext[N - j] = decay[j]
    return zext, dext


def hcc_tables():
    import ml_dtypes
    bf = ml_dtypes.bfloat16
    n = (np.arange(4)[None, :, None] * 128 + np.arange(128)[:, None, None]).astype(np.float64)
    k = np.arange(512)[None, None, :].astype(np.float64)
    th = 2 * np.pi * n * k / 512
    return {"COS": np.cos(th).astype(bf), "SIN": np.sin(th).astype(bf), "NSIN": (-np.sin(th)).astype(bf),
            "ones32": np.ones((128, 128), np.float32)}


def build_HCc():
    nc = bass.Bass("TRN2", target_bir_lowering=False)
    dU = nc.dram_tensor("U", [128, 2, 3, 128], F32, kind="ExternalInput").ap()
    dhid = nc.dram_tensor("hid", [64, 512], BF16, kind="ExternalInput").ap()
    dw4 = nc.dram_tensor("w4", [64, 2, 256], F32, kind="ExternalInput").ap()
    ddec = nc.dram_tensor("dec", [128, 4, 128], F32, kind="ExternalInput").ap()
    dskip = nc.dram_tensor("skip", [128, 2, 128], F32, kind="ExternalInput").ap()
    dC = nc.dram_tensor("COS", [128, 4, 512], BF16, kind="ExternalInput").ap()
    dS = nc.dram_tensor("SIN", [128, 4, 512], BF16, kind="ExternalInput").ap()
    dNS = nc.dram_tensor("NSIN", [128, 4, 512], BF16, kind="ExternalInput").ap()
    dones = nc.dram_tensor("ones32", [128, 128], F32, kind="ExternalInput").ap()
    dout = nc.dram_tensor("z2", [128, 2, 128], BF16, kind="ExternalOutput").ap()
    with ExitStack() as es:
        p = Prog(nc, es)
        U, bU = LOAD(p, "sp", [128, 2, 3, 128], F32, "U", dU)
        Hd, bHd = LOAD(p, "sp", [64, 512], BF16, "Hd", dhid)
        W4, bW4 = LOAD(p, "pool", [64, 2, 256], BF16, "W4", dw4)
        Dc, bDc = LOAD(p, "sp", [128, 4, 128], F32, "Dc", ddec)
        SK, bSK = LOAD(p, "sp", [128, 2, 128], F32, "SK", dskip)
        COS, bC = LOAD(p, "sp", [128, 4, 512], BF16, "COS", dC)
        SIN, bS = LOAD(p, "sp", [128, 4, 512], BF16, "SIN", dS)
        NSIN, bNS = LOAD(p, "sp", [128, 4, 512], BF16, "NSIN", dNS)
        ON, bON = LOAD(p, "sp", [128, 128], F32, "ON", dones)
        psp = Pool_(p, "ps", [128, 512], F32, 6, psum=True)
        Kt = p.sb("Kt", [128, 4, 256], F32); bKt = p.buf()
        Kb = p.sb("Kb", [128, 4, 256], BF16); bKb = p.buf()
        for j in range(4):
            ps, bps = psp.get()
            MM(p, ps[:, 0:256], [(Hd[:, j * 128:(j + 1) * 128], W4[:, j // 2, :])], [bHd, bW4], [bps])
            TT_(p, "dve", Kt[:, j, :].rearrange("p (o c) -> p o c", o=2), ps[:, 0:256].rearrange("p (o c) -> p o c", o=2),
                Dc[:, j, :].unsqueeze(1).to_broadcast([128, 2, 128]), ALU.mult, [bps, bDc], [bKt])
        CP(p, "act", Kb[:], Kt[:], [bKt], [bKb])
        red = p.sb("red", [128, 256], F32); bred = p.buf()
        p.op("dve", lambda e: e.tensor_reduce(out=red[:], in_=Kt[:].rearrange("p j oc -> p oc j"), axis=AX.X, op=ALU.add,
                                               apply_absolute_value=True), [bKt], [bred])
        sN = p.sb("sN", [128, 256], F32); bsN = p.buf()
        ps, bps = psp.get()
        MM(p, ps[:, 0:256], [(ON[:], red[:])], [bON, bred], [bps])
        TS(p, "dve", sN[:], ps[:, 0:256], 1e-6, None, ALU.add, None, [bps], [bsN])
        p.op("dve", lambda e: e.reciprocal(out=sN[:], in_=sN[:]), [bsN], [bsN])
        TS(p, "dve", sN[:], sN[:], 1.0 / 512, None, ALU.mult, None, [bsN], [bsN])
        Kf = p.sb("Kf", [128, 4, 2, 256], F32); bKf = p.buf()
        for kc in range(4):
            for ri, TB, bTB in ((0, COS, bC), (1, NSIN, bNS)):
                ps, bps = psp.get()
                MM(p, ps[:, 0:256], [(TB[:, j, kc * 128:(kc + 1) * 128], Kb[:, j, :]) for j in range(4)], [bTB, bKb], [bps])
                CP(p, "act", Kf[:, kc, ri, :], ps[:, 0:256], [bps], [bKf])
        Zf = p.sb("Zf", [128, 2, 128], F32); bZf = p.buf()
        Zb = p.sb("Zb", [128, 2, 128], BF16); bZb = p.buf()
        G = p.sb("G", [128, 4, 2, 128], BF16); bG = p.buf()
        O = p.sb("O", [128, 2, 128], BF16); bO = p.buf()
        tp = Pool_(p, "t", [128, 128], F32, 4)
        CP(p, "act", Zf[:], U[:, :, 0, :], [bU], [bZf])
        for o in range(2):
            CP(p, "act", Zb[:], Zf[:], [bZf], [bZb])
            for kc in range(4):
                pr, bpr = psp.get()
                MM(p, pr[:, 0:128], [(COS[:, j, kc * 128:(kc + 1) * 128], Zb[:, j, :]) for j in range(2)], [bC, bZb], [bpr])
                pi, bpi = psp.get()
                MM(p, pi[:, 0:128], [(NSIN[:, j, kc * 128:(kc + 1) * 128], Zb[:, j, :]) for j in range(2)], [bNS, bZb], [bpi])
                kr = Kf[:, kc, 0, o * 128:(o + 1) * 128]
                ki = Kf[:, kc, 1, o * 128:(o + 1) * 128]
                t1, b1 = tp.get()
                t2, b2 = tp.get()
                TT_(p, "dve", t1[:], pr[:, 0:128], kr, ALU.mult, [bpr, bKf], [b1])
                TT_(p, "dve", t2[:], pi[:, 0:128], ki, ALU.mult, [bpi, bKf], [b2])
                TT_(p, "pool", G[:, kc, 0, :], t1[:], t2[:], ALU.subtract, [b1, b2], [bG])
                t1, b1 = tp.get()
                t2, b2 = tp.get()
                TT_(p, "dve", t1[:], pr[:, 0:128], ki, ALU.mult, [bpr, bKf], [b1])
                TT_(p, "dve", t2[:], pi[:, 0:128], kr, ALU.mult, [bpi, bKf], [b2])
                TT_(p, "pool", G[:, kc, 1, :], t1[:], t2[:], ALU.add, [b1, b2], [bG])
            for i in range(2):
                py, bpy = psp.get()
                pairs = []
                for j in range(4):
                    pairs.append((COS[:, j, i * 128:(i + 1) * 128], G[:, j, 0, :]))
                    pairs.append((NSIN[:, j, i * 128:(i + 1) * 128], G[:, j, 1, :]))
                MM(p, py[:, 0:128], pairs, [bC, bNS, bG], [bpy])
                t1, b1 = tp.get()
                t2, b2 = tp.get()
                TT_(p, "dve", t1[:], py[:, 0:128], sN[:, o * 128:(o + 1) * 128], ALU.mult, [bpy, bsN], [b1])
                TT_(p, "pool", t2[:], Zf[:, i, :], SK[:, o, :], ALU.mult, [bZf, bSK], [b2])
                TT_(p, "pool", t2[:], t2[:], t1[:], ALU.add, [b1, b2], [b2])
                if o == 0:
                    TT_(p, "dve", Zf[:, i, :], t2[:], U[:, i, 1, :], ALU.mult, [b2, bU, bZb], [bZf])
                else:
                    TT_(p, "dve", O[:, i, :], t2[:], U[:, i, 2, :], ALU.mult, [b2, bU], [bO])
        p.dma("sp", dout, O[:], reads=[bO])
        p.emit()
    return nc


def run_LFc(inp, zext512):
    P = 512 // NCORE
    nc = build_LF(P)
    zp = np.ascontiguousarray(zext512.T)
    w23 = np.ascontiguousarray(np.stack([inp["hy_f_w2"][0], inp["hy_f_w3"][0]], axis=1))
    bf = np.ascontiguousarray(np.stack([inp["hy_f_b1"][0], inp["hy_f_b2"][0], inp["hy_f_b3"][0], inp["hy_f_freq"][0]], axis=1))
    maps = [{"zT": np.ascontiguousarray(zp[:, c * P:(c + 1) * P]), "w1": inp["hy_f_w1"][0], "w23": w23, "bf": bf}
            for c in range(NCORE)]
    res = run(nc, maps)
    return np.concatenate([np.asarray(r["hid"]) for r in res], axis=1)


def run_HCc(inp, uc, hid512, dext512):
    nc = build_HCc()
    tabs = hcc_tables()
    w4 = inp["hy_f_w4"][0].reshape(64, 2, 2, 1024)
    sk = inp["hy_skip"][0]
    maps = []
    for c in range(NCORE):
        ch = slice(c * 128, (c + 1) * 128)
        U = np.ascontiguousarray(uc.reshape(2, 128, 3, 1024)[:, :, :, ch].transpose(1, 0, 2, 3))
        W = np.ascontiguousarray(w4[:, :, :, ch].transpose(0, 2, 1, 3).reshape(64, 2, 256))
        D = np.ascontiguousarray(dext512[:, ch].reshape(4, 128, 128).transpose(1, 0, 2))
        S = np.ascontiguousarray(np.broadcast_to(sk[:, ch][None], (128, 2, 128)))
        m = {"U": U, "hid": hid512, "w4": W, "dec": D, "skip": S}
        m.update(tabs)
        maps.append(m)
    res = run(nc, maps)
    zs = [np.asarray(r["z2"]).transpose(1, 0, 2).reshape(256, 128) for r in res]
    return np.concatenate(zs, axis=1)


def kernel(**inp):
    inp = {k: np.asarray(v) for k, v in inp.items()}
    x = inp["x"][0]
    ctx = inp["ctx"][0]
    T0 = TX + TC
    mods = run_L0(inp)
    xsl = shard_tokens(x, ctx)
    h = run_L1(xsl, mods, 0, inp["norm_mix"][0], T0)
    hx, hc = unshard_tokens([np.asarray(a) for a in h], True)
    u, uc = run_HA(inp, hx, hc)
    tabs = hc_tables()
    nc_hc = build_HC()
    zext, dext = hyena_pos_tables(16384)
    hid = run_LF(inp, zext)
    z = run_HC(inp, u, hid, dext, tabs, nc=nc_hc)
    zext_c, dext_c = ctx_pos_tables()
    hid_c = run_LFc(inp, zext_c)
    zc = run_HCc(inp, uc, hid_c, dext_c)
    zsl = shard_tokens(np.asarray(z), np.asarray(zc))
    x1, h2, aff = run_L3(zsl, xsl, inp["hy_w_out"][0], inp["hy_b_out"][0], mods, 0, inp["norm_ffn"][0],
                         inp["moe_router"][0], T0, True)
    x2 = run_L4([np.asarray(a) for a in x1], [np.asarray(a) for a in h2], aff, mods, 0,
                inp["moe_w_gate"][0], inp["moe_w_up"][0], inp["moe_w_down"][0], T0, True)
    x2 = [np.asarray(a) for a in x2]
    q, k, v = run_L5(inp, x2, mods, T0)
    o = run_L6(inp, q, k, v)
    xs1 = [np.ascontiguousarray(a[:, :, :TX]) for a in x2]
    x3, h3, aff1 = run_L3(o, xs1, inp["da_w_out"][0], None, mods, 1, inp["norm_ffn"][1], inp["moe_router"][1], TX, False)
    x4 = run_L4([np.asarray(a) for a in x3], [np.asarray(a) for a in h3], aff1, mods, 1,
                inp["moe_w_gate"][1], inp["moe_w_up"][1], inp["moe_w_down"][1], TX, False)
    xo, _ = unshard_tokens([np.asarray(a) for a in x4], False)
    return np.ascontiguousarray(xo[None]).astype(np.float32)
```

```python
import numpy as np
from contextlib import ExitStack
import concourse.bass as bass
import concourse.mybir as mybir
from concourse.bass_utils import run_bass_kernel_spmd

F32 = mybir.dt.float32
BF16 = mybir.dt.bfloat16
I32 = mybir.dt.int32
ALU = mybir.AluOpType
AF = mybir.ActivationFunctionType
AX = mybir.AxisListType

EPOCH = 30000
N_DMA_SEMS = 24


class Buf:
    __slots__ = ("name", "w", "r")

    def __init__(self, name=""):
        self.name = name
        self.w = None
        self.r = {}


class Prog:
    ENGS = ("pe", "act", "dve", "pool", "sp")

    def __init__(self, nc, es):
        self.nc = nc
        self.es = es
        self.items = {e: [] for e in self.ENGS}
        self.cnt = {e: 0 for e in self.ENGS}
        self.esems = {e: [es.enter_context(nc.semaphore(f"s_{e}_0"))] for e in self.ENGS}
        self.seen = {e: {} for e in self.ENGS}
        self.dsems = {}
        self.dcount = {}
        self.drr = {}
        for q in ("sp", "pool", "act"):
            self.dsems[q] = [es.enter_context(nc.semaphore(f"d_{q}_{i}")) for i in range(N_DMA_SEMS)]
            self.dcount[q] = [0] * N_DMA_SEMS
            self.drr[q] = 0
        self.all_dma_tokens = []
        self.nbuf = 0

    def sb(self, name, shape, dt):
        t = self.es.enter_context(self.nc.sbuf_tensor("sb_" + name, list(shape), dt))
        return t

    def ps(self, name, shape, dt=F32):
        t = self.es.enter_context(self.nc.psum_tensor("ps_" + name, list(shape), dt))
        return t

    def buf(self, name=""):
        self.nbuf += 1
        return Buf(name or f"b{self.nbuf}")

    def _wait(self, eng, tok):
        if tok is None:
            return
        key, sem, val = tok
        if self.seen[eng].get(key, 0) >= val:
            return
        self.seen[eng][key] = val
        self.items[eng].append(("wait", sem, val))

    def _deps(self, eng, reads, writes):
        for b in reads:
            if b.w is not None:
                self._dep1(eng, b.w)
        for b in writes:
            if b.w is not None and b.w[0][0] != eng:
                self._dep1(eng, b.w)
            for t in b.r.values():
                if t[0][0] != eng:
                    self._dep1(eng, t)

    def _dep1(self, eng, tok):
        if eng == "pe" and tok[0][0] == "pe":
            return
        self._wait(eng, tok)

    def _mark(self, eng, tok, reads, writes):
        for b in reads:
            b.r[tok[0]] = tok
        for b in writes:
            b.w = tok
            b.r = {}

    def op(self, eng, fn, reads=(), writes=()):
        self._deps(eng, reads, writes)
        ep = self.cnt[eng] // EPOCH
        if ep >= len(self.esems[eng]):
            self.esems[eng].append(self.es.enter_context(self.nc.semaphore(f"s_{eng}_{ep}")))
        self.cnt[eng] += 1
        sem = self.esems[eng][ep]
        val = self.cnt[eng] - ep * EPOCH
        self.items[eng].append(("ins", fn, sem, 1))
        tok = ((eng, ep), sem, val)
        self._mark(eng, tok, reads, writes)
        return tok

    def group(self, eng, fns, reads=(), writes=()):
        self._deps(eng, reads, writes)
        tok = None
        for fn in fns:
            ep = self.cnt[eng] // EPOCH
            if ep >= len(self.esems[eng]):
                self.esems[eng].append(self.es.enter_context(self.nc.semaphore(f"s_{eng}_{ep}")))
            self.cnt[eng] += 1
            sem = self.esems[eng][ep]
            val = self.cnt[eng] - ep * EPOCH
            self.items[eng].append(("ins", fn, sem, 1))
            tok = ((eng, ep), sem, val)
        self._mark(eng, tok, reads, writes)
        return tok

    def dma(self, q, out, in_, reads=(), writes=(), **kw):
        self._deps(q, reads, writes)
        k = self.drr[q]
        self.drr[q] = (k + 1) % N_DMA_SEMS
        n = self.dcount[q][k]
        sem = self.dsems[q][k]
        if n > 0:
            self._wait(q, (("d", q, k), sem, 16 * n))
        self.dcount[q][k] = n + 1
        tok = (("d", q, k), sem, 16 * (n + 1))

        def fn(e, out=out, in_=in_, kw=kw):
            return e.dma_start(out=out, in_=in_, **kw)

        self.items[q].append(("ins", fn, sem, 16))
        self._mark(q, tok, reads, writes)
        self.all_dma_tokens.append(tok)
        return tok

    def coll(self, kind, in_ap, out_ap, reads=(), writes=(), groups=None):
        q = "pool"
        if not hasattr(self, "ccsem"):
            self.ccsem = self.es.enter_context(self.nc.semaphore("cc_sem"))
            self.ccn = 0
        self._deps(q, reads, writes)
        self.ccn += 1
        tok = (("cc",), self.ccsem, self.ccn)
        groups = groups or [list(range(8))]

        def fn(e):
            return e.collective_compute(kind, mybir.AluOpType.bypass, replica_groups=groups, ins=[in_ap.opt()], outs=[out_ap.opt()])

        self.items[q].append(("ins", fn, self.ccsem, 1))
        self._mark(q, tok, reads, writes)
        self.all_dma_tokens.append(tok)
        return tok

    def finish(self):
        last = {}
        for tok in self.all_dma_tokens:
            last[tok[0]] = tok
        for tok in last.values():
            self._wait("sp", tok)

    def emit(self):
        nc = self.nc
        self.finish()
        items = self.items
        with nc.Block() as block:
            def run(e, lst):
                for it in lst:
                    if it[0] == "wait":
                        e.wait_ge(it[1], it[2])
                    else:
                        it[1](e).then_inc(it[2], it[3])

            @block.tensor
            def _(e):
                run(e, items["pe"])

            @block.scalar
            def _(e):
                run(e, items["act"])

            @block.vector
            def _(e):
                run(e, items["dve"])

            @block.gpsimd
            def _(e):
                run(e, items["pool"])

            @block.sync
            def _(e):
                run(e, items["sp"])


class TT:
    def __init__(self, p, name, shape, dt, ntiles):
        self.t = p.sb(name, shape, dt)
        self.b = [p.buf(f"{name}_{i}") for i in range(ntiles)]


class Pool_:
    def __init__(self, p, name, shape, dt, n, psum=False):
        mk = p.ps if psum else p.sb
        self.ts = [mk(f"{name}{i}", shape, dt) for i in range(n)]
        self.bs = [p.buf(f"{name}{i}") for i in range(n)]
        self.i = 0

    def get(self):
        k = self.i
        self.i = (k + 1) % len(self.ts)
        return self.ts[k], self.bs[k]


def col_tiles(T, w=512):
    out = []
    c = 0
    while c < T:
        out.append((c, min(w, T - c)))
        c += w
    return out


def ACT(p, out, in_, func, reads, writes, **kw):
    return p.op("act", lambda e: e.activation(out=out, in_=in_, func=func, **kw), reads, writes)


def TT_(p, eng, out, in0, in1, op, reads, writes):
    return p.op(eng, lambda e: e.tensor_tensor(out=out, in0=in0, in1=in1, op=op), reads, writes)


def TS(p, eng, out, in0, s1, s2, op0, op1, reads, writes, **kw):
    if op1 is None:
        return p.op(eng, lambda e: e.tensor_scalar(out=out, in0=in0, scalar1=s1, scalar2=None, op0=op0, **kw), reads, writes)
    return p.op(eng, lambda e: e.tensor_scalar(out=out, in0=in0, scalar1=s1, scalar2=s2, op0=op0, op1=op1, **kw), reads, writes)


def STT(p, out, in0, scalar, in1, op0, op1, reads, writes):
    return p.op("dve", lambda e: e.scalar_tensor_tensor(out=out, in0=in0, scalar=scalar, in1=in1, op0=op0, op1=op1), reads, writes)


def CP(p, eng, out, in_, reads, writes):
    if eng == "act":
        return p.op("act", lambda e: e.activation(out=out, in_=in_, func=AF.Copy), reads, writes)
    return p.op(eng, lambda e: e.tensor_copy(out=out, in_=in_), reads, writes)


def MM(p, out, pairs, reads, writes):
    n = len(pairs)
    fns = []
    for i, (l, r) in enumerate(pairs):
        fns.append(lambda e, l=l, r=r, i=i: e.matmul(out, l, r, start=(i == 0), stop=(i == n - 1)))
    return p.group("pe", fns, reads, writes)


def LOAD(p, q, shape, dt, name, src, es=None):
    t = p.sb(name, shape, dt)
    b = p.buf(name)
    p.dma(q, t[:], src, writes=[b])
    return t, b


class Ctx:
    pass


def make_common(p, nc, ones_src, nps=4):
    C = Ctx()
    C.ones, C.bones = LOAD(p, "pool", [128, 128], BF16, "ones", ones_src)
    C.sq = Pool_(p, "sq", [128, 8, 512], BF16, 2)
    C.ps = Pool_(p, "psA", [128, 512], F32, nps, psum=True)
    C.rs = Pool_(p, "rs", [128, 512], F32, 2)
    C.tmp = Pool_(p, "tmp", [128, 512], F32, 3)
    return C


def emit_gmod(p, name, g, bg, scale, bscale, ncol=8):
    gm = p.sb(name, [128, ncol], F32)
    bgm = p.buf(name)
    STT(p, gm[:], scale, 1.0, g, ALU.add, ALU.mult, [bg, bscale], [bgm])
    return gm, bgm


def emit_rms_mod(p, C, X, tiles, mods_of_tile, OUT, OUTF=None, eps=1e-6, D=1024):
    for ti, (c0, w) in enumerate(tiles):
        gm, bgm, sh, bsh = mods_of_tile(ti)
        sq, bsq = C.sq.get()
        ACT(p, sq[:, :, 0:w], X.t[:, :, c0:c0 + w], AF.Square, [X.b[ti]], [bsq])
        ps, bps = C.ps.get()
        MM(p, ps[:, 0:w], [(C.ones[:], sq[:, kc, 0:w]) for kc in range(8)], [bsq, C.bones], [bps])
        rs, brs = C.rs.get()
        ACT(p, rs[:, 0:w], ps[:, 0:w], AF.Sqrt, [bps], [brs], scale=1.0 / D, bias=eps)
        p.op("dve", lambda e, rs=rs, w=w: e.reciprocal(out=rs[:, 0:w], in_=rs[:, 0:w]), [brs], [brs])
        for kc in range(8):
            tmp, btmp = C.tmp.get()
            STT(p, tmp[:, 0:w], X.t[:, kc, c0:c0 + w], gm[:, kc:kc + 1], rs[:, 0:w], ALU.mult, ALU.mult,
                [X.b[ti], brs, bgm], [btmp])
            ACT(p, OUT.t[:, kc, c0:c0 + w], tmp[:, 0:w], AF.Identity, [btmp, bsh], [OUT.b[ti]],
                bias=sh[:, kc:kc + 1], scale=1.0)
            if OUTF is not None:
                ACT(p, OUTF.t[:, kc, c0:c0 + w], tmp[:, 0:w], AF.Identity, [btmp, bsh], [OUTF.b[ti]],
                    bias=sh[:, kc:kc + 1], scale=1.0)


def fm(a):
    T = a.shape[0]
    return np.ascontiguousarray(a.T.reshape(8, 128, T).transpose(1, 0, 2))


def unfm(a):
    T = a.shape[2]
    return np.ascontiguousarray(a.transpose(1, 0, 2).reshape(1024, T).T)


def vec_fm(v, n=8):
    return np.ascontiguousarray(v.reshape(n, 128).T)


NCORE = 8
TX = 2048
TC = 32
ONES_F32 = np.ones((128, 128), np.float32)


def run(nc, in_maps):
    res = run_bass_kernel_spmd(nc, in_maps, core_ids=list(range(NCORE)))
    return res.results


def build_L0():
    nc = bass.Bass("TRN2", target_bir_lowering=False)
    condT = nc.dram_tensor("condT", [128, 8, 2], F32, kind="ExternalInput").ap()
    w = nc.dram_tensor("w", [2, 128, 8, 768], F32, kind="ExternalInput").ap()
    b = nc.dram_tensor("b", [2, 2, 768], F32, kind="ExternalInput").ap()
    out = nc.dram_tensor("out", [2, 2, 768], F32, kind="ExternalOutput").ap()
    with ExitStack() as es:
        p = Prog(nc, es)
        ct, bct = LOAD(p, "sp", [128, 8, 2], F32, "ct", condT)
        sc = p.sb("sc", [128, 8, 2], F32); bsc = p.buf()
        ACT(p, sc[:], ct[:], AF.Silu, [bct], [bsc])
        psp = Pool_(p, "ps", [128, 512], F32, 2, psum=True)
        for l in range(2):
            wt, bwt = LOAD(p, "sp", [128, 8, 768], F32, f"w{l}", w[l])
            bt, bbt = LOAD(p, "sp", [2, 768], F32, f"b{l}", b[l])
            ot = p.sb(f"o{l}", [2, 768], F32); bot = p.buf()
            for h in range(2):
                ps, bps = psp.get()
                MM(p, ps[0:2, 0:384], [(sc[:, kc, :], wt[:, kc, h * 384:(h + 1) * 384]) for kc in range(8)],
                   [bsc, bwt], [bps])
                TT_(p, "dve", ot[:, h * 384:(h + 1) * 384], ps[0:2, 0:384], bt[:, h * 384:(h + 1) * 384], ALU.add,
                    [bps, bbt], [bot])
            p.dma("sp", out[l], ot[:], reads=[bot])
        p.emit()
    return nc


def run_L0(inp):
    nc = build_L0()
    cond = np.stack([inp["c"][0], inp["c_ctx"]], axis=1)
    condT = np.ascontiguousarray(cond.reshape(8, 128, 2).transpose(1, 0, 2))
    maps = []
    for c in range(NCORE):
        sl = slice(c * 768, (c + 1) * 768)
        w = np.ascontiguousarray(inp["ada_w"][:, :, sl].reshape(2, 8, 128, 768).transpose(0, 2, 1, 3))
        b = np.ascontiguousarray(np.broadcast_to(inp["ada_b"][:, None, sl], (2, 2, 768)))
        maps.append({"condT": condT, "w": w, "b": b})
    res = run(nc, maps)
    mods = np.concatenate([r["out"] for r in res], axis=2)
    return mods


def mods_fm(mods, l):
    m = mods[l].reshape(2, 6, 8, 128)
    return np.ascontiguousarray(m.transpose(3, 0, 1, 2))


def tiles_of(T):
    return col_tiles(T, 512)


def build_L1(T):
    nc = bass.Bass("TRN2", target_bir_lowering=False)
    xT = nc.dram_tensor("xT", [128, 8, T], F32, kind="ExternalInput").ap()
    md = nc.dram_tensor("md", [128, 2, 6, 8], F32, kind="ExternalInput").ap()
    g = nc.dram_tensor("g", [128, 8], F32, kind="ExternalInput").ap()
    ones = nc.dram_tensor("ones", [128, 128], F32, kind="ExternalInput").ap()
    hT = nc.dram_tensor("hT", [128, 8, T], BF16, kind="ExternalOutput").ap()
    tiles = tiles_of(T)
    with ExitStack() as es:
        p = Prog(nc, es)
        C = make_common(p, nc, ones)
        mdt, bmd = LOAD(p, "sp", [128, 2, 6, 8], F32, "md", md)
        gt, bg = LOAD(p, "sp", [128, 8], F32, "g", g)
        X = TT(p, "X", [128, 8, T], F32, len(tiles))
        H = TT(p, "H", [128, 8, T], BF16, len(tiles))
        for ti, (c0, w) in enumerate(tiles):
            p.dma("sp", X.t[:, :, c0:c0 + w], xT[:, :, c0:c0 + w], writes=[X.b[ti]])
        gmx, bgmx = emit_gmod(p, "gmx", gt[:], bg, mdt[:, 0, 1, :], bmd)
        gmc, bgmc = emit_gmod(p, "gmc", gt[:], bg, mdt[:, 1, 1, :], bmd)

        def mods_of_tile(ti):
            c0, w = tiles[ti]
            if c0 >= TX:
                return gmc, bgmc, mdt[:, 1, 0, :], bmd
            return gmx, bgmx, mdt[:, 0, 0, :], bmd

        emit_rms_mod(p, C, X, tiles, mods_of_tile, H)
        for ti, (c0, w) in enumerate(tiles):
            p.dma("sp", hT[:, :, c0:c0 + w], H.t[:, :, c0:c0 + w], reads=[H.b[ti]])
        p.emit()
    return nc


def shard_tokens(x, ctx):
    out = []
    for c in range(NCORE):
        parts = [x[c * TX:(c + 1) * TX]]
        if ctx is not None:
            parts.append(ctx[c * TC:(c + 1) * TC])
        out.append(fm(np.concatenate(parts, axis=0)))
    return out


def unshard_tokens(slabs, has_ctx):
    xs, cs = [], []
    for s in slabs:
        a = unfm(s)
        xs.append(a[:TX])
        if has_ctx:
            cs.append(a[TX:])
    return np.concatenate(xs, 0), (np.concatenate(cs, 0) if has_ctx else None)


def run_L1(xsl, mods, l, g, T):
    nc = build_L1(T)
    md = mods_fm(mods, l)
    maps = [{"xT": xsl[c], "md": md, "g": vec_fm(g), "ones": ONES_F32} for c in range(NCORE)]
    res = run(nc, maps)
    return [r["hT"] for r in res]


PI = float(np.pi)


def build_LF(P):
    nc = bass.Bass("TRN2", target_bir_lowering=False)
    zT = nc.dram_tensor("zT", [33, P], F32, kind="ExternalInput").ap()
    w1 = nc.dram_tensor("w1", [33, 64], F32, kind="ExternalInput").ap()
    w23 = nc.dram_tensor("w23", [64, 2, 64], F32, kind="ExternalInput").ap()
    bf = nc.dram_tensor("bf", [64, 4], F32, kind="ExternalInput").ap()
    hid = nc.dram_tensor("hid", [64, P], BF16, kind="ExternalOutput").ap()
    tiles = col_tiles(P)
    with ExitStack() as es:
        p = Prog(nc, es)
        w1t, bw1 = LOAD(p, "sp", [33, 64], F32, "w1", w1)
        w23t, bw23 = LOAD(p, "sp", [64, 2, 64], F32, "w23", w23)
        bft, bbf = LOAD(p, "sp", [64, 4], F32, "bf", bf)
        bfr = p.sb("bfr", [64, 3], F32); bbfr = p.buf()
        TS(p, "dve", bfr[:], bft[:, 0:3], bft[:, 3:4], None, ALU.mult, None, [bbf], [bbfr])
        zp = Pool_(p, "z", [33, 512], F32, 2)
        psp = Pool_(p, "ps", [64, 512], F32, 3, psum=True)
        ap_ = Pool_(p, "arg", [64, 512], F32, 3)
        hp = Pool_(p, "h", [64, 512], F32, 3)
        op_ = Pool_(p, "o", [64, 512], BF16, 2)
        wp = Pool_(p, "wr", [64, 512], F32, 2)
        for (c0, w) in tiles:
            zt, bz = zp.get()
            p.dma("sp", zt[:, 0:w], zT[:, c0:c0 + w], writes=[bz])
            cur, bcur = zt, bz
            for l in range(3):
                ps, bps = psp.get()
                lhsT = w1t[:] if l == 0 else w23t[:, l - 1, :]
                bl = bw1 if l == 0 else bw23
                MM(p, ps[:, 0:w], [(lhsT, cur[:, 0:w])], [bl, bcur], [bps])
                a, ba = ap_.get()
                ACT(p, a[:, 0:w], ps[:, 0:w], AF.Identity, [bps, bbf, bbfr], [ba], scale=bft[:, 3:4], bias=bfr[:, l:l + 1])
                wt_, bwt_ = wp.get()
                TS(p, "dve", wt_[:, 0:w], a[:, 0:w], -PI, 2 * PI, ALU.is_lt, ALU.mult, [ba], [bwt_])
                TT_(p, "dve", a[:, 0:w], a[:, 0:w], wt_[:, 0:w], ALU.add, [ba, bwt_], [ba])
                wt_, bwt_ = wp.get()
                TS(p, "dve", wt_[:, 0:w], a[:, 0:w], PI, -2 * PI, ALU.is_gt, ALU.mult, [ba], [bwt_])
                TT_(p, "dve", a[:, 0:w], a[:, 0:w], wt_[:, 0:w], ALU.add, [ba, bwt_], [ba])
                if l < 2:
                    h, bh = hp.get()
                else:
                    h, bh = op_.get()
                ACT(p, h[:, 0:w], a[:, 0:w], AF.Sin, [ba], [bh])
                cur, bcur = h, bh
            p.dma("sp", hid[:, c0:c0 + w], cur[:, 0:w], reads=[bcur])
        p.emit()
    return nc


def hyena_pos_tables(L):
    N = 32768
    t = np.linspace(0.0, 1.0, L, dtype=np.float32)[:, None]
    w = (2.0 * np.float32(np.pi) * np.arange(L, dtype=np.float32)[:, None] / np.float32(L)).astype(np.float32)
    bands = np.linspace(1e-4, 15, 16, dtype=np.float32)[None]
    z = np.concatenate([t, np.cos(w * bands), -np.sin(w * bands)], axis=-1).astype(np.float32)
    maxd = np.log(1e-2) / 0.3
    mind = np.log(1e-2) / 1.5
    deltas = np.linspace(mind, maxd, 1024, dtype=np.float32)
    decay = np.exp(-t * np.abs(deltas)).astype(np.float32)
    zext = np.zeros((N, 33), np.float32)
    dext = np.zeros((N, 1024), np.float32)
    zext[:L] = z
    dext[:L] = decay
    j = np.arange(1, L)
    zext[N - j] = z[j]
    dext[N - j] = decay[j]
    return zext, dext


def perm_pos(a):
    F_ = a.shape[1]
    return np.ascontiguousarray(a.reshape(256, 128, F_).transpose(2, 1, 0))


def run_LF(inp, zext):
    P = 32768 // NCORE
    nc = build_LF(P)
    zp = perm_pos(zext).reshape(33, 32768)
    w23 = np.ascontiguousarray(np.stack([inp["hy_f_w2"][0], inp["hy_f_w3"][0]], axis=1))
    bf = np.ascontiguousarray(np.stack([inp["hy_f_b1"][0], inp["hy_f_b2"][0], inp["hy_f_b3"][0], inp["hy_f_freq"][0]], axis=1))
    maps = [{"zT": np.ascontiguousarray(zp[:, c * P:(c + 1) * P]), "w1": inp["hy_f_w1"][0], "w23": w23, "bf": bf}
            for c in range(NCORE)]
    res = run(nc, maps)
    hid = np.concatenate([np.asarray(r["hid"]) for r in res], axis=1)
    return hid.reshape(64, 128, 256)


def build_HA(segs):
    Th = sum(n + 2 for _, n in segs)
    T = sum(n for _, n in segs)
    nc = bass.Bass("TRN2", target_bir_lowering=False)
    hT = nc.dram_tensor("hT", [128, 8, Th], BF16, kind="ExternalInput").ap()
    valid = nc.dram_tensor("valid", [1, Th], BF16, kind="ExternalInput").ap()
    win = nc.dram_tensor("win", [24, 128, 8, 128], F32, kind="ExternalInput").ap()
    cw = nc.dram_tensor("cw", [128, 24, 3], F32, kind="ExternalInput").ap()
    brow = nc.dram_tensor("brow", [1, 3072], F32, kind="ExternalInput").ap()
    cb = nc.dram_tensor("cb", [128, 24], F32, kind="ExternalInput").ap()
    uT = nc.dram_tensor("uT", [128, 24, T], F32, kind="ExternalOutput").ap()
    tiles = []
    o0 = 0
    for hb, n in segs:
        for (c0, w) in col_tiles(n, 510):
            tiles.append((hb + c0, o0 + c0, w))
        o0 += n
    with ExitStack() as es:
        p = Prog(nc, es)
        H, bH = LOAD(p, "sp", [128, 8, Th], BF16, "H", hT)
        V, bV = LOAD(p, "sp", [1, Th], BF16, "V", valid)
        BR, bBR = LOAD(p, "pool", [1, 3072], BF16, "BR", brow)
        CB, bCB = LOAD(p, "sp", [128, 24], F32, "CB", cb)
        CW, bCW = LOAD(p, "sp", [128, 24, 3], F32, "CW", cw)
        wp = Pool_(p, "w", [128, 8, 128], BF16, 3)
        psp = Pool_(p, "ps", [128, 512], F32, 6, psum=True)
        op_ = Pool_(p, "o", [128, T], F32, 3)
        for m in range(24):
            wt, bw = wp.get()
            p.dma("pool", wt[:], win[m], writes=[bw])
            ot, bo = op_.get()
            for (hb, ob, w) in tiles:
                ps, bps = psp.get()
                pairs = [(wt[:, kc, :], H[:, kc, hb:hb + w + 2]) for kc in range(8)]
                pairs.append((BR[0:1, m * 128:(m + 1) * 128], V[0:1, hb:hb + w + 2]))
                MM(p, ps[:, 0:w + 2], pairs, [bw, bH, bV, bBR], [bps])
                ACT(p, ot[:, ob:ob + w], ps[:, 1:w + 1], AF.Identity, [bps, bCB, bCW], [bo], bias=CB[:, m:m + 1], scale=CW[:, m, 1:2])
                STT(p, ot[:, ob:ob + w], ps[:, 0:w], CW[:, m, 0:1], ot[:, ob:ob + w], ALU.mult, ALU.add, [bps, bCW, bo], [bo])
                STT(p, ot[:, ob:ob + w], ps[:, 2:w + 2], CW[:, m, 2:3], ot[:, ob:ob + w], ALU.mult, ALU.add, [bps, bCW, bo], [bo])
            p.dma("sp", uT[:, m, :], ot[:], reads=[bo])
        p.emit()
    return nc


def halo_slabs(hx, hc):
    import ml_dtypes
    out = []
    for c in range(NCORE):
        parts, val = [], []
        for a, n in ((hx, TX), (hc, TC)):
            if a is None:
                continue
            L = a.shape[0]
            seg = np.zeros((n + 2, 1024), a.dtype)
            v = np.zeros((n + 2,), np.float32)
            lo, hi = c * n - 1, (c + 1) * n + 1
            slo, shi = max(lo, 0), min(hi, L)
            seg[slo - lo:shi - lo] = a[slo:shi]
            v[slo - lo:shi - lo] = 1.0
            parts.append(seg)
            val.append(v)
        out.append((fm(np.concatenate(parts, 0)), np.concatenate(val)[None].astype(ml_dtypes.bfloat16)))
    return out


def run_HA(inp, hx, hc):
    segs = [(0, TX)] + ([(TX + 2, TC)] if hc is not None else [])
    nc = build_HA(segs)
    W = inp["hy_w_in"][0]
    win = np.ascontiguousarray(W.reshape(8, 128, 24, 128).transpose(2, 1, 0, 3))
    cwv = inp["hy_conv_w"][0]
    cw = np.ascontiguousarray(cwv.reshape(3, 24, 128).transpose(2, 1, 0))
    brow = np.ascontiguousarray(inp["hy_b_in"][0][None])
    cb = vec_fm(inp["hy_conv_b"][0], 24)
    sl = halo_slabs(hx, hc)
    maps = [{"hT": sl[c][0], "valid": sl[c][1], "win": win, "cw": cw, "brow": brow, "cb": cb} for c in range(NCORE)]
    res = run(nc, maps)
    us, ucs = [], []
    for r in res:
        a = np.asarray(r["uT"]).transpose(1, 0, 2).reshape(3072, -1).T
        us.append(a[:TX])
        ucs.append(a[TX:])
    return np.concatenate(us, 0), (np.concatenate(ucs, 0) if hc is not None else None)


NG, GC = 8, 16


def hc_tables():
    import ml_dtypes
    bf = ml_dtypes.bfloat16
    N = 32768
    n1 = np.arange(128)[:, None].astype(np.float64)
    k1 = np.arange(256)[None].astype(np.float64)
    F1 = np.zeros((128, 2, 512), np.float64)
    for h in range(2):
        a = 2 * np.pi * (n1 + 128 * h) * k1 / 256
        F1[:, h, :256] = np.cos(a)
        F1[:, h, 256:] = -np.sin(a)
    phi = 2 * np.pi * np.arange(128)[:, None] * np.arange(256)[None] / N
    TW = np.stack([np.cos(phi), np.sin(phi)], 1)
    th = 2 * np.pi * np.arange(128)[:, None] * np.arange(128)[None] / 128
    F2 = np.stack([np.cos(th), np.sin(th), -np.sin(th)], 1)
    M3 = np.zeros((128, 2, 256), np.float64)
    M3[:, 0, :128] = np.cos(th); M3[:, 0, 128:] = np.sin(th)
    M3[:, 1, :128] = -np.sin(th); M3[:, 1, 128:] = np.cos(th)
    TWI = np.zeros((128, 2, 2, 128), np.float64)
    S4 = np.zeros((128, 2, 2, 128), np.float64)
    kp = np.arange(128)[:, None]
    for half in range(2):
        ph = 2 * np.pi * np.arange(128)[None] * (half * 128 + kp) / N
        TWI[:, half, 0] = np.cos(ph); TWI[:, half, 1] = np.sin(ph)
        ps_ = 2 * np.pi * np.arange(128)[None] * (half * 128 + kp) / 256
        S4[:, half, 0] = np.cos(ps_) / N; S4[:, half, 1] = -np.sin(ps_) / N
    return {"F1": F1.astype(bf), "TW": TW.astype(np.float32), "F2": F2.astype(bf), "M3": M3.astype(bf),
            "TWI": TWI.astype(np.float32), "S4": S4.astype(bf), "ones32": np.ones((128, 128), np.float32)}


HC_STOP = [0]


class _Stop(Exception):
    pass


def _chk(n):
    if HC_STOP[0] == n:
        raise _Stop()


def build_HC():
    nc = bass.Bass("TRN2", target_bir_lowering=False)
    dU = nc.dram_tensor("U", [NG, 128, 3, GC, 128], F32, kind="ExternalInput").ap()
    dhid = nc.dram_tensor("hid", [64, 128, 256], BF16, kind="ExternalInput").ap()
    dw4 = nc.dram_tensor("w4", [NG, 64, 2, 2 * GC], F32, kind="ExternalInput").ap()
    ddec = nc.dram_tensor("dec", [NG, 128, 2, 128, GC], F32, kind="ExternalInput").ap()
    dskip = nc.dram_tensor("skip", [128, NG, 2, GC], F32, kind="ExternalInput").ap()
    dF1 = nc.dram_tensor("F1", [128, 2, 512], BF16, kind="ExternalInput").ap()
    dTW = nc.dram_tensor("TW", [128, 2, 256], F32, kind="ExternalInput").ap()
    dF2 = nc.dram_tensor("F2", [128, 3, 128], BF16, kind="ExternalInput").ap()
    dM3 = nc.dram_tensor("M3", [128, 2, 256], BF16, kind="ExternalInput").ap()
    dTWI = nc.dram_tensor("TWI", [128, 2, 2, 128], F32, kind="ExternalInput").ap()
    dS4 = nc.dram_tensor("S4", [128, 2, 2, 128], BF16, kind="ExternalInput").ap()
    dones = nc.dram_tensor("ones32", [128, 128], F32, kind="ExternalInput").ap()
    dout = nc.dram_tensor("z2", [NG, 128, GC, 128], BF16, kind="ExternalOutput").ap()
    with ExitStack() as es:
        p = Prog(nc, es)
        F1, bF1 = LOAD(p, "sp", [128, 2, 512], BF16, "F1", dF1)
        TW, bTW = LOAD(p, "sp", [128, 2, 256], F32, "TW", dTW)
        F2, bF2 = LOAD(p, "sp", [128, 3, 128], BF16, "F2", dF2)
        M3, bM3 = LOAD(p, "sp", [128, 2, 256], BF16, "M3", dM3)
        TWI, bTWI = LOAD(p, "sp", [128, 2, 2, 128], F32, "TWI", dTWI)
        S4, bS4 = LOAD(p, "sp", [128, 2, 2, 128], BF16, "S4", dS4)
        ON, bON = LOAD(p, "sp", [128, 128], F32, "ON", dones)
        SK, bSK = LOAD(p, "sp", [128, NG, 2, GC], F32, "SK", dskip)
        cst = [bF1, bTW, bF2, bM3, bTWI, bS4]
        Up = Pool_(p, "U", [128, 3, GC, 128], F32, 1)
        Dp = Pool_(p, "D", [128, 2, 128, GC], F32, 1)
        W4p = Pool_(p, "W4", [64, 2, 2 * GC], BF16, 2)
        Hp = Pool_(p, "Hd", [64, 16, 256], BF16, 2)
        Kt = p.sb("Kt", [128, 2, GC, 2, 128], BF16); bKt = p.buf()
        Kr = p.sb("Kr", [128, 2, 128, 2, GC], BF16); bKr = [p.buf(), p.buf()]
        A = p.sb("A", [128, GC, 2, 256], BF16); bA = [p.buf() for _ in range(GC // 2)]
        Kf = p.sb("Kf", [128, GC, 2, 256], BF16); bKf = [p.buf() for _ in range(GC // 2)]
        G = p.sb("G", [128, GC, 2, 256], BF16); bG = [p.buf() for _ in range(GC // 2)]
        Bp = p.sb("Bp", [128, 2, 2, GC, 128], BF16); bBp = [p.buf() for _ in range(GC // 4)]
        Zb = p.sb("Zb", [128, GC, 128], BF16); bZb = [p.buf() for _ in range(GC // 4)]
        Z1 = p.sb("Z1", [128, GC, 128], F32); bZ1 = [p.buf() for _ in range(GC // 4)]
        Op = Pool_(p, "O", [128, GC, 128], BF16, 2)
        red = p.sb("red", [128, 2 * GC], F32); bred = p.buf()
        sN = p.sb("sN", [128, 2 * GC], F32); bsN = p.buf()
        psSp = Pool_(p, "psS", [128, 512], F32, 2, psum=True)
        ps1 = Pool_(p, "ps1", [128, 512], F32, 2, psum=True)
        psY = Pool_(p, "psY", [128, 512], F32, 3, psum=True)
        ps4 = Pool_(p, "ps4", [128, 512], F32, 1, psum=True)
        T1p = Pool_(p, "T1", [128, 2, 256], F32, 3)
        T2p = Pool_(p, "T2", [128, 2, 256], F32, 3)
        E1p = Pool_(p, "E1", [128, 4, 128], F32, 2)
        E2p = Pool_(p, "E2", [128, 4, 128], F32, 2)

        def fwd_twiddle(ps, bps, c, bdst):
            t1, b1 = T1p.get()
            t2, b2 = T2p.get()
            pv = ps[:, :].rearrange("p (r k) -> p r k", r=2)
            TT_(p, "dve", t1[:], pv, TW[:, 0, :].unsqueeze(1).to_broadcast([128, 2, 256]), ALU.mult, [bps, bTW], [b1])
            TT_(p, "dve", t2[:], pv, TW[:, 1, :].unsqueeze(1).to_broadcast([128, 2, 256]), ALU.mult, [bps, bTW], [b2])
            TT_(p, "pool", A[:, c, 0, :], t1[:, 0, :], t2[:, 1, :], ALU.add, [b1, b2], [bdst])
            TT_(p, "pool", A[:, c, 1, :], t1[:, 1, :], t2[:, 0, :], ALU.subtract, [b1, b2], [bdst])

        def stage2(pr):
            c0 = 2 * pr
            yr, byr = psY.get()
            yi, byi = psY.get()
            ar = A[:, c0:c0 + 2, 0, :]
            ai = A[:, c0:c0 + 2, 1, :]
            MM(p, yr[:, :], [(F2[:, 0, :], ar), (F2[:, 1, :], ai)], [bF2, bA[pr]], [byr])
            MM(p, yi[:, :], [(F2[:, 0, :], ai), (F2[:, 2, :], ar)], [bF2, bA[pr]], [byi])
            return yr, byr, yi, byi

        for g in range(NG):
          try:
            Ug, bU = Up.get()
            p.dma("sp", Ug[:], dU[g], writes=[bU])
            Dg, bD = Dp.get()
            p.dma("sp", Dg[:], ddec[g], writes=[bD])
            W4, bW4 = W4p.get()
            p.dma("pool", W4[:], dw4[g], writes=[bW4])
            _chk(10)
            for nb in range(8):
                Hd, bHd = Hp.get()
                p.dma("sp", Hd[:], dhid[:, nb * 16:(nb + 1) * 16, :], writes=[bHd])
                for jb in range(2):
                    bank, bbank = psSp.get()
                    fns = []
                    for jj in range(8):
                        j = jb * 8 + jj
                        for h in range(2):
                            k = jj * 2 + h
                            fns.append(lambda e, bank=bank, k=k, j=j, h=h, Hd=Hd, W4=W4: e.matmul(
                                bank[:, k * 32:(k + 1) * 32], Hd[:, j, h * 128:(h + 1) * 128], W4[:, h, :], start=True, stop=True))
                    p.group("pe", fns, [bHd, bW4], [bbank])
                    for jj in range(8):
                        j = jb * 8 + jj
                        n2 = nb * 16 + j
                        for h in range(2):
                            k = jj * 2 + h
                            TT_(p, "dve", Kr[:, h, n2, :, :], bank[:, k * 32:(k + 1) * 32].rearrange("p (o c) -> p o c", o=2),
                                Dg[:, h, n2, :].unsqueeze(1).to_broadcast([128, 2, GC]), ALU.mult, [bbank, bD], [bKr[h]])
            for o_ in range(2):
                for c_ in range(GC):
                    CP(p, "act", Kt[:, o_, c_, :, :], Kr[:, :, :, o_, c_], bKr, [bKt])
            _chk(1)
            p.op("dve", lambda e: e.tensor_reduce(out=red[:], in_=Kt[:].rearrange("p o c h n -> p (o c) (h n)"),
                                                   axis=AX.X, op=ALU.add, apply_absolute_value=True), [bKt], [bred])
            bank, bbank = psSp.get()
            sl = bank[:, 0:32]
            MM(p, sl, [(ON[:], red[:])], [bON, bred], [bbank])
            TS(p, "dve", sN[:], sl, 1e-6, None, ALU.add, None, [bbank], [bsN])
            p.op("dve", lambda e: e.reciprocal(out=sN[:], in_=sN[:]), [bsN], [bsN])
            _chk(2)
            Og, bO = Op.get()
            for o in range(2):
                for c in range(GC):
                    ps, bps = ps1.get()
                    MM(p, ps[:, :], [(Kt[:, o, c, 0, :], F1[:, 0, :]), (Kt[:, o, c, 1, :], F1[:, 1, :])], [bKt, bF1], [bps])
                    fwd_twiddle(ps, bps, c, bA[c // 2])
                for pr in range(GC // 2):
                    yr, byr, yi, byi = stage2(pr)
                    CP(p, "act", Kf[:, 2 * pr:2 * pr + 2, 0, :], yr[:, :].rearrange("p (c k) -> p c k", c=2), [byr], [bKf[pr]])
                    CP(p, "act", Kf[:, 2 * pr:2 * pr + 2, 1, :], yi[:, :].rearrange("p (c k) -> p c k", c=2), [byi], [bKf[pr]])
                _chk(3)
                if o == 0:
                    for q in range(GC // 4):
                        CP(p, "act", Zb[:, 4 * q:4 * q + 4, :], Ug[:, 0, 4 * q:4 * q + 4, :], [bU], [bZb[q]])
                for c in range(GC):
                    ps, bps = ps1.get()
                    MM(p, ps[:, :], [(Zb[:, c, :], F1[:, 0, :])], [bZb[c // 4], bF1], [bps])
                    fwd_twiddle(ps, bps, c, bA[c // 2])
                for pr in range(GC // 2):
                    c0 = 2 * pr
                    yr, byr, yi, byi = stage2(pr)
                    yrv = yr[:, :].rearrange("p (c k) -> p c k", c=2)
                    yiv = yi[:, :].rearrange("p (c k) -> p c k", c=2)
                    kr = Kf[:, c0:c0 + 2, 0, :]
                    ki = Kf[:, c0:c0 + 2, 1, :]
                    t1, b1 = T1p.get()
                    t2, b2 = T2p.get()
                    TT_(p, "dve", t1[:], yrv, kr, ALU.mult, [byr, bKf[pr]], [b1])
                    TT_(p, "dve", t2[:], yiv, ki, ALU.mult, [byi, bKf[pr]], [b2])
                    TT_(p, "pool", G[:, c0:c0 + 2, 0, :], t1[:], t2[:], ALU.subtract, [b1, b2], [bG[pr]])
                    t1, b1 = T1p.get()
                    t2, b2 = T2p.get()
                    TT_(p, "dve", t1[:], yrv, ki, ALU.mult, [byr, bKf[pr]], [b1])
                    TT_(p, "dve", t2[:], yiv, kr, ALU.mult, [byi, bKf[pr]], [b2])
                    TT_(p, "pool", G[:, c0:c0 + 2, 1, :], t1[:], t2[:], ALU.add, [b1, b2], [bG[pr]])
                _chk(4)
                for c in range(GC):
                    for half in range(2):
                        ps, bps = ps1.get()
                        MM(p, ps[:, 0:256], [(G[:, c, 0, half * 128:(half + 1) * 128], M3[:, 0, :]),
                                             (G[:, c, 1, half * 128:(half + 1) * 128], M3[:, 1, :])], [bG[c // 2], bM3], [bps])
                        t1, b1 = T1p.get()
                        t2, b2 = T2p.get()
                        pv = ps[:, 0:256].rearrange("p (r k) -> p r k", r=2)
                        TT_(p, "dve", t1[:, :, 0:128], pv, TWI[:, half, 0, :].unsqueeze(1).to_broadcast([128, 2, 128]), ALU.mult,
                            [bps, bTWI], [b1])
                        TT_(p, "dve", t2[:, :, 0:128], pv, TWI[:, half, 1, :].unsqueeze(1).to_broadcast([128, 2, 128]), ALU.mult,
                            [bps, bTWI], [b2])
                        TT_(p, "pool", Bp[:, half, 0, c, :], t1[:, 0, 0:128], t2[:, 1, 0:128], ALU.subtract, [b1, b2], [bBp[c // 4]])
                        TT_(p, "pool", Bp[:, half, 1, c, :], t2[:, 0, 0:128], t1[:, 1, 0:128], ALU.add, [b1, b2], [bBp[c // 4]])
                _chk(5)
                for q in range(GC // 4):
                    c0 = 4 * q
                    ps, bps = ps4.get()
                    MM(p, ps[:, :], [(S4[:, half, ri, :], Bp[:, half, ri, c0:c0 + 4, :]) for half in range(2) for ri in range(2)],
                       [bS4, bBp[q]], [bps])
                    e1, be1 = E1p.get()
                    e2, be2 = E2p.get()
                    pv = ps[:, :].rearrange("p (c n) -> p c n", c=4)
                    TT_(p, "dve", e1[:], pv, sN[:, o * GC + c0:o * GC + c0 + 4].unsqueeze(2).to_broadcast([128, 4, 128]), ALU.mult,
                        [bps, bsN], [be1])
                    if o == 0:
                        zc, bzc = Ug[:, 0, c0:c0 + 4, :], bU
                    else:
                        zc, bzc = Z1[:, c0:c0 + 4, :], bZ1[q]
                    TT_(p, "pool", e2[:], zc, SK[:, g, o, c0:c0 + 4].unsqueeze(2).to_broadcast([128, 4, 128]), ALU.mult,
                        [bzc, bSK], [be2])
                    TT_(p, "pool", e2[:], e2[:], e1[:], ALU.add, [be1, be2], [be2])
                    if o == 0:
                        TT_(p, "dve", Z1[:, c0:c0 + 4, :], e2[:], Ug[:, 1, c0:c0 + 4, :], ALU.mult, [be2, bU], [bZ1[q]])
                        CP(p, "act", Zb[:, c0:c0 + 4, :], Z1[:, c0:c0 + 4, :], [bZ1[q]], [bZb[q]])
                    else:
                        TT_(p, "dve", Og[:, c0:c0 + 4, :], e2[:], Ug[:, 2, c0:c0 + 4, :], ALU.mult, [be2, bU], [bO])
            p.dma("sp", dout[g], Og[:], reads=[bO])
          except _Stop:
            break
        p.emit()
    return nc


def run_HC(inp, u, hid, dext, tabs, nc=None):
    if nc is None:
        nc = build_HC()
    L = u.shape[0]
    up = np.zeros((16384, 3072), np.float32)
    up[:L] = u
    u4 = up.reshape(128, 128, 3, 1024)
    d4 = dext.reshape(2, 128, 128, 1024).transpose(1, 0, 2, 3)
    w4 = inp["hy_f_w4"][0].reshape(64, 2, 2, 1024)
    sk = inp["hy_skip"][0]
    maps = []
    for c in range(NCORE):
        ch = slice(c * 128, (c + 1) * 128)
        U = np.ascontiguousarray(u4[:, :, :, ch].reshape(128, 128, 3, NG, GC).transpose(3, 0, 2, 4, 1))
        D = np.ascontiguousarray(d4[:, :, :, ch].reshape(128, 2, 128, NG, GC).transpose(3, 0, 1, 2, 4))
        W = np.ascontiguousarray(w4[:, :, :, ch].reshape(64, 2, 2, NG, GC).transpose(3, 0, 2, 1, 4).reshape(NG, 64, 2, 2 * GC))
        S = np.ascontiguousarray(np.broadcast_to(sk[:, ch].reshape(2, NG, GC).transpose(1, 0, 2)[None], (128, NG, 2, GC)))
        m = {"U": U, "hid": hid, "w4": W, "dec": D, "skip": S}
        m.update(tabs)
        maps.append(m)
    res = run(nc, maps)
    zs = []
    for r in res:
        a = np.asarray(r["z2"])
        zs.append(a.transpose(1, 3, 0, 2).reshape(16384, 128))
    return np.concatenate(zs, axis=1)[:L]


def MMx(p, out, lhsT, rhs, start, stop, reads, writes):
    return p.group("pe", [lambda e: e.matmul(out, lhsT, rhs, start=start, stop=stop)], reads, writes)


def rms_mod_tile(p, C, xs, bx, w, gm, bgm, sh, bsh, obf, bobf, of=None, bof=None, eps=1e-6, D=1024):
    sq, bsq = C.sq.get()
    for kc in range(8):
        ACT(p, sq[:, kc, 0:w], xs(kc), AF.Square, [bx], [bsq])
    ps, bps = C.ps.get()
    MM(p, ps[:, 0:w], [(C.ones[:], sq[:, kc, 0:w]) for kc in range(8)], [bsq, C.bones], [bps])
    rs, brs = C.rs.get()
    ACT(p, rs[:, 0:w], ps[:, 0:w], AF.Sqrt, [bps], [brs], scale=1.0 / D, bias=eps)
    p.op("dve", lambda e: e.reciprocal(out=rs[:, 0:w], in_=rs[:, 0:w]), [brs], [brs])
    for kc in range(8):
        tmp, btmp = C.tmp.get()
        STT(p, tmp[:, 0:w], xs(kc), gm[:, kc:kc + 1], rs[:, 0:w], ALU.mult, ALU.mult, [bx, brs, bgm], [btmp])
        ACT(p, obf(kc), tmp[:, 0:w], AF.Identity, [btmp, bsh], [bobf], bias=sh[:, kc:kc + 1], scale=1.0)
        if of is not None:
            ACT(p, of(kc), tmp[:, 0:w], AF.Identity, [btmp, bsh], [bof], bias=sh[:, kc:kc + 1], scale=1.0)


def build_L3(T, has_bias):
    nc = bass.Bass("TRN2", target_bir_lowering=False)
    dz = nc.dram_tensor("zT", [128, 8, T], BF16, kind="ExternalInput").ap()
    dx = nc.dram_tensor("xT", [128, 8, T], F32, kind="ExternalInput").ap()
    dw = nc.dram_tensor("wout", [1024, 1024], F32, kind="ExternalInput").ap()
    db = nc.dram_tensor("bout", [128, 8], F32, kind="ExternalInput").ap()
    dmd = nc.dram_tensor("md", [128, 2, 6, 8], F32, kind="ExternalInput").ap()
    dg = nc.dram_tensor("g", [128, 8], F32, kind="ExternalInput").ap()
    dwr = nc.dram_tensor("wr", [128, 8, 16], F32, kind="ExternalInput").ap()
    dones = nc.dram_tensor("ones", [128, 128], F32, kind="ExternalInput").ap()
    ox = nc.dram_tensor("x1T", [128, 8, T], F32, kind="ExternalOutput").ap()
    oh = nc.dram_tensor("h2T", [128, 8, T], BF16, kind="ExternalOutput").ap()
    oa = nc.dram_tensor("aff", [T, 16], F32, kind="ExternalOutput").ap()
    tiles = tiles_of(T)
    with ExitStack() as es:
        p = Prog(nc, es)
        C = make_common(p, nc, dones)
        mdt, bmd = LOAD(p, "sp", [128, 2, 6, 8], F32, "md", dmd)
        gt, bg = LOAD(p, "sp", [128, 8], F32, "g", dg)
        bt, bb = LOAD(p, "sp", [128, 8], F32, "bo", db)
        wr, bwr = LOAD(p, "sp", [128, 8, 16], F32, "wr", dwr)
        W = p.sb("W", [128, 8, 1024], BF16); bW = p.buf()
        for kc in range(8):
            p.dma("pool", W[:, kc, :], dw[kc * 128:(kc + 1) * 128, :], writes=[bW])
        X = TT(p, "X", [128, 8, T], F32, len(tiles))
        Z = TT(p, "Z", [128, 8, T], BF16, len(tiles))
        for ti, (c0, w) in enumerate(tiles):
            p.dma("sp", Z.t[:, :, c0:c0 + w], dz[:, :, c0:c0 + w], writes=[Z.b[ti]])
            p.dma("sp", X.t[:, :, c0:c0 + w], dx[:, :, c0:c0 + w], writes=[X.b[ti]])
        gmx, bgmx = emit_gmod(p, "gmx", gt[:], bg, mdt[:, 0, 4, :], bmd)
        gmc, bgmc = emit_gmod(p, "gmc", gt[:], bg, mdt[:, 1, 4, :], bmd)
        Hb = Pool_(p, "Hb", [128, 8, 512], BF16, 2)
        Hf = Pool_(p, "Hf", [128, 8, 512], F32, 2)
        psL = Pool_(p, "psL", [128, 512], F32, 2, psum=True)
        sm = Pool_(p, "sm", [128, 40], F32, 3)
        for ti, (c0, w) in enumerate(tiles):
            cond = 1 if c0 >= TX else 0
            for m in range(8):
                ps, bps = C.ps.get()
                MM(p, ps[:, 0:w], [(W[:, kc, m * 128:(m + 1) * 128], Z.t[:, kc, c0:c0 + w]) for kc in range(8)], [bW, Z.b[ti]], [bps])
                if has_bias:
                    tmp, btmp = C.tmp.get()
                    ACT(p, tmp[:, 0:w], ps[:, 0:w], AF.Identity, [bps, bb], [btmp], bias=bt[:, m:m + 1], scale=1.0)
                    src, bsrc = tmp, btmp
                else:
                    src, bsrc = ps, bps
                STT(p, X.t[:, m, c0:c0 + w], src[:, 0:w], mdt[:, cond, 2, m:m + 1], X.t[:, m, c0:c0 + w], ALU.mult, ALU.add,
                    [bsrc, bmd, X.b[ti]], [X.b[ti]])
            p.dma("sp", ox[:, :, c0:c0 + w], X.t[:, :, c0:c0 + w], reads=[X.b[ti]])
            hb, bhb = Hb.get()
            hf, bhf = Hf.get()
            gm, bgm = (gmc, bgmc) if cond else (gmx, bgmx)
            rms_mod_tile(p, C, lambda kc: X.t[:, kc, c0:c0 + w], X.b[ti], w, gm, bgm, mdt[:, cond, 3, :], bmd,
                         lambda kc: hb[:, kc, 0:w], bhb, lambda kc: hf[:, kc, 0:w], bhf)
            p.dma("sp", oh[:, :, c0:c0 + w], hb[:, :, 0:w], reads=[bhb])
            for j0 in range(0, w, 128):
                tw = min(128, w - j0)
                pl, bpl = psL.get()
                MM(p, pl[0:tw, 0:16], [(hf[:, kc, j0:j0 + tw], wr[:, kc, :]) for kc in range(8)], [bhf, bwr], [bpl])
                s_, bs_ = sm.get()
                p.op("dve", lambda e, s_=s_, pl=pl, tw=tw: e.tensor_reduce(out=s_[0:tw, 32:33], in_=pl[0:tw, 0:16], axis=AX.X, op=ALU.max),
                     [bpl], [bs_])
                TS(p, "dve", s_[0:tw, 33:34], s_[0:tw, 32:33], -1.0, None, ALU.mult, None, [bs_], [bs_])
                ACT(p, s_[0:tw, 0:16], pl[0:tw, 0:16], AF.Exp, [bpl, bs_], [bs_], bias=s_[0:tw, 33:34], scale=1.0,
                    accum_out=s_[0:tw, 34:35])
                p.op("dve", lambda e, s_=s_, tw=tw: e.reciprocal(out=s_[0:tw, 35:36], in_=s_[0:tw, 34:35]), [bs_], [bs_])
                TS(p, "dve", s_[0:tw, 16:32], s_[0:tw, 0:16], s_[0:tw, 35:36], None, ALU.mult, None, [bs_], [bs_])
                p.dma("sp", oa[c0 + j0:c0 + j0 + tw, :], s_[0:tw, 16:32], reads=[bs_])
        p.emit()
    return nc


def run_L3(zsl, xsl, wout, bout, mods, l, g, wr, T, has_bias):
    nc = build_L3(T, has_bias)
    md = mods_fm(mods, l)
    wrl = np.ascontiguousarray(wr.reshape(8, 128, 16).transpose(1, 0, 2))
    bo = vec_fm(bout) if bout is not None else np.zeros((128, 8), np.float32)
    maps = [{"zT": zsl[c], "xT": xsl[c], "wout": wout, "bout": bo, "md": md, "g": vec_fm(g), "wr": wrl, "ones": ONES_F32}
            for c in range(NCORE)]
    res = run(nc, maps)
    return [r["x1T"] for r in res], [r["h2T"] for r in res], [np.asarray(r["aff"]) for r in res]


def emit_bisect(p, name, a, ba, n, cap, Gm, bGm, psb, iters=30):
    st = p.sb(name + "_st", [128, 8], F32); bst = p.buf()
    cmp_ = p.sb(name + "_cmp", [128, n], BF16); bcmp = p.buf()
    p.op("dve", lambda e: e.memset(st[:, 0:1], 0.0), [], [bst])
    p.op("dve", lambda e: e.memset(st[:, 1:2], 1.0), [bst], [bst])
    p.op("dve", lambda e: e.memset(st[:, 2:3], 0.5), [bst], [bst])
    p.op("dve", lambda e: e.memset(st[:, 3:5], 0.0), [bst], [bst])
    for it in range(iters):
        p.op("dve", lambda e: e.tensor_scalar(out=cmp_[:], in0=a, scalar1=st[:, 2:3], scalar2=0.0, op0=ALU.is_ge, op1=ALU.add,
                                               accum_out=st[:, 3:4]), [ba, bst], [bcmp, bst])
        ps, bps = psb
        MM(p, ps[:, 0:2], [(Gm, st[:, 3:5])], [bGm, bst], [bps])
        TS(p, "dve", st[:, 5:6], ps[:, 0:1], float(cap) - 0.5, None, ALU.is_ge, None, [bps, bst], [bst])
        TT_(p, "dve", st[:, 6:7], st[:, 2:3], st[:, 0:1], ALU.subtract, [bst], [bst])
        STT(p, st[:, 0:1], st[:, 6:7], st[:, 5:6], st[:, 0:1], ALU.mult, ALU.add, [bst], [bst])
        TT_(p, "dve", st[:, 6:7], st[:, 1:2], st[:, 2:3], ALU.subtract, [bst], [bst])
        STT(p, st[:, 1:2], st[:, 6:7], st[:, 5:6], st[:, 2:3], ALU.mult, ALU.add, [bst], [bst])
        TT_(p, "dve", st[:, 6:7], st[:, 0:1], st[:, 1:2], ALU.add, [bst], [bst])
        TS(p, "dve", st[:, 2:3], st[:, 6:7], 0.5, None, ALU.mult, None, [bst], [bst])
    return st[:, 0:1], bst


def moe_consts():
    Gm = np.zeros((128, 128), np.float32)
    for k in range(128):
        Gm[k, (k // 8) * 8:(k // 8) * 8 + 8] = 1.0
    Sel = np.zeros((128, 16, 128), np.float32)
    for e in range(16):
        Sel[e * 8, e, :] = 1.0
    return Gm, Sel


def build_L4(T, has_ctx):
    nc = bass.Bass("TRN2", target_bir_lowering=False)
    dx = nc.dram_tensor("xT", [128, 8, T], F32, kind="ExternalInput").ap()
    dh = nc.dram_tensor("hT", [128, 8, T], BF16, kind="ExternalInput").ap()
    daf = nc.dram_tensor("afull", [128, 2048], F32, kind="ExternalInput").ap()
    dac = nc.dram_tensor("acfull", [128, 32], F32, kind="ExternalInput").ap()
    dao = nc.dram_tensor("aown", [128, T], F32, kind="ExternalInput").ap()
    dmd = nc.dram_tensor("md", [128, 2, 6, 8], F32, kind="ExternalInput").ap()
    dGm = nc.dram_tensor("Gm", [128, 128], F32, kind="ExternalInput").ap()
    dSel = nc.dram_tensor("Sel", [128, 16, 128], F32, kind="ExternalInput").ap()
    dwg = nc.dram_tensor("wg", [16, 1024, 1024], F32, kind="ExternalInput").ap()
    dwu = nc.dram_tensor("wu", [16, 1024, 1024], F32, kind="ExternalInput").ap()
    dwd = nc.dram_tensor("wd", [16, 1024, 1024], F32, kind="ExternalInput").ap()
    ox = nc.dram_tensor("x2T", [128, 8, T], F32, kind="ExternalOutput").ap()
    tiles = tiles_of(T)
    with ExitStack() as es:
        p = Prog(nc, es)
        mdt, bmd = LOAD(p, "sp", [128, 2, 6, 8], F32, "md", dmd)
        Gm, bGm = LOAD(p, "sp", [128, 128], F32, "Gm", dGm)
        Sel, bSel = LOAD(p, "sp", [128, 16, 128], F32, "Sel", dSel)
        af, baf = LOAD(p, "sp", [128, 2048], F32, "af", daf)
        ao, bao = LOAD(p, "sp", [128, T], F32, "ao", dao)
        X = TT(p, "X", [128, 8, T], F32, len(tiles))
        H = TT(p, "H", [128, 8, T], BF16, len(tiles))
        for ti, (c0, w) in enumerate(tiles):
            p.dma("sp", H.t[:, :, c0:c0 + w], dh[:, :, c0:c0 + w], writes=[H.b[ti]])
            p.dma("sp", X.t[:, :, c0:c0 + w], dx[:, :, c0:c0 + w], writes=[X.b[ti]])
        psb = Pool_(p, "psb", [128, 512], F32, 1, psum=True)
        tau, btau = emit_bisect(p, "bx", af[:], baf, 2048, 2048, Gm[:], bGm, (psb.ts[0], psb.bs[0]))
        gw, bgw = ao, bao
        STT(p, gw[:, 0:TX], ao[:, 0:TX], tau, ao[:, 0:TX], ALU.is_ge, ALU.mult, [bao, btau], [bgw])
        if has_ctx:
            ac, bac = LOAD(p, "sp", [128, 32], F32, "ac", dac)
            tauc, btauc = emit_bisect(p, "bc", ac[:], bac, 32, 32, Gm[:], bGm, (psb.ts[0], psb.bs[0]))
            STT(p, gw[:, TX:T], ao[:, TX:T], tauc, ao[:, TX:T], ALU.is_ge, ALU.mult, [bao, btauc, bgw], [bgw])
        Wp = Pool_(p, "W", [128, 8, 1024], BF16, 4)
        Ap = Pool_(p, "A", [128, 8, 512], BF16, 1)
        sgp = Pool_(p, "sg", [128, 512], BF16, 2)
        atp = Pool_(p, "at", [128, 512], BF16, 2)
        gbp = Pool_(p, "gb", [128, 512], BF16, 2)
        psG = Pool_(p, "psG", [128, 512], F32, 4, psum=True)
        psD = Pool_(p, "psD", [128, 512], F32, 2, psum=True)
        psB = Pool_(p, "psB", [128, 512], F32, 1, psum=True)

        def loadw(src):
            wt, bw = Wp.get()
            for kc in range(8):
                p.dma("pool", wt[:, kc, :], src[kc * 128:(kc + 1) * 128, :], writes=[bw])
            return wt, bw

        for e in range(16):
            Wg, bWg = loadw(dwg[e])
            Wu, bWu = loadw(dwu[e])
            Wd, bWd = loadw(dwd[e])
            for ti, (c0, w) in enumerate(tiles):
                cond = 1 if c0 >= TX else 0
                pb, bpb = psB.get()
                MM(p, pb[:, 0:w], [(Sel[:, e, :], gw[:, c0:c0 + w])], [bSel, bgw], [bpb])
                gb, bgb = gbp.get()
                CP(p, "act", gb[:, 0:w], pb[:, 0:w], [bpb], [bgb])
                A_, bA = Ap.get()
                for fc in range(8):
                    pg, bpg = psG.get()
                    MM(p, pg[:, 0:w], [(Wg[:, kc, fc * 128:(fc + 1) * 128], H.t[:, kc, c0:c0 + w]) for kc in range(8)],
                       [bWg, H.b[ti]], [bpg])
                    sg, bsg = sgp.get()
                    ACT(p, sg[:, 0:w], pg[:, 0:w], AF.Silu, [bpg], [bsg])
                    pu, bpu = psG.get()
                    MM(p, pu[:, 0:w], [(Wu[:, kc, fc * 128:(fc + 1) * 128], H.t[:, kc, c0:c0 + w]) for kc in range(8)],
                       [bWu, H.b[ti]], [bpu])
                    at, bat = atp.get()
                    TT_(p, "dve", at[:, 0:w], pu[:, 0:w], sg[:, 0:w], ALU.mult, [bpu, bsg], [bat])
                    TT_(p, "dve", A_[:, fc, 0:w], at[:, 0:w], gb[:, 0:w], ALU.mult, [bat, bgb], [bA])
                for dc in range(8):
                    pd, bpd = psD.get()
                    MM(p, pd[:, 0:w], [(Wd[:, fc, dc * 128:(dc + 1) * 128], A_[:, fc, 0:w]) for fc in range(8)], [bWd, bA], [bpd])
                    STT(p, X.t[:, dc, c0:c0 + w], pd[:, 0:w], mdt[:, cond, 5, dc:dc + 1], X.t[:, dc, c0:c0 + w], ALU.mult, ALU.add,
                        [bpd, bmd, X.b[ti]], [X.b[ti]])
        for ti, (c0, w) in enumerate(tiles):
            p.dma("sp", ox[:, :, c0:c0 + w], X.t[:, :, c0:c0 + w], reads=[X.b[ti]])
        p.emit()
    return nc


def grp_layout(aT):
    n = aT.shape[1]
    return np.ascontiguousarray(aT.reshape(16, 8, n // 8).reshape(128, n // 8))


def run_L4(xsl, hsl, affs, mods, l, wg, wu, wd, T, has_ctx, nc=None):
    if nc is None:
        nc = build_L4(T, has_ctx)
    md = mods_fm(mods, l)
    Gm, Sel = moe_consts()
    ax = np.concatenate([a[:TX] for a in affs], 0)
    afull = grp_layout(np.ascontiguousarray(ax.T))
    if has_ctx:
        ac = np.concatenate([a[TX:] for a in affs], 0)
        acfull = grp_layout(np.ascontiguousarray(ac.T))
    else:
        acfull = np.zeros((128, 32), np.float32)
    maps = []
    for c in range(NCORE):
        aown = np.ascontiguousarray(np.repeat(affs[c].T, 8, axis=0))
        maps.append({"xT": xsl[c], "hT": hsl[c], "afull": afull, "acfull": acfull, "aown": aown, "md": md, "Gm": Gm, "Sel": Sel,
                     "wg": wg, "wu": wu, "wd": wd})
    res = run(nc, maps)
    return [r["x2T"] for r in res]


def rope_consts(core):
    t = core * TX + np.arange(TX)
    row = (t // 64).astype(np.float32)
    col = (t % 64).astype(np.float32)
    inv = (10000.0 ** (-np.arange(0, 32, 2, dtype=np.float32) / 32)).astype(np.float32)
    C = np.zeros((128, TX), np.float32)
    S = np.zeros((128, TX), np.float32)
    for p_ in range(128):
        d = p_ % 64
        pos = row if d < 32 else col
        i = (d % 32) % 16
        ang = pos * inv[i]
        C[p_] = np.cos(ang)
        S[p_] = np.sin(ang)
    PT = np.zeros((128, 128), np.float32)
    for m in range(128):
        if (m % 32) < 16:
            PT[m + 16, m] = -1.0
        else:
            PT[m - 16, m] = 1.0
    BD = np.zeros((128, 128), np.float32)
    BD[:64, :64] = 1.0
    BD[64:, 64:] = 1.0
    return C, S, PT, BD


def build_L5(T):
    nc = bass.Bass("TRN2", target_bir_lowering=False)
    dx = nc.dram_tensor("xT", [128, 8, T], F32, kind="ExternalInput").ap()
    dmd = nc.dram_tensor("md", [128, 2, 6, 8], F32, kind="ExternalInput").ap()
    dg = nc.dram_tensor("g", [128, 8], F32, kind="ExternalInput").ap()
    dw = nc.dram_tensor("wqkv", [1024, 3072], F32, kind="ExternalInput").ap()
    dqk = nc.dram_tensor("qkg", [128, 2], F32, kind="ExternalInput").ap()
    dC = nc.dram_tensor("ropeC", [128, TX], F32, kind="ExternalInput").ap()
    dS = nc.dram_tensor("ropeS", [128, TX], F32, kind="ExternalInput").ap()
    dPT = nc.dram_tensor("PT", [128, 128], F32, kind="ExternalInput").ap()
    dBD = nc.dram_tensor("BD", [128, 128], F32, kind="ExternalInput").ap()
    dones = nc.dram_tensor("ones", [128, 128], F32, kind="ExternalInput").ap()
    oq = nc.dram_tensor("qT", [128, 8, TX], BF16, kind="ExternalOutput").ap()
    ok = nc.dram_tensor("kT", [128, 8, T], BF16, kind="ExternalOutput").ap()
    ov = nc.dram_tensor("v", [T, 1024], BF16, kind="ExternalOutput").ap()
    tiles = tiles_of(T)
    with ExitStack() as es:
        p = Prog(nc, es)
        C = make_common(p, nc, dones, nps=2)
        mdt, bmd = LOAD(p, "sp", [128, 2, 6, 8], F32, "md", dmd)
        gt, bg = LOAD(p, "sp", [128, 8], F32, "g", dg)
        qkg, bqkg = LOAD(p, "sp", [128, 2], F32, "qkg", dqk)
        RC, bRC = LOAD(p, "sp", [128, TX], F32, "RC", dC)
        RS, bRS = LOAD(p, "sp", [128, TX], F32, "RS", dS)
        PT, bPT = LOAD(p, "sp", [128, 128], F32, "PT", dPT)
        BD, bBD = LOAD(p, "pool", [128, 128], BF16, "BD", dBD)
        X = TT(p, "X", [128, 8, T], F32, len(tiles))
        H = TT(p, "H", [128, 8, T], BF16, len(tiles))
        for ti, (c0, w) in enumerate(tiles):
            p.dma("sp", X.t[:, :, c0:c0 + w], dx[:, :, c0:c0 + w], writes=[X.b[ti]])
        gmx, bgmx = emit_gmod(p, "gmx", gt[:], bg, mdt[:, 0, 1, :], bmd)
        gmc, bgmc = emit_gmod(p, "gmc", gt[:], bg, mdt[:, 1, 1, :], bmd)
        for ti, (c0, w) in enumerate(tiles):
            cond = 1 if c0 >= TX else 0
            gm, bgm = (gmc, bgmc) if cond else (gmx, bgmx)
            rms_mod_tile(p, C, lambda kc: X.t[:, kc, c0:c0 + w], X.b[ti], w, gm, bgm, mdt[:, cond, 0, :], bmd,
                         lambda kc: H.t[:, kc, c0:c0 + w], H.b[ti])
        qr = Pool_(p, "qr", [128, 512], F32, 3)
        sqp = Pool_(p, "sq1", [128, 512], BF16, 3)
        qn = Pool_(p, "qn", [128, 512], F32, 3)
        t1p = Pool_(p, "t1", [128, 512], F32, 2)
        t2p = Pool_(p, "t2", [128, 512], F32, 2)
        ob = Pool_(p, "ob", [128, 512], BF16, 3)
        ps2 = Pool_(p, "ps2", [128, 512], F32, 6, psum=True)
        Wqk = []
        for hh in range(2):
            wt_ = p.sb(f"Wqk{hh}", [128, 8, 1024], BF16)
            bw_ = p.buf()
            for kc in range(8):
                p.dma("pool", wt_[:, kc, :], dw[kc * 128:(kc + 1) * 128, hh * 1024:(hh + 1) * 1024], writes=[bw_])
            Wqk.append((wt_, bw_))
        for m in range(16):
            isq = m < 8
            wfull, bw = Wqk[m // 8]
            wt = wfull[:, :, (m % 8) * 128:(m % 8) * 128 + 128]
            gcol = qkg[:, 0:1] if isq else qkg[:, 1:2]
            for ti, (c0, w) in enumerate(tiles):
                isctx = c0 >= TX
                if isq and isctx:
                    continue
                ps, bps = ps2.get()
                MM(p, ps[:, 0:w], [(wt[:, kc, :], H.t[:, kc, c0:c0 + w]) for kc in range(8)], [bw, H.b[ti]], [bps])
                q_, bq_ = qr.get()
                CP(p, "act", q_[:, 0:w], ps[:, 0:w], [bps], [bq_])
                sq, bsq = sqp.get()
                ACT(p, sq[:, 0:w], ps[:, 0:w], AF.Square, [bps], [bsq])
                pss, bpss = ps2.get()
                MM(p, pss[:, 0:w], [(BD[:], sq[:, 0:w])], [bBD, bsq], [bpss])
                rs, brs = C.rs.get()
                ACT(p, rs[:, 0:w], pss[:, 0:w], AF.Sqrt, [bpss], [brs], scale=1.0 / 64, bias=1e-6)
                p.op("dve", lambda e, rs=rs, w=w: e.reciprocal(out=rs[:, 0:w], in_=rs[:, 0:w]), [brs], [brs])
                n_, bn_ = qn.get()
                STT(p, n_[:, 0:w], q_[:, 0:w], gcol, rs[:, 0:w], ALU.mult, ALU.mult, [bq_, bqkg, brs], [bn_])
                o_, bo_ = ob.get()
                if not isctx:
                    pr, bpr = ps2.get()
                    MM(p, pr[:, 0:w], [(PT[:], n_[:, 0:w])], [bPT, bn_], [bpr])
                    t1, b1 = t1p.get()
                    t2, b2 = t2p.get()
                    TT_(p, "pool", t1[:, 0:w], n_[:, 0:w], RC[:, c0:c0 + w], ALU.mult, [bn_, bRC], [b1])
                    TT_(p, "dve", t2[:, 0:w], pr[:, 0:w], RS[:, c0:c0 + w], ALU.mult, [bpr, bRS], [b2])
                    TT_(p, "pool", o_[:, 0:w], t1[:, 0:w], t2[:, 0:w], ALU.add, [b1, b2], [bo_])
                else:
                    CP(p, "act", o_[:, 0:w], n_[:, 0:w], [bn_], [bo_])
                if isq:
                    p.dma("sp", oq[:, m, c0:c0 + w], o_[:, 0:w], reads=[bo_])
                else:
                    p.dma("sp", ok[:, m - 8, c0:c0 + w], o_[:, 0:w], reads=[bo_])
        Wv, bWv = Wqk[0]
        for kc in range(8):
            p.dma("pool", Wv[:, kc, :], dw[kc * 128:(kc + 1) * 128, 2048:3072], writes=[bWv])
        vb = Pool_(p, "vb", [128, 512], BF16, 3)
        for ti, (c0, w) in enumerate(tiles):
            for j0 in range(0, w, 128):
                tw = min(128, w - j0)
                for hh in range(2):
                    ps, bps = ps2.get()
                    MM(p, ps[0:tw, :], [(H.t[:, kc, c0 + j0:c0 + j0 + tw], Wv[:, kc, hh * 512:(hh + 1) * 512]) for kc in range(8)],
                       [H.b[ti], bWv], [bps])
                    v_, bv_ = vb.get()
                    CP(p, "act", v_[0:tw, :], ps[0:tw, :], [bps], [bv_])
                    p.dma("sp", ov[c0 + j0:c0 + j0 + tw, hh * 512:(hh + 1) * 512], v_[0:tw, :], reads=[bv_])
        p.emit()
    return nc


def run_L5(inp, xsl, mods, T):
    nc = build_L5(T)
    md = mods_fm(mods, 1)
    qkg = np.ascontiguousarray(np.stack([np.tile(inp["da_q_norm"][0], 2), np.tile(inp["da_k_norm"][0], 2)], axis=1))
    maps = []
    for c in range(NCORE):
        C_, S_, PT, BD = rope_consts(c)
        maps.append({"xT": xsl[c], "md": md, "g": vec_fm(inp["norm_mix"][1]), "wqkv": inp["da_w_qkv"][0], "qkg": qkg,
                     "ropeC": C_, "ropeS": S_, "PT": PT, "BD": BD, "ones": ONES_F32})
    res = run(nc, maps)
    return [np.asarray(r["qT"]) for r in res], [np.asarray(r["kT"]) for r in res], [np.asarray(r["v"]) for r in res]


LAM_INIT = 0.8 - 0.6 * float(np.exp(-0.3 * 1))
NKEY = 16384 + 256
NKC = NKEY // 128


def build_L6():
    nc = bass.Bass("TRN2", target_bir_lowering=False)
    dq = nc.dram_tensor("qT", [128, 8, TX], BF16, kind="ExternalInput").ap()
    dk = nc.dram_tensor("kT", [8, 128, NKEY], BF16, kind="ExternalInput").ap()
    dv = nc.dram_tensor("v", [8, 128, NKC, 128], BF16, kind="ExternalInput").ap()
    dlam = nc.dram_tensor("lamv", [128, 4, 64], F32, kind="ExternalInput").ap()
    dgn = nc.dram_tensor("gains", [128, 2, 64], F32, kind="ExternalInput").ap()
    dsub = nc.dram_tensor("subln", [128, 1], F32, kind="ExternalInput").ap()
    dones = nc.dram_tensor("ones", [128, 128], F32, kind="ExternalInput").ap()
    oo_ = nc.dram_tensor("oT", [128, 8, TX], BF16, kind="ExternalOutput").ap()
    with ExitStack() as es:
        p = Prog(nc, es)
        ones, bones = LOAD(p, "pool", [128, 128], BF16, "ones", dones)
        lamv, blamv = LOAD(p, "sp", [128, 4, 64], F32, "lamv", dlam)
        gn, bgn = LOAD(p, "sp", [128, 2, 64], F32, "gn", dgn)
        st = p.sb("st", [128, 16], F32); bst = p.buf()
        p.dma("sp", st[:, 0:1], dsub, writes=[bst])
        sc = p.sb("scr", [128, 64], F32); bsc = p.buf()
        for i in range(2):
            TT_(p, "dve", sc[:], lamv[:, 2 * i, :], lamv[:, 2 * i + 1, :], ALU.mult, [blamv, bsc], [bsc])
            p.op("dve", lambda e, i=i: e.tensor_reduce(out=st[:, 1 + i:2 + i], in_=sc[:], axis=AX.X, op=ALU.add), [bsc, bst], [bst])
        ACT(p, st[:, 1:3], st[:, 1:3], AF.Exp, [bst], [bst])
        TT_(p, "dve", st[:, 3:4], st[:, 2:3], st[:, 1:2], ALU.subtract, [bst], [bst])
        TS(p, "dve", st[:, 3:4], st[:, 3:4], -LAM_INIT, None, ALU.add, None, [bst], [bst])
        for i in range(2):
            p.op("dve", lambda e, i=i: e.tensor_reduce(out=st[:, 4 + i:5 + i], in_=gn[:, i, :], axis=AX.X, op=ALU.max,
                                                       apply_absolute_value=True), [bgn, bst], [bst])
        TT_(p, "dve", st[:, 6:7], st[:, 4:5], st[:, 5:6], ALU.mult, [bst], [bst])
        TS(p, "dve", st[:, 6:7], st[:, 6:7], -8.0, None, ALU.mult, None, [bst], [bst])
        TS(p, "dve", st[:, 7:8], st[:, 0:1], 1.0 - LAM_INIT, None, ALU.mult, None, [bst], [bst])
        Q, bQ = LOAD(p, "sp", [128, 8, TX], BF16, "Q", dq)
        ones32, bones32 = LOAD(p, "sp", [128, 128], F32, "ones32", dones)
        Kp = Pool_(p, "K", [128, NKEY], BF16, 2)
        Vp = Pool_(p, "V", [128, NKC, 128], BF16, 2)
        Pp = Pool_(p, "P", [128, 1024], BF16, 3)
        psS = Pool_(p, "psS", [128, 1024], F32, 2, psum=True)
        psO = [p.ps(f"psO{i}", [128, 512], F32) for i in range(3)]
        bO = [p.buf() for _ in range(3)]
        psD = Pool_(p, "psD", [128, 512], F32, 1, psum=True)
        Dacc = p.sb("Dacc", [128, 512], F32)
        bDa = p.buf()
        ep = Pool_(p, "ep", [128, 512], F32, 6)
        sqp = Pool_(p, "sqe", [128, 512], BF16, 2)
        obp = Pool_(p, "obe", [128, 512], BF16, 2)
        for hp in range(8):
            Kh, bK = Kp.get()
            p.dma("sp", Kh[:], dk[hp], writes=[bK])
            Vh, bV = Vp.get()
            p.dma("sp", Vh[:], dv[hp], writes=[bV])
            for qt in range(TX // 512):
                q0 = qt * 512

                def S(kc):
                    ps, bps = psS.get()
                    for s_ in range(2):
                        lo = 64 * s_
                        MM(p, ps[:, 512 * s_:512 * s_ + 512],
                           [(Kh[lo:lo + 64, kc * 128:(kc + 1) * 128], Q[lo:lo + 64, hp, q0:q0 + 512])], [bK, bQ], [bps])
                    return ps, bps

                cur = S(0)
                for kc in range(NKC):
                    nxt = S(kc + 1) if kc + 1 < NKC else None
                    ps, bps = cur
                    P_, bP = Pp.get()
                    ACT(p, P_[:], ps[:, :], AF.Exp, [bps, bst], [bP], scale=0.125, bias=st[:, 6:7])
                    for s_ in range(2):
                        MMx(p, psO[s_][:, :], Vh[:, kc, :], P_[:, 512 * s_:512 * s_ + 512], kc == 0, kc == NKC - 1, [bV, bP], [bO[s_]])
                    MMx(p, psO[2][:, :], ones[:], P_[:, 0:512], kc == 0, kc == NKC - 1, [bones, bP], [bO[2]])
                    if kc == 0:
                        CP(p, "dve", Dacc[:], P_[:, 512:1024], [bP], [bDa])
                    else:
                        TT_(p, "dve", Dacc[:], Dacc[:], P_[:, 512:1024], ALU.add, [bP, bDa], [bDa])
                    cur = nxt
                ts_ = []
                for s_ in range(2):
                    if s_ == 0:
                        pd, bpd = psO[2], bO[2]
                    else:
                        pd, bpd = psD.get()
                        MM(p, pd[:, :], [(ones32[:], Dacc[:])], [bones32, bDa], [bpd])
                    r_, br_ = ep.get()
                    p.op("dve", lambda e, r_=r_, pd=pd: e.reciprocal(out=r_[:], in_=pd[:, :]), [bpd], [br_])
                    t_, bt_ = ep.get()
                    TT_(p, "dve", t_[:], psO[s_][:, :], r_[:], ALU.mult, [bO[s_], br_], [bt_])
                    ts_.append((t_, bt_))
                (t0, bt0), (t1, bt1) = ts_
                o_, bo_ = ep.get()
                STT(p, o_[:], t1[:], st[:, 3:4], t0[:], ALU.mult, ALU.add, [bt1, bt0, bst], [bo_])
                sq, bsq = sqp.get()
                ACT(p, sq[:], o_[:], AF.Square, [bo_], [bsq])
                pss, bpss = psD.get()
                MM(p, pss[:, :], [(ones[:], sq[:])], [bones, bsq], [bpss])
                rs, brs = ep.get()
                ACT(p, rs[:], pss[:, :], AF.Sqrt, [bpss], [brs], scale=1.0 / 128, bias=1e-5)
                p.op("dve", lambda e, rs=rs: e.reciprocal(out=rs[:], in_=rs[:]), [brs], [brs])
                ob, bob = obp.get()
                STT(p, ob[:], o_[:], st[:, 7:8], rs[:], ALU.mult, ALU.mult, [bo_, bst, brs], [bob])
                p.dma("sp", oo_[:, hp, q0:q0 + 512], ob[:], reads=[bob])
        p.emit()
    return nc


def run_L6(inp, q, k, v):
    nc = build_L6()
    kx = np.concatenate([a[:, :, :TX] for a in k], axis=2)
    kc = np.concatenate([a[:, :, TX:] for a in k], axis=2)
    kall = np.ascontiguousarray(np.concatenate([kc, kx], axis=2).transpose(1, 0, 2))
    vx = np.concatenate([a[:TX] for a in v], axis=0)
    vc = np.concatenate([a[TX:] for a in v], axis=0)
    vall = np.concatenate([vc, vx], axis=0).reshape(NKC, 128, 8, 128)
    vall = np.ascontiguousarray(vall.transpose(2, 1, 0, 3))
    lamv = np.ascontiguousarray(np.broadcast_to(np.stack([inp["da_lam_q1"][0], inp["da_lam_k1"][0], inp["da_lam_q2"][0],
                                                          inp["da_lam_k2"][0]])[None], (128, 4, 64)))
    gains = np.ascontiguousarray(np.broadcast_to(np.stack([inp["da_q_norm"][0], inp["da_k_norm"][0]])[None], (128, 2, 64)))
    sub = np.ascontiguousarray(inp["da_subln"][0].reshape(128, 1))
    maps = [{"qT": q[c], "kT": kall, "v": vall, "lamv": lamv, "gains": gains, "subln": sub, "ones": ONES_F32} for c in range(NCORE)]
    res = run(nc, maps)
    return [np.asarray(r["oT"]) for r in res]


def ctx_pos_tables():
    L, N = 256, 512
    t = np.linspace(0.0, 1.0, L, dtype=np.float32)[:, None]
    w = (2.0 * np.float32(np.pi) * np.arange(L, dtype=np.float32)[:, None] / np.float32(L)).astype(np.float32)
    bands = np.linspace(1e-4, 15, 16, dtype=np.float32)[None]
    z = np.concatenate([t, np.cos(w * bands), -np.sin(w * bands)], axis=-1).astype(np.float32)
    maxd = np.log(1e-2) / 0.3
    mind = np.log(1e-2) / 1.5
    deltas = np.linspace(mind, maxd, 1024, dtype=np.float32)
    decay = np.exp(-t * np.abs(deltas)).astype(np.float32)
    zext = np.zeros((N, 33), np.float32)
    dext = np.zeros((N, 1024), np.float32)
    zext[:L] = z
    dext[:L] = decay
    j = np.arange(1, L)
    zext[N - j] = z[j]
    dext[N - j] = decay[j]
    return zext, dext


def hcc_tables():
    import ml_dtypes
    bf = ml_dtypes.bfloat16
    n = (np.arange(4)[None, :, None] * 128 + np.arange(128)[:, None, None]).astype(np.float64)
    k = np.arange(512)[None, None, :].astype(np.float64)
    th = 2 * np.pi * n * k / 512
    return {"COS": np.cos(th).astype(bf), "SIN": np.sin(th).astype(bf), "NSIN": (-np.sin(th)).astype(bf),
            "ones32": np.ones((128, 128), np.float32)}


def build_HCc():
    nc = bass.Bass("TRN2", target_bir_lowering=False)
    dU = nc.dram_tensor("U", [128, 2, 3, 128], F32, kind="ExternalInput").ap()
    dhid = nc.dram_tensor("hid", [64, 512], BF16, kind="ExternalInput").ap()
    dw4 = nc.dram_tensor("w4", [64, 2, 256], F32, kind="ExternalInput").ap()
    ddec = nc.dram_tensor("dec", [128, 4, 128], F32, kind="ExternalInput").ap()
    dskip = nc.dram_tensor("skip", [128, 2, 128], F32, kind="ExternalInput").ap()
    dC = nc.dram_tensor("COS", [128, 4, 512], BF16, kind="ExternalInput").ap()
    dS = nc.dram_tensor("SIN", [128, 4, 512], BF16, kind="ExternalInput").ap()
    dNS = nc.dram_tensor("NSIN", [128, 4, 512], BF16, kind="ExternalInput").ap()
    dones = nc.dram_tensor("ones32", [128, 128], F32, kind="ExternalInput").ap()
    dout = nc.dram_tensor("z2", [128, 2, 128], BF16, kind="ExternalOutput").ap()
    with ExitStack() as es:
        p = Prog(nc, es)
        U, bU = LOAD(p, "sp", [128, 2, 3, 128], F32, "U", dU)
        Hd, bHd = LOAD(p, "sp", [64, 512], BF16, "Hd", dhid)
        W4, bW4 = LOAD(p, "pool", [64, 2, 256], BF16, "W4", dw4)
        Dc, bDc = LOAD(p, "sp", [128, 4, 128], F32, "Dc", ddec)
        SK, bSK = LOAD(p, "sp", [128, 2, 128], F32, "SK", dskip)
        COS, bC = LOAD(p, "sp", [128, 4, 512], BF16, "COS", dC)
        SIN, bS = LOAD(p, "sp", [128, 4, 512], BF16, "SIN", dS)
        NSIN, bNS = LOAD(p, "sp", [128, 4, 512], BF16, "NSIN", dNS)
        ON, bON = LOAD(p, "sp", [128, 128], F32, "ON", dones)
        psp = Pool_(p, "ps", [128, 512], F32, 6, psum=True)
        Kt = p.sb("Kt", [128, 4, 256], F32); bKt = p.buf()
        Kb = p.sb("Kb", [128, 4, 256], BF16); bKb = p.buf()
        for j in range(4):
            ps, bps = psp.get()
            MM(p, ps[:, 0:256], [(Hd[:, j * 128:(j + 1) * 128], W4[:, j // 2, :])], [bHd, bW4], [bps])
            TT_(p, "dve", Kt[:, j, :].rearrange("p (o c) -> p o c", o=2), ps[:, 0:256].rearrange("p (o c) -> p o c", o=2),
                Dc[:, j, :].unsqueeze(1).to_broadcast([128, 2, 128]), ALU.mult, [bps, bDc], [bKt])
        CP(p, "act", Kb[:], Kt[:], [bKt], [bKb])
        red = p.sb("red", [128, 256], F32); bred = p.buf()
        p.op("dve", lambda e: e.tensor_reduce(out=red[:], in_=Kt[:].rearrange("p j oc -> p oc j"), axis=AX.X, op=ALU.add,
                                               apply_absolute_value=True), [bKt], [bred])
        sN = p.sb("sN", [128, 256], F32); bsN = p.buf()
        ps, bps = psp.get()
        MM(p, ps[:, 0:256], [(ON[:], red[:])], [bON, bred], [bps])
        TS(p, "dve", sN[:], ps[:, 0:256], 1e-6, None, ALU.add, None, [bps], [bsN])
        p.op("dve", lambda e: e.reciprocal(out=sN[:], in_=sN[:]), [bsN], [bsN])
        TS(p, "dve", sN[:], sN[:], 1.0 / 512, None, ALU.mult, None, [bsN], [bsN])
        Kf = p.sb("Kf", [128, 4, 2, 256], F32); bKf = p.buf()
        for kc in range(4):
            for ri, TB, bTB in ((0, COS, bC), (1, NSIN, bNS)):
                ps, bps = psp.get()
                MM(p, ps[:, 0:256], [(TB[:, j, kc * 128:(kc + 1) * 128], Kb[:, j, :]) for j in range(4)], [bTB, bKb], [bps])
                CP(p, "act", Kf[:, kc, ri, :], ps[:, 0:256], [bps], [bKf])
        Zf = p.sb("Zf", [128, 2, 128], F32); bZf = p.buf()
        Zb = p.sb("Zb", [128, 2, 128], BF16); bZb = p.buf()
        G = p.sb("G", [128, 4, 2, 128], BF16); bG = p.buf()
        O = p.sb("O", [128, 2, 128], BF16); bO = p.buf()
        tp = Pool_(p, "t", [128, 128], F32, 4)
        CP(p, "act", Zf[:], U[:, :, 0, :], [bU], [bZf])
        for o in range(2):
            CP(p, "act", Zb[:], Zf[:], [bZf], [bZb])
            for kc in range(4):
                pr, bpr = psp.get()
                MM(p, pr[:, 0:128], [(COS[:, j, kc * 128:(kc + 1) * 128], Zb[:, j, :]) for j in range(2)], [bC, bZb], [bpr])
                pi, bpi = psp.get()
                MM(p, pi[:, 0:128], [(NSIN[:, j, kc * 128:(kc + 1) * 128], Zb[:, j, :]) for j in range(2)], [bNS, bZb], [bpi])
                kr = Kf[:, kc, 0, o * 128:(o + 1) * 128]
                ki = Kf[:, kc, 1, o * 128:(o + 1) * 128]
                t1, b1 = tp.get()
                t2, b2 = tp.get()
                TT_(p, "dve", t1[:], pr[:, 0:128], kr, ALU.mult, [bpr, bKf], [b1])
                TT_(p, "dve", t2[:], pi[:, 0:128], ki, ALU.mult, [bpi, bKf], [b2])
                TT_(p, "pool", G[:, kc, 0, :], t1[:], t2[:], ALU.subtract, [b1, b2], [bG])
                t1, b1 = tp.get()
                t2, b2 = tp.get()
                TT_(p, "dve", t1[:], pr[:, 0:128], ki, ALU.mult, [bpr, bKf], [b1])
                TT_(p, "dve", t2[:], pi[:, 0:128], kr, ALU.mult, [bpi, bKf], [b2])
                TT_(p, "pool", G[:, kc, 1, :], t1[:], t2[:], ALU.add, [b1, b2], [bG])
            for i in range(2):
                py, bpy = psp.get()
                pairs = []
                for j in range(4):
                    pairs.append((COS[:, j, i * 128:(i + 1) * 128], G[:, j, 0, :]))
                    pairs.append((NSIN[:, j, i * 128:(i + 1) * 128], G[:, j, 1, :]))
                MM(p, py[:, 0:128], pairs, [bC, bNS, bG], [bpy])
                t1, b1 = tp.get()
                t2, b2 = tp.get()
                TT_(p, "dve", t1[:], py[:, 0:128], sN[:, o * 128:(o + 1) * 128], ALU.mult, [bpy, bsN], [b1])
                TT_(p, "pool", t2[:], Zf[:, i, :], SK[:, o, :], ALU.mult, [bZf, bSK], [b2])
                TT_(p, "pool", t2[:], t2[:], t1[:], ALU.add, [b1, b2], [b2])
                if o == 0:
                    TT_(p, "dve", Zf[:, i, :], t2[:], U[:, i, 1, :], ALU.mult, [b2, bU, bZb], [bZf])
                else:
                    TT_(p, "dve", O[:, i, :], t2[:], U[:, i, 2, :], ALU.mult, [b2, bU], [bO])
        p.dma("sp", dout, O[:], reads=[bO])
        p.emit()
    return nc


def run_LFc(inp, zext512):
    P = 512 // NCORE
    nc = build_LF(P)
    zp = np.ascontiguousarray(zext512.T)
    w23 = np.ascontiguousarray(np.stack([inp["hy_f_w2"][0], inp["hy_f_w3"][0]], axis=1))
    bf = np.ascontiguousarray(np.stack([inp["hy_f_b1"][0], inp["hy_f_b2"][0], inp["hy_f_b3"][0], inp["hy_f_freq"][0]], axis=1))
    maps = [{"zT": np.ascontiguousarray(zp[:, c * P:(c + 1) * P]), "w1": inp["hy_f_w1"][0], "w23": w23, "bf": bf}
            for c in range(NCORE)]
    res = run(nc, maps)
    return np.concatenate([np.asarray(r["hid"]) for r in res], axis=1)


def run_HCc(inp, uc, hid512, dext512):
    nc = build_HCc()
    tabs = hcc_tables()
    w4 = inp["hy_f_w4"][0].reshape(64, 2, 2, 1024)
    sk = inp["hy_skip"][0]
    maps = []
    for c in range(NCORE):
        ch = slice(c * 128, (c + 1) * 128)
        U = np.ascontiguousarray(uc.reshape(2, 128, 3, 1024)[:, :, :, ch].transpose(1, 0, 2, 3))
        W = np.ascontiguousarray(w4[:, :, :, ch].transpose(0, 2, 1, 3).reshape(64, 2, 256))
        D = np.ascontiguousarray(dext512[:, ch].reshape(4, 128, 128).transpose(1, 0, 2))
        S = np.ascontiguousarray(np.broadcast_to(sk[:, ch][None], (128, 2, 128)))
        m = {"U": U, "hid": hid512, "w4": W, "dec": D, "skip": S}
        m.update(tabs)
        maps.append(m)
    res = run(nc, maps)
    zs = [np.asarray(r["z2"]).transpose(1, 0, 2).reshape(256, 128) for r in res]
    return np.concatenate(zs, axis=1)


def kernel(**inp):
    inp = {k: np.asarray(v) for k, v in inp.items()}
    x = inp["x"][0]
    ctx = inp["ctx"][0]
    T0 = TX + TC
    mods = run_L0(inp)
    xsl = shard_tokens(x, ctx)
    h = run_L1(xsl, mods, 0, inp["norm_mix"][0], T0)
    hx, hc = unshard_tokens([np.asarray(a) for a in h], True)
    u, uc = run_HA(inp, hx, hc)
    tabs = hc_tables()
    nc_hc = build_HC()
    zext, dext = hyena_pos_tables(16384)
    hid = run_LF(inp, zext)
    z = run_HC(inp, u, hid, dext, tabs, nc=nc_hc)
    zext_c, dext_c = ctx_pos_tables()
    hid_c = run_LFc(inp, zext_c)
    zc = run_HCc(inp, uc, hid_c, dext_c)
    zsl = shard_tokens(np.asarray(z), np.asarray(zc))
    x1, h2, aff = run_L3(zsl, xsl, inp["hy_w_out"][0], inp["hy_b_out"][0], mods, 0, inp["norm_ffn"][0],
                         inp["moe_router"][0], T0, True)
    x2 = run_L4([np.asarray(a) for a in x1], [np.asarray(a) for a in h2], aff, mods, 0,
                inp["moe_w_gate"][0], inp["moe_w_up"][0], inp["moe_w_down"][0], T0, True)
    x2 = [np.asarray(a) for a in x2]
    q, k, v = run_L5(inp, x2, mods, T0)
    o = run_L6(inp, q, k, v)
    xs1 = [np.ascontiguousarray(a[:, :, :TX]) for a in x2]
    x3, h3, aff1 = run_L3(o, xs1, inp["da_w_out"][0], None, mods, 1, inp["norm_ffn"][1], inp["moe_router"][1], TX, False)
    x4 = run_L4([np.asarray(a) for a in x3], [np.asarray(a) for a in h3], aff1, mods, 1,
                inp["moe_w_gate"][1], inp["moe_w_up"][1], inp["moe_w_down"][1], TX, False)
    xo, _ = unshard_tokens([np.asarray(a) for a in x4], False)
    return np.ascontiguousarray(xo[None]).astype(np.float32)
```
